# Optimizing a Trainium2 kernel written in Bass

```python
import jax, jax.numpy as jnp
from jax import lax
import numpy as np

D_MODEL = 1024
BATCH = 4
SEQ = 4096
DEPTH = 2

N_MEM = 256
EPS = 1e-6
SC_WIDTH = D_MODEL // 2
SC_GROUPS = 8
CONV_K = 3
POOL_WIDTH = D_MODEL // 2
POOL_WINDOWS = (2, 4, 8, 16)
POOL_GROUP = POOL_WIDTH // len(POOL_WINDOWS)
EVEN_IN = 3 * SC_WIDTH + POOL_WIDTH
EVEN_MIX = SC_WIDTH + POOL_WIDTH
ML_HEADS = 4
ML_HEAD_DIM = 128
ML_WIDTH = ML_HEADS * ML_HEAD_DIM
ML_CHUNK = 64
MLA_HEADS = 8
QK_NOPE = 64
QK_ROPE = 32
V_DIM = 64
Q_LORA = 384
KV_LORA = 256
MLA_WIDTH = MLA_HEADS * V_DIM
Q_BLOCK = 128
ROPE_THETA = 10000.0
ODD_SIZES = (ML_WIDTH, ML_WIDTH, ML_WIDTH, ML_WIDTH, 2 * ML_HEADS, Q_LORA, KV_LORA, QK_ROPE)
ODD_SPLIT = tuple(int(v) for v in np.cumsum(ODD_SIZES)[:-1])
ODD_IN = sum(ODD_SIZES)
ODD_MIX = ML_WIDTH + MLA_WIDTH
XA_HEADS = 4
XA_HEAD_DIM = 128
XA_WIDTH = XA_HEADS * XA_HEAD_DIM
FFN_HIDDEN = ((8 * D_MODEL + 3 * 256 - 1) // (3 * 256)) * 256
N_EVEN = (DEPTH + 1) // 2
N_ODD = DEPTH // 2

kernel_name = 'hybrid_conv_pool_mlstm_mla_trunk'


def rmsnorm(x, g):
    xf = x.astype(jnp.float32)
    y = xf * lax.rsqrt(jnp.mean(xf * xf, axis=-1, keepdims=True) + EPS)
    return (y * g.astype(jnp.float32)).astype(x.dtype)


def causal_dwconv(u, w):
    c = u.shape[-1]
    return lax.conv_general_dilated(u, w[:, None, :].astype(u.dtype), window_strides=(1,),
                                    padding=[(CONV_K - 1, 0)],
                                    dimension_numbers=('NWC', 'WIO', 'NWC'),
                                    feature_group_count=c)


def causal_pool_mixer(u, pool_w, pool_scale):
    B, S, _ = u.shape
    ug = u.reshape(B, S, len(POOL_WINDOWS), POOL_GROUP)
    csum = jnp.cumsum(ug.astype(jnp.float32), axis=1)
    t1 = jnp.arange(1, S + 1)
    means = []
    for gi, w in enumerate(POOL_WINDOWS):
        cg = csum[:, :, gi]
        shifted = jnp.pad(cg, ((0, 0), (w, 0), (0, 0)))[:, :S]
        cnt = jnp.minimum(t1, w).astype(jnp.float32)
        means.append((cg - shifted) / cnt[None, :, None])
    pooled = jnp.stack(means, axis=2).astype(u.dtype)
    y = jnp.einsum('bsgc,gcd->bsgd', pooled - ug, pool_w)
    return y.reshape(B, S, POOL_WIDTH) * pool_scale


def even_mixer(h, w_in, conv_w, pool_w, pool_scale, w_out):
    z = h @ w_in
    g_b, g_c, xa, xb = jnp.split(z, [SC_WIDTH, 2 * SC_WIDTH, 3 * SC_WIDTH], axis=-1)
    ya = g_b * causal_dwconv(g_c * xa, conv_w)
    yb = causal_pool_mixer(xb, pool_w, pool_scale)
    return jnp.concatenate([ya, yb], axis=-1) @ w_out


def mlstm_chunkwise(q, k, v, i_pre, f_pre):
    B, S, H, D = q.shape
    L = ML_CHUNK
    NC = S // L

    def chunk(t):
        return t.reshape(B, NC, L, H, D).transpose(0, 3, 1, 2, 4)

    def chunk_g(t):
        return t.reshape(B, NC, L, H).transpose(0, 3, 1, 2)

    qc, kc, vc = chunk(q), chunk(k) * (D ** -0.5), chunk(v)
    ig = chunk_g(i_pre)
    bcum = jnp.cumsum(chunk_g(jax.nn.log_sigmoid(f_pre)), axis=-1)
    gtot = bcum[..., -1]

    a = gtot[..., None] - bcum + ig
    m_loc = jnp.max(a, axis=-1)
    w_loc = jnp.exp(a - m_loc[..., None])
    C_loc = jnp.einsum('bhcl,bhclv,bhclk->bhcvk', w_loc, vc, kc)
    n_loc = jnp.einsum('bhcl,bhclk->bhck', w_loc, kc)

    def step(carry, xs):
        C, n, m = carry
        g_c, m_l, C_l, n_l = xs
        m_new = jnp.maximum(g_c + m, m_l)
        s_old = jnp.exp(g_c + m - m_new)
        s_new = jnp.exp(m_l - m_new)
        C_new = s_old[..., None, None] * C + s_new[..., None, None] * C_l
        n_new = s_old[..., None] * n + s_new[..., None] * n_l
        return (C_new, n_new, m_new), (C, n, m)

    init = (jnp.zeros((B, H, D, D), jnp.float32), jnp.zeros((B, H, D), jnp.float32),
            jnp.zeros((B, H), jnp.float32))
    xs = (jnp.moveaxis(gtot, 2, 0), jnp.moveaxis(m_loc, 2, 0),
          jnp.moveaxis(C_loc, 2, 0), jnp.moveaxis(n_loc, 2, 0))
    _, (C_prev, n_prev, m_prev) = lax.scan(step, init, xs)
    C_prev = jnp.moveaxis(C_prev, 0, 2)
    n_prev = jnp.moveaxis(n_prev, 0, 2)
    m_prev = jnp.moveaxis(m_prev, 0, 2)

    causal = jnp.tril(jnp.ones((L, L), dtype=bool))
    dmat = jnp.where(causal, bcum[..., :, None] - bcum[..., None, :] + ig[..., None, :], -jnp.inf)
    inter = bcum + m_prev[..., None]
    m_t = jnp.maximum(inter, jnp.max(dmat, axis=-1))
    w_intra = jnp.exp(dmat - m_t[..., None])
    w_inter = jnp.exp(inter - m_t)
    sw = w_intra * jnp.einsum('bhcld,bhcsd->bhcls', qc, kc)
    num = (w_inter[..., None] * jnp.einsum('bhcvk,bhclk->bhclv', C_prev, qc)
           + jnp.einsum('bhcls,bhcsv->bhclv', sw, vc))
    den_raw = w_inter * jnp.einsum('bhck,bhclk->bhcl', n_prev, qc) + jnp.sum(sw, axis=-1)
    den = jnp.maximum(jnp.abs(den_raw), jnp.exp(-m_t))
    hc = num / den[..., None]
    return hc.transpose(0, 2, 3, 1, 4).reshape(B, S, H, D)


def rope_tables(positions):
    half = QK_ROPE // 2
    inv_freq = ROPE_THETA ** (-jnp.arange(half, dtype=jnp.float32) / half)
    ang = positions.astype(jnp.float32)[..., None] * inv_freq
    return jnp.cos(ang)[:, :, None, :], jnp.sin(ang)[:, :, None, :]


def apply_rope(t, cos, sin):
    half = t.shape[-1] // 2
    tf = t.astype(jnp.float32)
    t1, t2 = tf[..., :half], tf[..., half:]
    return jnp.concatenate([t1 * cos - t2 * sin, t1 * sin + t2 * cos], axis=-1).astype(t.dtype)


def mla_attention(q_nope, q_rope, k_nope, k_rope, v):
    B, S, H, _ = q_nope.shape
    nb = S // Q_BLOCK
    scale = (QK_NOPE + QK_ROPE) ** -0.5
    k_idx = jnp.arange(S)

    def to_blocks(t):
        return jnp.moveaxis(t.reshape(B, nb, Q_BLOCK, H, t.shape[-1]), 1, 0)

    def attend_block(args):
        blk, qn, qr = args
        s = (jnp.einsum('bqhd,bkhd->bhqk', qn, k_nope)
             + jnp.einsum('bqhr,bkr->bhqk', qr, k_rope)).astype(jnp.float32) * scale
        q_idx = blk * Q_BLOCK + jnp.arange(Q_BLOCK)
        s = jnp.where(k_idx[None, :] <= q_idx[:, None], s, -jnp.inf)
        p = jax.nn.softmax(s, axis=-1).astype(v.dtype)
        return jnp.einsum('bhqk,bkhd->bqhd', p, v)

    out = lax.map(attend_block, (jnp.arange(nb), to_blocks(q_nope), to_blocks(q_rope)))
    return jnp.moveaxis(out, 0, 1).reshape(B, S, H * V_DIM)


def odd_mixer(h, positions, w_in, gate_bias, ml_norm_g, q_norm_g, kv_norm_g, w_uq, w_ukv, w_out):
    B, S, _ = h.shape
    z = h @ w_in
    q_m, k_m, v_m, o_m, gates, c_q, c_kv, k_r = jnp.split(z, ODD_SPLIT, axis=-1)
    gates = (gates + gate_bias).astype(jnp.float32)
    i_pre, f_pre = gates[..., :ML_HEADS], gates[..., ML_HEADS:]
    hs = (B, S, ML_HEADS, ML_HEAD_DIM)
    hm = mlstm_chunkwise(q_m.reshape(hs).astype(jnp.float32), k_m.reshape(hs).astype(jnp.float32),
                         v_m.reshape(hs).astype(jnp.float32), i_pre, f_pre).astype(h.dtype)
    hm = jax.nn.sigmoid(o_m).reshape(hs) * hm
    hm = rmsnorm(hm, ml_norm_g.reshape(ML_HEADS, ML_HEAD_DIM)).reshape(B, S, ML_WIDTH)

    q = (rmsnorm(c_q, q_norm_g) @ w_uq).reshape(B, S, MLA_HEADS, QK_NOPE + QK_ROPE)
    kv = (rmsnorm(c_kv, kv_norm_g) @ w_ukv).reshape(B, S, MLA_HEADS, QK_NOPE + V_DIM)
    cos, sin = rope_tables(positions)
    q_nope, q_rope = q[..., :QK_NOPE], apply_rope(q[..., QK_NOPE:], cos, sin)
    k_nope, v = kv[..., :QK_NOPE], kv[..., QK_NOPE:]
    k_rope = apply_rope(k_r[:, :, None, :], cos, sin)[:, :, 0]
    ha = mla_attention(q_nope, q_rope, k_nope, k_rope, v)
    return jnp.concatenate([hm, ha], axis=-1) @ w_out


def memory_cross_attention(h, mem_n, wq, wkv, wo):
    B, S, _ = h.shape
    q = (h @ wq).reshape(B, S, XA_HEADS, XA_HEAD_DIM)
    k, v = jnp.split(mem_n @ wkv, 2, axis=-1)
    k = k.reshape(B, N_MEM, XA_HEADS, XA_HEAD_DIM)
    v = v.reshape(B, N_MEM, XA_HEADS, XA_HEAD_DIM)
    s = jnp.einsum('bshd,bmhd->bhsm', q, k).astype(jnp.float32) * (XA_HEAD_DIM ** -0.5)
    p = jax.nn.softmax(s, axis=-1).astype(v.dtype)
    return jnp.einsum('bhsm,bmhd->bshd', p, v).reshape(B, S, XA_WIDTH) @ wo


def swiglu(h, w_gate_up, w_down):
    g, u = jnp.split(h @ w_gate_up, 2, axis=-1)
    return (jax.nn.silu(g) * u) @ w_down


def setup_inputs(seed: int = 0) -> dict:
    key = jax.random.key(seed)
    ks = iter(jax.random.split(key, 40))
    f32 = jnp.float32

    def dense(shape, fan_in):
        return jax.random.normal(next(ks), shape, f32) * (fan_in ** -0.5)

    def gain(shape):
        return 1.0 + 0.05 * jax.random.normal(next(ks), shape, f32)

    x = jax.random.normal(next(ks), (BATCH, SEQ, D_MODEL), f32)
    mem = jax.random.normal(next(ks), (BATCH, N_MEM, D_MODEL), f32)
    offsets = jax.random.randint(next(ks), (BATCH, 1), 0, 1024, dtype=jnp.int32)
    positions = offsets + jnp.arange(SEQ, dtype=jnp.int32)[None, :]

    f_bias = jnp.linspace(3.0, 6.0, ML_HEADS, dtype=f32)[None, :] + 0.1 * jax.random.normal(next(ks), (N_ODD, ML_HEADS), f32)
    i_bias = 0.1 * jax.random.normal(next(ks), (N_ODD, ML_HEADS), f32)

    return {
        'x': x,
        'mem': mem,
        'positions': positions,
        'norm_mix_g': gain((DEPTH, D_MODEL)),
        'norm_xattn_g': gain((DEPTH, D_MODEL)),
        'mem_norm_g': gain((DEPTH, D_MODEL)),
        'xattn_wq': dense((DEPTH, D_MODEL, XA_WIDTH), D_MODEL),
        'xattn_wkv': dense((DEPTH, D_MODEL, 2 * XA_WIDTH), D_MODEL),
        'xattn_wo': dense((DEPTH, XA_WIDTH, D_MODEL), XA_WIDTH),
        'norm_ffn_g': gain((DEPTH, D_MODEL)),
        'ffn_w_gate_up': dense((DEPTH, D_MODEL, 2 * FFN_HIDDEN), D_MODEL),
        'ffn_w_down': dense((DEPTH, FFN_HIDDEN, D_MODEL), FFN_HIDDEN),
        'ev_w_in': dense((N_EVEN, D_MODEL, EVEN_IN), D_MODEL),
        'ev_conv_w': dense((N_EVEN, CONV_K, SC_WIDTH), CONV_K),
        'ev_pool_w': dense((N_EVEN, len(POOL_WINDOWS), POOL_GROUP, POOL_GROUP), POOL_GROUP),
        'ev_pool_scale': 1.0 + 0.1 * jax.random.normal(next(ks), (N_EVEN, POOL_WIDTH), f32),
        'ev_w_out': dense((N_EVEN, EVEN_MIX, D_MODEL), EVEN_MIX),
        'od_w_in': dense((N_ODD, D_MODEL, ODD_IN), D_MODEL),
        'od_gate_bias': jnp.concatenate([i_bias, f_bias], axis=-1),
        'od_ml_norm_g': gain((N_ODD, ML_WIDTH)),
        'od_q_norm_g': gain((N_ODD, Q_LORA)),
        'od_kv_norm_g': gain((N_ODD, KV_LORA)),
        'od_w_uq': dense((N_ODD, Q_LORA, MLA_HEADS * (QK_NOPE + QK_ROPE)), Q_LORA),
        'od_w_ukv': dense((N_ODD, KV_LORA, MLA_HEADS * (QK_NOPE + V_DIM)), KV_LORA),
        'od_w_out': dense((N_ODD, ODD_MIX, D_MODEL), ODD_MIX),
        'final_norm_g': gain((D_MODEL,)),
    }


def reference(x, mem, positions, norm_mix_g, norm_xattn_g, mem_norm_g, xattn_wq, xattn_wkv, xattn_wo,
              norm_ffn_g, ffn_w_gate_up, ffn_w_down, ev_w_in, ev_conv_w, ev_pool_w, ev_pool_scale,
              ev_w_out, od_w_in, od_gate_bias, od_ml_norm_g, od_q_norm_g, od_kv_norm_g, od_w_uq,
              od_w_ukv, od_w_out, final_norm_g):
    for layer in range(DEPTH):
        h = rmsnorm(x, norm_mix_g[layer])
        if layer % 2 == 0:
            e = layer // 2
            mix = even_mixer(h, ev_w_in[e], ev_conv_w[e], ev_pool_w[e], ev_pool_scale[e], ev_w_out[e])
        else:
            o = layer // 2
            mix = odd_mixer(h, positions, od_w_in[o], od_gate_bias[o], od_ml_norm_g[o], od_q_norm_g[o],
                            od_kv_norm_g[o], od_w_uq[o], od_w_ukv[o], od_w_out[o])
        x = x + mix
        mem_n = rmsnorm(mem, mem_norm_g[layer])
        x = x + memory_cross_attention(rmsnorm(x, norm_xattn_g[layer]), mem_n,
                                       xattn_wq[layer], xattn_wkv[layer], xattn_wo[layer])
        x = x + swiglu(rmsnorm(x, norm_ffn_g[layer]), ffn_w_gate_up[layer], ffn_w_down[layer])
    return rmsnorm(x, final_norm_g)
```

```python
import numpy as np
from contextlib import ExitStack
import concourse.bass as bass
import concourse.mybir as mybir
from concourse.bass_utils import run_bass_kernel_spmd

F32, BF16, I32 = mybir.dt.float32, mybir.dt.bfloat16, mybir.dt.int32
AF = mybir.ActivationFunctionType
ALU = mybir.AluOpType


def TT(out, in0, in1, op):
    return lambda e: e.tensor_tensor(out=out, in0=in0, in1=in1, op=op)


def TS(out, in0, s1, op0, s2=None, op1=None):
    if op1 is None:
        return lambda e: e.tensor_scalar(out=out, in0=in0, scalar1=s1, scalar2=None, op0=op0)
    return lambda e: e.tensor_scalar(out=out, in0=in0, scalar1=s1, scalar2=s2, op0=op0, op1=op1)


def STT(out, in0, scalar, in1, op0, op1):
    return lambda e: e.scalar_tensor_tensor(out=out, in0=in0, scalar=scalar, in1=in1, op0=op0, op1=op1)


def ACTF(out, in_, func, scale=None, bias=None):
    kw = {}
    if scale is not None:
        kw["scale"] = scale
    if bias is not None:
        kw["bias"] = bias
    return lambda e: e.activation(out=out, in_=in_, func=func, **kw)


def RECIP(out, in_):
    return lambda e: e.reciprocal(out=out, in_=in_)


def RECIPA(out, in_, scratch):
    return lambda e: e.reciprocal_approx_accurate(out=out, in_=in_, scratch=scratch)


def MEMSET(ap, v):
    return lambda e: e.memset(ap, v)


def COPY(out, in_):
    return lambda e: e.tensor_copy(out=out, in_=in_)


def MM1(out, l, r, start, stop):
    return lambda e: e.matmul(out, l, r, start=start, stop=stop)

T = 2048
HALO = 16
TB = 512
NTB = T // TB
D = 1024
NK = 8
FFN_H = 2816
EPS = 1e-6
TWO_PI = float(2.0 * np.pi)
PI = float(np.pi)

COLS = {}
_ncol = 0


def _defcol(name, n):
    global _ncol
    COLS[name] = _ncol
    _ncol += n


for _l in range(2):
    _defcol(f"norm_mix{_l}", 8)
    _defcol(f"norm_xattn{_l}", 8)
    _defcol(f"mem_norm{_l}", 8)
    _defcol(f"norm_ffn{_l}", 8)
_defcol("final_norm", 8)
_defcol("conv_w", 12)
_defcol("pool_scale", 4)
_defcol("ml_norm", 4)
_defcol("q_norm", 3)
_defcol("kv_norm", 2)
_defcol("inv_freq", 1)
_defcol("rope_sign", 1)
_defcol("flag", 1)
_defcol("gate_bias", 32)
_defcol("poolcorr", 64)
NCOL = _ncol


class KB:
    ENG = ("pe", "act", "dve", "pool", "sp")

    def __init__(self, nc, es):
        self.nc, self.es = nc, es
        self.prog = {e: [] for e in self.ENG}
        self.sem, self.cnt = {}, {}
        self.nroll = 0
        for e in self.ENG[:4]:
            self._newsem(e)
        self.seen = {e: {} for e in self.ENG}
        self.lastw, self.reads = {}, {}
        self.dsems = [[es.enter_context(nc.semaphore(f"dq{i}")), 0] for i in range(40)]
        self.dnext = 0
        self.nbank = 0
        self.held = [False] * 8
        self.banks = [es.enter_context(nc.psum_tensor(f"bank{i}", [128, 512], F32)) for i in range(8)]

    def _newsem(self, e):
        self.sem[e] = self.es.enter_context(self.nc.semaphore(f"c_{e}_{self.nroll}"))
        self.nroll += 1
        self.cnt[e] = 0

    def ps(self, hold=False):
        for _ in range(16):
            i = self.nbank % 8
            self.nbank += 1
            if not self.held[i]:
                break
        else:
            raise RuntimeError("all PSUM banks held")
        if hold:
            self.held[i] = True
        return self.banks[i], ("bank", i)

    def free(self, key):
        self.held[key[1]] = False

    def _wait(self, eng, tok):
        sem, val, src = tok
        if src == eng and eng == "pe":
            return
        d = self.seen[eng]
        if d.get(id(sem), 0) >= val:
            return
        d[id(sem)] = val
        self.prog[eng].append(("w", sem, val))

    def _deps(self, eng, R, W):
        for k in R:
            t = self.lastw.get(k)
            if t:
                self._wait(eng, t)
        for k in W:
            t = self.lastw.get(k)
            if t:
                self._wait(eng, t)
            rd = self.reads.get(k)
            if rd:
                for src, toks in rd.items():
                    if src == eng:
                        continue
                    for t in toks:
                        self._wait(eng, t)

    def _record(self, tok, R, W):
        src = tok[2]
        for k in R:
            rd = self.reads.setdefault(k, {})
            if src == "dma":
                rd.setdefault(src, []).append(tok)
            else:
                rd[src] = [tok]
        for k in W:
            self.lastw[k] = tok
            self.reads[k] = {}

    def op(self, eng, fn, R=(), W=()):
        self._deps(eng, R, W)
        self.cnt[eng] += 1
        sem = self.sem[eng]
        tok = (sem, self.cnt[eng], eng)
        self.prog[eng].append(("o", fn, sem))
        self._record(tok, R, W)
        if self.cnt[eng] >= 30000:
            self._newsem(eng)

    def mm(self, out, pairs, R=(), W=()):
        n = len(pairs)

        def fn(e):
            for i, (l, r) in enumerate(pairs):
                ins = e.matmul(out, l, r, start=(i == 0), stop=(i == n - 1))
            return ins
        self.op("pe", fn, R, W)

    def dma(self, q, out, in_, R=(), W=()):
        self._deps(q, R, W)
        ds = self.dsems[self.dnext]
        self.dnext = (self.dnext + 1) % len(self.dsems)
        if ds[1] > 0:
            self._wait(q, (ds[0], ds[1], "dma"))
        ds[1] += 16
        tok = (ds[0], ds[1], "dma")
        self.prog[q].append(("d", out, in_, ds[0]))
        self._record(tok, R, W)
        return tok

    def cc(self, in_ap, out_ap, R=(), W=()):
        self._deps("pool", R, W)
        sem = self.es.enter_context(self.nc.semaphore(f"ccs{self.nroll}"))
        self.nroll += 1
        tok = (sem, 1, "cc")
        self.prog["pool"].append(("c", in_ap, out_ap, sem))
        self._record(tok, R, W)

    def wait_tok(self, eng, tok):
        self._wait(eng, tok)

    def raw(self, eng, fn):
        self.prog[eng].append(("r", fn))

    def flush(self):
        with self.nc.Block(no_gpsimd_drain=True) as block:
            for e, deco in (("pe", block.tensor), ("act", block.scalar), ("dve", block.vector),
                            ("pool", block.gpsimd), ("sp", block.sync)):
                items = self.prog[e]
                self.prog[e] = []

                def body(eng, items=items):
                    for it in items:
                        if it[0] == "w":
                            eng.wait_ge(it[1], it[2])
                        elif it[0] == "o":
                            it[1](eng).then_inc(it[2], 1)
                        elif it[0] == "d":
                            eng.dma_start(out=it[1], in_=it[2]).then_inc(it[3], 16)
                        elif it[0] == "c":
                            eng.collective_compute("AllGather", ALU.bypass,
                                                   replica_groups=[[0, 1], [2, 3], [4, 5], [6, 7]],
                                                   ins=[it[1]], outs=[it[2]]).then_inc(it[3])
                        else:
                            it[1](eng)
                deco(body)


def build(stop_after=99):
    nc = bass.Bass("TRN2", target_bir_lowering=False)

    def din(name, shape, dt=F32):
        return nc.dram_tensor(name, list(shape), dt, kind="ExternalInput").ap()

    xT_d = din("xT", [D, T + HALO])
    memT_d = din("memT", [D, 256])
    pos_d = din("posb", [128, T], I32)
    cols_d = din("cols", [128, NCOL])
    tri_d = din("tri", [128, 128])
    shE_d = din("shiftE", [128, 128])
    shO_d = din("shiftO", [128, 128])
    W = {}
    W["xattn_wq"] = din("xattn_wq", [2, D, 512])
    W["xattn_wkv"] = din("xattn_wkv", [2, D, 1024])
    W["xattn_wo"] = din("xattn_wo", [2, 512, D])
    W["ffn_gu"] = din("ffn_w_gate_up", [2, D, 2 * FFN_H])
    W["ffn_down"] = din("ffn_w_down", [2, FFN_H, D])
    W["ev_w_in"] = din("ev_w_in", [1, D, 2048])
    W["ev_pool_w"] = din("ev_pool_w", [1, 4, 128, 128])
    W["ev_w_out"] = din("ev_w_out", [1, D, D])
    W["od_w_in"] = din("od_w_in", [1, D, 2728])
    W["od_w_uq"] = din("od_w_uq", [1, 384, 768])
    W["od_w_ukv"] = din("od_w_ukv", [1, 256, 1024])
    W["od_w_out"] = din("od_w_out", [1, D, D])
    out_d = nc.dram_tensor("outT", [D, T], F32, kind="ExternalOutput").ap()
    cin_st = nc.dram_tensor("cin_st", [128, 1024], F32, kind="Internal").ap()
    cout_st = nc.dram_tensor("cout_st", [256, 1024], F32, kind="Internal").ap()
    cin_kv = nc.dram_tensor("cin_kv", [128, 2 * T], BF16, kind="Internal").ap()
    cout_kv = nc.dram_tensor("cout_kv", [256, 2 * T], BF16, kind="Internal").ap()
    cin_kr = nc.dram_tensor("cin_kr", [32, T], BF16, kind="Internal").ap()
    cout_kr = nc.dram_tensor("cout_kr", [64, T], BF16, kind="Internal").ap()

    with ExitStack() as es:
        kb = KB(nc, es)

        uid = [0]

        def sb(name, shape, dt, stack=es):
            uid[0] += 1
            return stack.enter_context(nc.sbuf_tensor(f"{name}_{uid[0]}", list(shape), dt))

        xT = sb("xT_s", [128, NK, T + HALO], F32)
        hT_holder = [None]
        cols = sb("cols_s", [128, NCOL], F32)
        ones_bf = sb("ones_bf", [128, 128], BF16)
        ones_f = sb("ones_f", [128, 128], F32)
        tri_f = sb("tri_f", [128, 128], F32)
        tri_bf = sb("tri_bf", [128, 128], BF16)
        shE = sb("shE", [128, 128], F32)
        shO = sb("shO", [128, 128], F32)
        wA = [sb(f"wA{i}", [128, NK * 512], BF16) for i in range(2)]
        wB = [sb(f"wB{i}", [128, 2, D], BF16) for i in range(2)]
        wAn = [0]
        wBn = [0]

        def col(name, i=0):
            c = COLS[name] + i
            return cols[:, c:c + 1]

        def nextA():
            i = wAn[0] % 2
            wAn[0] += 1
            return wA[i], ("wA", i)

        def nextB():
            i = wBn[0] % 2
            wBn[0] += 1
            return wB[i], ("wB", i)

        PRE = {}

        def loadA(src2d, segs, name=None):
            if name is not None and name in PRE:
                return PRE.pop(name)
            t, k = nextA()
            t = t[:, :].rearrange("p (kc n) -> p kc n", n=512)
            o = 0
            v = src2d.rearrange("(kc p) n -> p kc n", p=128)
            for c0, n in segs:
                kb.dma("pool", t[:, :, o:o + n], v[:, :, c0:c0 + n], W=[k])
                o += n
            return t, k

        def loadB(src2d, rows, name=None):
            if name is not None and name in PRE:
                return PRE.pop(name)
            t, k = nextB()
            for i, r0 in enumerate(rows):
                kb.dma("pool", t[:, i, :], src2d[r0:r0 + 128, :], W=[k])
            return t, k

        for k in range(NK):
            kb.dma("sp", xT[:, k, 0:HALO + TB], xT_d[k * 128:(k + 1) * 128, 0:HALO + TB], W=[("x", k, 0), ("x", k, "halo")])
        for tb in range(1, NTB):
            for k in range(NK):
                cs_ = slice(HALO + tb * TB, HALO + (tb + 1) * TB)
                kb.dma("sp", xT[:, k, cs_], xT_d[k * 128:(k + 1) * 128, cs_], W=[("x", k, tb)])
        kb.dma("sp", cols[:], cols_d[:, :], W=["cols"])
        kb.dma("sp", tri_f[:], tri_d[:, :], W=["tri_f"])
        kb.dma("sp", shE[:], shE_d[:, :], W=["shE"])
        kb.dma("sp", shO[:], shO_d[:, :], W=["shO"])
        kb.dma("pool", tri_bf[:], tri_d[:, :], W=["tri_bf"])
        kb.op("dve", MEMSET(ones_bf[:], 1.0), W=["ones_bf"])
        kb.op("dve", MEMSET(ones_f[:], 1.0), W=["ones_f"])

        def mk_norm_tmp(ss):
            st = {"sq": [sb(f"sq{i}", [128, TB], BF16, ss) for i in range(2)], "n": 0,
                  "rs": sb("rs", [128, TB], F32, ss), "eps": sb("epsc", [128, 1], F32, ss)}
            kb.op("dve", MEMSET(st["eps"][:], EPS), W=["epsc"])
            return st

        def rms_block(srcs, dsts, gname, N, Dn, st):
            nk = len(srcs)
            ps, pk = kb.ps()
            for i, (s, sk) in enumerate(srcs):
                sq, sqk = st["sq"][st["n"] % 2], ("sq", st["n"] % 2)
                st["n"] += 1
                kb.op("act", ACTF(sq[:, :N], s, AF.Square), R=[sk], W=[sqk])
                kb.op("pe", MM1(ps[:, :N], ones_bf[:], sq[:, :N], i == 0, i == nk - 1),
                      R=[sqk, "ones_bf"], W=[pk])
            rs = st["rs"]
            kb.op("act", ACTF(rs[:, :N], ps[:, :N], AF.Ln, scale=1.0 / Dn, bias=st["eps"][:, 0:1]),
                  R=[pk, "epsc"], W=["rs"])
            kb.op("act", ACTF(ps[:, :N], rs[:, :N], AF.Exp, scale=-0.5), R=["rs"], W=[pk])
            for i, ((s, sk), (d, dk)) in enumerate(zip(srcs, dsts)):
                kb.op("dve", STT(d, s, col(gname, i), ps[:, :N], ALU.mult, ALU.mult),
                      R=[sk, pk, "cols"], W=[dk])

        st_holder = [None]

        def hkeys(tb):
            return [("h", k, tb) for k in range(NK)]

        def norm_block(gname, tb):
            hT = hT_holder[0]
            c0, n = (0, HALO) if tb == "halo" else (HALO + tb * TB, TB)
            srcs = [(xT[:, k, c0:c0 + n], ("x", k, tb)) for k in range(NK)]
            dsts = [(hT[:, k, c0:c0 + n], ("h", k, tb)) for k in range(NK)]
            rms_block(srcs, dsts, gname, n, D, st_holder[0])

        def accum_x(tb, o, ps, pk):
            xs = xT[:, o, HALO + tb * TB: HALO + (tb + 1) * TB]
            kb.op("dve", TT(xs, xs, ps[:, :], ALU.add), R=[pk, ("x", o, tb)], W=[("x", o, tb)])

        def contract(wt, wk, chunks, tb):
            for o in range(NK):
                ps, pk = kb.ps()
                kb.mm(ps[:, :], [(wt[:, c, o * 128:(o + 1) * 128], a) for c, (a, _) in enumerate(chunks)],
                      R=[wk] + [k for _, k in chunks], W=[pk])
                accum_x(tb, o, ps, pk)

        def run_interleaved(work, make_gen, nslots=2, pre=None):
            active = []
            free_slots = list(range(nslots))
            wi = 0
            while wi < len(work) or active:
                while wi < len(work) and free_slots:
                    if pre is not None:
                        pre(work[wi])
                    sidx = free_slots.pop(0)
                    active.append((make_gen(work[wi], sidx), sidx))
                    wi += 1
                for g in list(active):
                    try:
                        next(g[0])
                    except StopIteration:
                        active.remove(g)
                        free_slots.append(g[1])

        HK = [("h", k) for k in range(NK)]

        def even_mixer(post=None):
            hT = hT_holder[0]
            w_in = W["ev_w_in"][0]
            w_out = W["ev_w_out"][0]
            with ExitStack() as ss:
                poolw = sb("poolw", [128, 4, 128], BF16, ss)
                kb.dma("pool", poolw[:], W["ev_pool_w"][0].rearrange("g c d -> c g d"), W=["poolw"])
                NW = TB + HALO
                SL = []
                for i in range(4):
                    SL.append(dict(z=sb(f"z{i}", [128, 4, NW], F32, ss), u=sb(f"u{i}", [128, NW], F32, ss),
                                   acc=sb(f"cacc{i}", [128, TB], F32, ss), s1=sb(f"s1{i}", [128, NW], F32, ss),
                                   s2=sb(f"s2{i}", [128, NW], F32, ss), pm=sb(f"pm{i}", [128, TB], BF16, ss),
                                   mix=sb(f"mix{i}", [128, 2, TB], BF16, ss)))
                pieces = {}

                def gen(item, si):
                    j, tb = item
                    S = SL[si]
                    zt, u, acc, s1, s2, pmt, mx = S["z"], S["u"], S["acc"], S["s1"], S["s2"], S["pm"], S["mix"]
                    K = lambda *n: n + (si,)
                    if j not in pieces:
                        pieces[j] = (loadA(w_in, [(j * 128, 128), (512 + j * 128, 128), (1024 + j * 128, 128), (1536 + j * 128, 128)]),
                                     loadB(w_out, [j * 128, 512 + j * 128]))
                    (wt, wk), (bt, bk) = pieces[j]
                    win = (2, 4, 8, 16)[j]
                    c0 = tb * TB
                    HKA = hkeys(tb)
                    HKB = hkeys(tb - 1 if tb > 0 else "halo")
                    for i in range(4):
                        psA, pkA = kb.ps()
                        kb.mm(psA[:, :], [(wt[:, k, i * 128:(i + 1) * 128], hT[:, k, c0 + HALO:c0 + HALO + TB])
                                          for k in range(NK)], R=[wk] + HKA, W=[pkA])
                        psB, pkB = kb.ps()
                        kb.mm(psB[:, :HALO], [(wt[:, k, i * 128:(i + 1) * 128], hT[:, k, c0:c0 + HALO])
                                              for k in range(NK)], R=[wk] + HKB, W=[pkB])
                        kb.op("act", ACTF(zt[:, i, HALO:], psA[:, :], AF.Copy), R=[pkA], W=[K("z", i)])
                        kb.op("act", ACTF(zt[:, i, :HALO], psB[:, :HALO], AF.Copy), R=[pkB], W=[K("z", i, "h")])
                        if i % 2 == 1:
                            yield

                    def zkeys(i):
                        return [K("z", i), K("z", i, "h")]
                    kb.op("dve", TT(u[:, :], zt[:, 1, :], zt[:, 2, :], ALU.mult), R=zkeys(1) + zkeys(2), W=[K("u")])
                    kb.op("dve", TS(acc[:, :], u[:, HALO - 2:HALO - 2 + TB], col("conv_w", j * 3 + 0), ALU.mult),
                          R=[K("u"), "cols"], W=[K("cacc")])
                    for kk in (1, 2):
                        kb.op("dve", STT(acc[:, :], u[:, HALO - 2 + kk:HALO - 2 + kk + TB],
                                         col("conv_w", j * 3 + kk), acc[:, :], ALU.mult, ALU.add),
                              R=[K("u"), K("cacc"), "cols"], W=[K("cacc")])
                    kb.op("dve", TT(mx[:, 0, :], zt[:, 0, HALO:], acc[:, :], ALU.mult),
                          R=zkeys(0) + [K("cacc")], W=[K("mix", 0)])
                    yield
                    src, srck = zt[:, 3, :], zkeys(3)
                    bufs = [(s1, K("s1")), (s2, K("s2"))]
                    bi = 0
                    step = 1
                    while step < win:
                        dst, dk = bufs[bi]
                        bi ^= 1
                        kb.op("pool", COPY(dst[:, 0:step], src[:, 0:step]), R=srck, W=[dk])
                        kb.op("pool", TT(dst[:, step:NW], src[:, step:NW], src[:, 0:NW - step], ALU.add),
                              R=srck, W=[dk])
                        src, srck = dst[:, :], [dk]
                        step *= 2
                    yield
                    if tb == 0:
                        pc = COLS["poolcorr"] + j * 16
                        kb.op("dve", TT(src[:, HALO:2 * HALO], src[:, HALO:2 * HALO], cols[:, pc:pc + 16], ALU.mult),
                              R=srck + ["cols"], W=srck[:1])
                    kb.op("dve", STT(pmt[:, :], src[:, HALO:], 1.0 / win, zt[:, 3, HALO:], ALU.mult, ALU.subtract),
                          R=srck + zkeys(3), W=[K("pm")])
                    psP, pkP = kb.ps()
                    kb.mm(psP[:, :], [(poolw[:, j, :], pmt[:, :])], R=["poolw", K("pm")], W=[pkP])
                    kb.op("act", ACTF(mx[:, 1, :], psP[:, :], AF.Copy, scale=col("pool_scale", j)),
                          R=[pkP, "cols"], W=[K("mix", 1)])
                    yield
                    for o in range(NK):
                        ps, pk = kb.ps()
                        kb.mm(ps[:, :], [(bt[:, c, o * 128:(o + 1) * 128], mx[:, c, :]) for c in range(2)],
                              R=[bk, K("mix", 0), K("mix", 1)], W=[pk])
                        accum_x(tb, o, ps, pk)
                        if o == 3:
                            yield
                def pre(item):
                    j, tb = item
                    if j == 0:
                        if tb == 0:
                            norm_block("norm_mix0", "halo")
                        norm_block("norm_mix0", tb)
                run_interleaved([(j, tb) for j in range(4) for tb in range(NTB)], gen, nslots=4, pre=pre)
                if post:
                    post()
                kb.flush()

        def xattn(l, post=None):
            hT = hT_holder[0]
            wq = W["xattn_wq"][l]
            wkv = W["xattn_wkv"][l]
            wo = W["xattn_wo"][l]
            with ExitStack() as ss:
                memf = sb("memf", [128, NK, 256], F32, ss)
                memn = sb("memn", [128, NK, 256], BF16, ss)
                kT = sb("kT", [128, 4, 256], BF16, ss)
                vt = sb("vt", [128, 2, 512], BF16, ss)
                st = st_holder[0]
                for k in range(NK):
                    kb.dma("sp", memf[:, k, :], memT_d[k * 128:(k + 1) * 128, :], W=[("memf", k)])
                rms_block([(memf[:, k, :], ("memf", k)) for k in range(NK)],
                          [(memn[:, k, :], ("memn", k)) for k in range(NK)], f"mem_norm{l}", 256, D, st)
                mk_all = [("memn", k) for k in range(NK)]
                wt, wk = loadA(wkv, [(0, 512)], name=f"xk{l}")
                for h in range(4):
                    ps, pk = kb.ps()
                    kb.mm(ps[:, :256], [(wt[:, k, h * 128:(h + 1) * 128], memn[:, k, :]) for k in range(NK)],
                          R=[wk] + mk_all, W=[pk])
                    kb.op("act", ACTF(kT[:, h, :], ps[:, :256], AF.Copy), R=[pk], W=[("kT", h)])
                wt, wk = loadA(wkv, [(512, 512)], name=f"xv{l}")
                for mc in range(2):
                    ps, pk = kb.ps()
                    kb.mm(ps[:, :], [(memn[:, k, mc * 128:(mc + 1) * 128], wt[:, k, :]) for k in range(NK)],
                          R=[wk] + mk_all, W=[pk])
                    kb.op("act", ACTF(vt[:, mc, :], ps[:, :], AF.Copy), R=[pk], W=[("vt", mc)])
                wqt, wqk = loadA(wq, [(0, 512)])
                qT = [sb(f"qT{i}", [128, TB], BF16, ss) for i in range(4)]
                pT = [sb(f"pT{i}", [128, 2, TB], BF16, ss) for i in range(4)]
                oh = [sb(f"oh{i}", [128, 2, TB], BF16, ss) for i in range(2)]
                rdens = [sb(f"rden{i}", [128, TB], F32, ss) for i in range(4)]
                scale = 128.0 ** -0.5
                pieces = {}

                def gen(item, si):
                    tb, hp, hh = item
                    h = hp * 2 + hh
                    if hp not in pieces:
                        pieces[hp] = loadB(wo, [hp * 256, hp * 256 + 128])
                    bt, bk = pieces[hp]
                    c0 = HALO + tb * TB
                    oi = hp % 2
                    ot, ok = oh[oi], ("oh", oi)
                    q, qk = qT[si], ("qT", si)
                    p, pkk = pT[si], ("pT", si)
                    rden, rk = rdens[si], ("rden", si)
                    ps, pk = kb.ps()
                    kb.mm(ps[:, :], [(wqt[:, k, h * 128:(h + 1) * 128], hT[:, k, c0:c0 + TB]) for k in range(NK)],
                          R=[wqk] + hkeys(tb), W=[pk])
                    kb.op("act", ACTF(q[:, :], ps[:, :], AF.Copy), R=[pk], W=[qk])
                    yield
                    for mc in range(2):
                        ps2, pk2 = kb.ps()
                        kb.mm(ps2[:, :], [(kT[:, h, mc * 128:(mc + 1) * 128], q[:, :])], R=[("kT", h), qk], W=[pk2])
                        kb.op("act", ACTF(p[:, mc, :], ps2[:, :], AF.Exp, scale=scale), R=[pk2], W=[pkk + (mc,)])
                    yield
                    pso, pko = kb.ps()
                    kb.mm(pso[:, :], [(vt[:, mc, h * 128:(h + 1) * 128], p[:, mc, :]) for mc in range(2)],
                          R=[("vt", 0), ("vt", 1), pkk + (0,), pkk + (1,)], W=[pko])
                    psd, pkd = kb.ps()
                    kb.mm(psd[:, :], [(ones_bf[:, :], p[:, mc, :]) for mc in range(2)],
                          R=["ones_bf", pkk + (0,), pkk + (1,)], W=[pkd])
                    kb.op("act", ACTF(rden[:, :], psd[:, :], AF.Ln), R=[pkd], W=[rk])
                    kb.op("act", ACTF(rden[:, :], rden[:, :], AF.Exp, scale=-1.0), R=[rk], W=[rk])
                    kb.op("dve", TT(ot[:, hh, :], pso[:, :], rden[:, :], ALU.mult), R=[pko, rk], W=[ok + (hh,)])
                    if hh == 1:
                        yield
                        for o in range(NK):
                            ps, pk = kb.ps()
                            kb.mm(ps[:, :], [(bt[:, c, o * 128:(o + 1) * 128], ot[:, c, :]) for c in range(2)],
                                  R=[bk, ok + (0,), ok + (1,)], W=[pk])
                            accum_x(tb, o, ps, pk)
                            if o == 3:
                                yield
                normed = set()

                def pre(item):
                    if item[0] not in normed:
                        normed.add(item[0])
                        norm_block(f"norm_xattn{l}", item[0])
                run_interleaved([(tb, hp, hh) for tb in range(NTB) for hp in range(2) for hh in range(2)], gen, nslots=4, pre=pre)
                if post:
                    post()
                kb.flush()

        def ffn(l, post=None):
            hT = hT_holder[0]
            gname = f"norm_ffn{l}"
            gu = W["ffn_gu"][l]
            dn = W["ffn_down"][l]
            with ExitStack() as ss:
                sg = [sb(f"sg{i}", [128, TB], F32, ss) for i in range(2)]
                act = [sb(f"act{i}", [128, 2, TB], BF16, ss) for i in range(2)]
                it = 0
                ia = 0
                for g in range(FFN_H // 256):
                    wt, wk = loadA(gu, [(g * 256, 256), (FFN_H + g * 256, 256)], name=f"ffnA{l}_{g}")
                    bt, bk = loadB(dn, [g * 256, g * 256 + 128], name=f"ffnB{l}_{g}")
                    for tb in range(NTB):
                        c0 = HALO + tb * TB
                        if g == 0:
                            norm_block(gname, tb)
                        HK = hkeys(tb)
                        at, ak = act[ia % 2], ("act", ia % 2)
                        ia += 1
                        for c in range(2):
                            psg, pkg = kb.ps()
                            kb.mm(psg[:, :], [(wt[:, k, c * 128:(c + 1) * 128], hT[:, k, c0:c0 + TB]) for k in range(NK)],
                                  R=[wk] + HK, W=[pkg])
                            psu, pku = kb.ps()
                            kb.mm(psu[:, :], [(wt[:, k, 256 + c * 128:256 + (c + 1) * 128], hT[:, k, c0:c0 + TB])
                                              for k in range(NK)], R=[wk] + HK, W=[pku])
                            s, sk = sg[it % 2], ("sg", it % 2)
                            it += 1
                            kb.op("act", ACTF(s[:, :], psg[:, :], AF.Silu), R=[pkg], W=[sk])
                            kb.op("dve", TT(at[:, c, :], psu[:, :], s[:, :], ALU.mult), R=[pku, sk], W=[ak + (c,)])
                        contract(bt, bk, [(at[:, 0, :], ak + (0,)), (at[:, 1, :], ak + (1,))], tb)
                if post:
                    post()
                kb.flush()

        def final_out(gname):
            with ExitStack() as ss:
                st = mk_norm_tmp(ss)
                ob = [sb(f"ob{i}", [128, NK, TB], F32, ss) for i in range(2)]
                toks = []
                for tb in range(NTB):
                    c0 = HALO + tb * TB
                    o, okk = ob[tb % 2], ("ob", tb % 2)
                    if gname is None:
                        for k in range(NK):
                            kb.op("act", ACTF(o[:, k, :], xT[:, k, c0:c0 + TB], AF.Copy), R=[("x", k, tb)], W=[okk + (k,)])
                    else:
                        rms_block([(xT[:, k, c0:c0 + TB], ("x", k, tb)) for k in range(NK)],
                                  [(o[:, k, :], okk + (k,)) for k in range(NK)], gname, TB, D, st)
                    for k in range(NK):
                        toks.append(kb.dma("sp", out_d[k * 128:(k + 1) * 128, tb * TB:(tb + 1) * TB], o[:, k, :],
                                           R=[okk + (k,)]))
                for t in toks:
                    kb.wait_tok("sp", t)
                kb.flush()

        def odd_mixer(sub=9, post=None):
            w_in = W["od_w_in"][0]
            w_out = W["od_w_out"][0]
            OQ, OKk, OV, OO, OG, OCQ, OCKV, OKR = 0, 512, 1024, 1536, 2048, 2056, 2440, 2696
            gb = COLS["gate_bias"]
            P = slice(64, 96)
            SC = 128.0 ** -0.5
            XK = [("x", k) for k in range(NK)]
            def HBt(tb):
                return [("hb", k, tb) for k in range(NK)]
            with ExitStack() as s1:
                s2 = ExitStack()
                hT1 = sb("hT1", [128, NK, T], BF16, s1)
                state = sb("state", [128, 4, 256], F32, s1)
                gI, gNB, gNLF, gEG, gW = [sb(n, [128, 16, 4], F32, s1) for n in ("gI", "gNB", "gNLF", "gEG", "gW")]
                wG = sb("wG", [128, NK, 8], BF16, s1)
                epsc = sb("epsc2", [128, 1], F32, s1)
                cqn = sb("cqn", [128, 3, T], BF16, s2)
                ckvn = sb("ckvn", [128, 2, 2 * T], BF16, s2)
                KT = sb("KT", [128, 2 * T], BF16, s2)
                ctab = sb("ctab", [128, T], BF16, s2)
                stab = sb("stab", [128, T], BF16, s2)
                kb.op("dve", MEMSET(epsc[:], EPS), W=["epsc2"])
                kb.dma("pool", wG[:], w_in.rearrange("(kc p) n -> p kc n", p=128)[:, :, OG:OG + 8], W=["wG"])
                for h in range(4):
                    kb.op("dve", MEMSET(state[:, h, :], 0.0), W=[("state", h)])

                with ExitStack() as ss:
                    posi = sb("posi", [128, TB], I32, ss)
                    ang = sb("ang", [128, TB], F32, ss)
                    rr = sb("rr", [128, TB], F32, ss)
                    tmp = sb("rtmp", [128, TB], F32, ss)
                    ki = sb("ki", [128, TB], I32, ss)
                    ifq = cols[64:96, COLS["inv_freq"]:COLS["inv_freq"] + 1]
                    sgn = cols[64:96, COLS["rope_sign"]:COLS["rope_sign"] + 1]
                    for tb in range(NTB):
                        cs = slice(tb * TB, (tb + 1) * TB)
                        kb.dma("sp", posi[P, :], pos_d[64:96, cs], W=["posi"])
                        kb.op("dve", COPY(ang[P, :], posi[P, :]), R=["posi"], W=["ang"])
                        kb.op("dve", TS(ang[P, :], ang[P, :], ifq, ALU.mult), R=["ang", "cols"], W=["ang"])
                        for dst, dk, shift, signed in ((stab, "stab", 0.0, True), (ctab, "ctab", PI / 2, False)):
                            kb.op("dve", TS(tmp[P, :], ang[P, :], shift, ALU.add, 1.0 / TWO_PI, ALU.mult), R=["ang"], W=["rtmp"])
                            kb.op("dve", COPY(ki[P, :], tmp[P, :]), R=["rtmp"], W=["ki"])
                            kb.op("dve", COPY(tmp[P, :], ki[P, :]), R=["ki"], W=["rtmp"])
                            kb.op("dve", STT(rr[P, :], tmp[P, :], -TWO_PI, ang[P, :], ALU.mult, ALU.add),
                                  R=["rtmp", "ang"], W=["rr"])
                            if shift:
                                kb.op("dve", TS(rr[P, :], rr[P, :], shift, ALU.add), R=["rr"], W=["rr"])
                            kb.op("dve", TS(tmp[P, :], rr[P, :], PI, ALU.is_gt, -TWO_PI, ALU.mult), R=["rr"], W=["rtmp"])
                            kb.op("dve", TT(rr[P, :], rr[P, :], tmp[P, :], ALU.add), R=["rr", "rtmp"], W=["rr"])
                            kb.op("dve", TS(tmp[P, :], rr[P, :], -PI, ALU.is_lt, TWO_PI, ALU.mult), R=["rr"], W=["rtmp"])
                            kb.op("dve", TT(rr[P, :], rr[P, :], tmp[P, :], ALU.add), R=["rr", "rtmp"], W=["rr"])
                            kb.op("dve", TS(rr[P, :], rr[P, :], PI, ALU.min, -PI, ALU.max), R=["rr"], W=["rr"])
                            if signed:
                                kb.op("act", ACTF(tmp[P, :], rr[P, :], AF.Sin), R=["rr"], W=["rtmp"])
                                kb.op("dve", TS(dst[P, cs], tmp[P, :], sgn, ALU.mult), R=["rtmp", "cols"], W=[(dk, tb)])
                            else:
                                kb.op("act", ACTF(dst[P, cs], rr[P, :], AF.Sin), R=["rr"], W=[(dk, tb)])
                    kb.flush()

                def gates(tb, psG, pkG, gf, ge, gt):
                    cs = slice(tb * 4, tb * 4 + 4)
                    gk = ("g", tb)
                    pg = psG[:, 0:32].rearrange("p (c g) -> p c g", g=8)
                    gb3 = cols[:, gb:gb + 32].rearrange("p (c g) -> p c g", g=8)
                    gbi = gb3[:, :, 0:4]
                    gbf = gb3[:, :, 4:8]
                    kb.op("dve", TT(gI[:, cs, :], pg[:, :, 0:4], gbi, ALU.add), R=[pkG, "cols"], W=[gk + ("I",)])
                    kb.op("dve", TT(gf[:, :, :], pg[:, :, 4:8], gbf, ALU.add), R=[pkG, "cols"], W=["gf"])
                    kb.op("act", ACTF(ge[:, :, :], gf[:, :, :], AF.Exp, scale=-1.0), R=["gf"], W=["ge"])
                    kb.op("act", ACTF(gNLF[:, cs, :], ge[:, :, :], AF.Ln, bias=ones_f[:, 0:1]), R=["ge", "ones_f"], W=[gk + ("NLF",)])
                    nlf16 = gNLF[:, cs, :].rearrange("p c g -> p (c g)")
                    psNB, pkNB = kb.ps()
                    kb.mm(psNB[:, :16], [(tri_f[:, :], nlf16)], R=["tri_f", gk + ("NLF",)], W=[pkNB])
                    kb.op("act", ACTF(gNB[:, cs, :].rearrange("p c g -> p (c g)"), psNB[:, :16], AF.Copy), R=[pkNB], W=[gk + ("NB",)])
                    psNG, pkNG = kb.ps()
                    kb.mm(psNG[:, :16], [(ones_f[:, :], nlf16)], R=["ones_f", gk + ("NLF",)], W=[pkNG])
                    kb.op("act", ACTF(gEG[:, cs, :].rearrange("p c g -> p (c g)"), psNG[:, :16], AF.Exp, scale=-1.0), R=[pkNG], W=[gk + ("EG",)])
                    g16 = gt[:, :, :].rearrange("p c g -> p (c g)")
                    kb.op("dve", TT(gt[:, :, :], gI[:, cs, :], gNB[:, cs, :], ALU.add), R=[gk + ("I",), gk + ("NB",)], W=["gt"])
                    kb.op("dve", TT(g16, g16, psNG[:, :16], ALU.subtract), R=["gt", pkNG], W=["gt"])
                    kb.op("act", ACTF(gW[:, cs, :], gt[:, :, :], AF.Exp), R=["gt"], W=[gk + ("W",)])

                A0, A1 = wA[0], wA[1]
                wuq = A0[:, 0:2304].rearrange("p (i n) -> p i n", n=768)
                wuqs = A0[:, 2304:3072].rearrange("p (i h c) -> p i h c", i=3, c=32)
                wukv = A1[:, 0:2048].rearrange("p (i n) -> p i n", n=1024)
                uq_d = W["od_w_uq"][0]

                def mla_weights():
                    kb.dma("pool", wuq, uq_d.rearrange("(kc p) n -> p kc n", p=128), W=[("wA", 0)])
                    for i in range(3):
                        uq3 = uq_d[i * 128:(i + 1) * 128, :].rearrange("p (h c) -> p h c", c=96)
                        kb.dma("pool", wuqs[:, i, :, 0:16], uq3[:, :, 80:96], W=[("wA", 0)])
                        kb.dma("pool", wuqs[:, i, :, 16:32], uq3[:, :, 64:80], W=[("wA", 0)])
                    kb.dma("pool", wukv, W["od_w_ukv"][0].rearrange("(kc p) n -> p kc n", p=128), W=[("wA", 1)])

                with ExitStack() as ss:
                    st = mk_norm_tmp(ss)
                    def norm1(tb):
                        c0 = HALO + tb * TB
                        rms_block([(xT[:, k, c0:c0 + TB], ("x", k, tb)) for k in range(NK)],
                                  [(hT1[:, k, tb * TB:(tb + 1) * TB], ("hb", k, tb)) for k in range(NK)], "norm_mix1", TB, D, st)
                    norm1(0)
                    lat = sb("lat", [128, 3, TB], F32, ss)
                    lat2_f = sb("lat2", [128, 4, 256], F32, ss)
                    lat2 = lat2_f[:, :, :].rearrange("p a b -> p (a b)").rearrange("p (i t) -> p i t", t=TB)
                    kra = sb("kra", [128, TB], F32, ss)
                    krb = sb("krb", [128, TB], F32, ss)
                    vaug = [sb(f"vaug{i}", [128, 4, 256], BF16, ss) for i in range(2)]
                    kw = [sb(f"kw{i}", [128, 4, 128], BF16, ss) for i in range(2)]
                    gf = sb("gf", [128, 4, 4], F32, ss)
                    ge = sb("ge", [128, 4, 4], F32, ss)
                    gt = sb("gt", [128, 4, 4], F32, ss)
                    for i in range(2):
                        kb.op("dve", MEMSET(vaug[i][:, :, 128:256], 1.0), W=[("vaug1", i)])
                    t1, k1 = loadA(w_in, [(OCQ, 384), (OKR, 32), (OKR + 16, 16), (OKR, 16)], name="oddL1")
                    t2, k2 = loadA(w_in, [(OCKV, 256)], name="oddL2")
                    for tb in range(NTB):
                        own = slice(T + tb * TB, T + (tb + 1) * TB)
                        hb = hT1[:, :, tb * TB:(tb + 1) * TB]
                        HB = HBt(tb)
                        if tb + 1 < NTB:
                            norm1(tb + 1)
                        for i in range(3):
                            ps, pk = kb.ps()
                            kb.mm(ps[:, :], [(t1[:, k, i * 128:(i + 1) * 128], hb[:, k, :]) for k in range(NK)], R=[k1] + HB, W=[pk])
                            kb.op("act", ACTF(lat[:, i, :], ps[:, :], AF.Copy), R=[pk], W=[("lat", i)])
                        rms_block([(lat[:, i, :], ("lat", i)) for i in range(3)],
                                  [(cqn[:, i, tb * TB:(tb + 1) * TB], ("cqn", tb)) for i in range(3)], "q_norm", TB, 384, st)
                        pst, pkt = kb.ps()
                        kb.mm(pst[P, :], [(t1[:, k, 384:416], hb[:, k, :]) for k in range(NK)], R=[k1] + HB, W=[pkt])
                        pss, pks = kb.ps()
                        kb.mm(pss[P, :], [(t1[:, k, 416:448], hb[:, k, :]) for k in range(NK)], R=[k1] + HB, W=[pks])
                        kb.op("dve", TT(kra[P, :], pst[P, :], ctab[P, tb * TB:(tb + 1) * TB], ALU.mult), R=[pkt, ("ctab", tb)], W=["kra"])
                        kb.op("dve", TT(krb[P, :], pss[P, :], stab[P, tb * TB:(tb + 1) * TB], ALU.mult), R=[pks, ("stab", tb)], W=["krb"])
                        kb.op("dve", TT(KT[P, own], kra[P, :], krb[P, :], ALU.add), R=["kra", "krb"], W=[("KTr", 4 + tb)])
                        for i in range(2):
                            ps, pk = kb.ps()
                            kb.mm(ps[:, :], [(t2[:, k, i * 128:(i + 1) * 128], hb[:, k, :]) for k in range(NK)], R=[k2] + HB, W=[pk])
                            kb.op("dve", COPY(lat2[:, i, :], ps[:, :]), R=[pk], W=[("lat2", i)])
                        rms_block([(lat2[:, i, :], ("lat2", i)) for i in range(2)],
                                  [(ckvn[:, i, own], ("ckvn", 4 + tb)) for i in range(2)], "kv_norm", TB, 256, st)
                    tK, kK = loadA(w_in, [(OKk, 512)])
                    tV, kV = loadA(w_in, [(OV, 512)])
                    kb.dma("sp", cin_kv.rearrange("p (i t) -> p i t", i=2), ckvn[:, :, T:2 * T], R=[("ckvn", 4 + tb) for tb in range(4)], W=["cin_kv"])
                    kb.dma("sp", cin_kr[:, :], KT[P, T:2 * T], R=[("KTr", 4 + tb) for tb in range(4)], W=["cin_kr"])
                    kb.cc(cin_kv[:, :], cout_kv[:, :], R=["cin_kv"], W=["cout_kv"])
                    kb.cc(cin_kr[:, :], cout_kr[:, :], R=["cin_kr"], W=["cout_kr"])
                    kb.dma("sp", ckvn[:, :, 0:T], cout_kv[0:128, :].rearrange("p (i t) -> p i t", i=2), R=["cout_kv"],
                           W=[("ckvn", b) for b in range(4)])
                    kb.dma("sp", KT[P, 0:T], cout_kr[0:32, :], R=["cout_kr"], W=[("KTr", b) for b in range(4)])
                    for tb in range(NTB):
                        hb = hT1[:, :, tb * TB:(tb + 1) * TB]
                        HB = HBt(tb)
                        psG, pkG = kb.ps()

                        def fng(e, psG=psG, hb=hb):
                            for cc in range(4):
                                for k in range(NK):
                                    ins = e.matmul(psG[:, cc * 8:(cc + 1) * 8], hb[:, k, cc * 128:(cc + 1) * 128], wG[:, k, :],
                                                   start=(k == 0), stop=(k == NK - 1))
                            return ins
                        kb.op("pe", fng, R=["wG"] + HB, W=[pkG])
                        gates(tb, psG, pkG, gf, ge, gt)
                        for cc in range(4):
                            c = tb * 4 + cc
                            t0 = cc * 128
                            psK, pkK = kb.ps()
                            kb.mm(psK[:, :], [(hb[:, k, t0:t0 + 128], tK[:, k, :]) for k in range(NK)], R=[kK] + HB, W=[pkK])
                            psV, pkV = kb.ps()
                            kb.mm(psV[:, :], [(hb[:, k, t0:t0 + 128], tV[:, k, :]) for k in range(NK)], R=[kV] + HB, W=[pkV])
                            kwt, kwk = kw[c % 2], ("kw", c % 2)
                            vat, vak = vaug[c % 2], ("vaug", c % 2)
                            for h in range(4):
                                kb.op("dve", TS(kwt[:, h, :], psK[:, h * 128:(h + 1) * 128], gW[:, c, h:h + 1], ALU.mult, SC, ALU.mult),
                                      R=[pkK, ("g", c // 4, "W")], W=[kwk + (h,)])
                                kb.op("act", ACTF(vat[:, h, 0:128], psV[:, h * 128:(h + 1) * 128], AF.Copy), R=[pkV], W=[vak + (h,)])
                                psD, pkD = kb.ps()
                                kb.mm(psD[:, :256], [(kwt[:, h, :], vat[:, h, :])], R=[kwk + (h,), vak + (h,), ("vaug1", c % 2)], W=[pkD])
                                kb.op("dve", STT(state[:, h, :], state[:, h, :], gEG[:, c, h:h + 1], psD[:, :256], ALU.mult, ALU.add),
                                      R=[pkD, ("g", c // 4, "EG"), ("state", h)], W=[("state", h)])
                    mla_weights()
                    kb.dma("sp", cin_st.rearrange("p (h n) -> p h n", h=4), state[:, :, :], R=[("state", h) for h in range(4)], W=["cin_st"])
                    kb.cc(cin_st[:, :], cout_st[:, :], R=["cin_st"], W=["cout_st"])
                    kb.flush()
                if sub < 2:
                    return

                with ExitStack() as ss:
                    Vaug = sb("Vaug", [128, 32, 128], BF16, ss)
                    QTs = [sb(f"QT{i}", [128, T], BF16, ss) for i in range(2)]
                    PT = [sb(f"PT{i}", [128, TB], BF16, ss) for i in range(5)]
                    accS = sb("accS", [128, TB], F32, ss)
                    rden = sb("rden2", [128, TB], F32, ss)
                    mixa = sb("mixa", [128, T], BF16, ss)
                    kra = sb("kra2", [128, TB], F32, ss)
                    krb = sb("krb2", [128, TB], F32, ss)
                    ip = 0
                    nacc = 0
                    QSC = 96.0 ** -0.5
                    def qsetup(h):
                        QTh = QTs[h % 2]
                        for tb in range(NTB):
                            cs = slice(tb * TB, (tb + 1) * TB)
                            ps, pk = kb.ps()
                            kb.mm(ps[0:64, :], [(wuq[:, i, h * 96:h * 96 + 64], cqn[:, i, cs]) for i in range(3)],
                                  R=[("wA", 0), ("cqn", tb)], W=[pk])
                            kb.op("dve", COPY(QTh[0:64, cs], ps[0:64, :]), R=[pk], W=[("QT", h % 2, tb)])
                            pst, pkt = kb.ps()
                            kb.mm(pst[P, :], [(wuq[:, i, h * 96 + 64:h * 96 + 96], cqn[:, i, cs]) for i in range(3)],
                                  R=[("wA", 0), ("cqn", tb)], W=[pkt])
                            pss, pks = kb.ps()
                            kb.mm(pss[P, :], [(wuqs[:, i, h, :], cqn[:, i, cs]) for i in range(3)],
                                  R=[("wA", 0), ("cqn", tb)], W=[pks])
                            kb.op("dve", TT(kra[P, :], pst[P, :], ctab[P, cs], ALU.mult), R=[pkt, ("ctab", tb)], W=["kra2"])
                            kb.op("dve", TT(krb[P, :], pss[P, :], stab[P, cs], ALU.mult), R=[pks, ("stab", tb)], W=["krb2"])
                            kb.op("dve", TT(QTh[P, cs], kra[P, :], krb[P, :], ALU.add), R=["kra2", "krb2"], W=[("QTr", h % 2, tb)])

                    qsetup(0)
                    for h in range(8):
                        par = h % 2
                        vo, oo = (0, 64) if par == 0 else (64, 0)
                        lo, hi = (0, 64) if par == 0 else (64, 128)
                        kb.op("pool", MEMSET(Vaug[:, 16:32, oo:oo + 64], 1.0), W=["Vaug"])
                        kb.op("pool", MEMSET(Vaug[:, 0:16, oo:oo + 64], 1.0), W=["Vaug"])
                        kb.op("dve", TS(Vaug[:, 0:16, oo:oo + 64], Vaug[:, 0:16, oo:oo + 64], col("flag"), ALU.mult, 1.0, ALU.mult), R=["Vaug", "cols"], W=["Vaug"])
                        for blk in range(8):
                            ps, pk = kb.ps()
                            bs = slice(blk * TB, (blk + 1) * TB)
                            kb.mm(ps[0:64, :], [(wukv[:, i, h * 128:h * 128 + 64], ckvn[:, i, bs]) for i in range(2)],
                                  R=[("wA", 1), ("ckvn", blk)], W=[pk])
                            kb.op("dve", COPY(KT[0:64, bs], ps[0:64, :]), R=[pk], W=[("KTn", blk)])
                        for g4 in range(8):
                            ps, pk = kb.ps()

                            def fnv(e, ps=ps, g4=g4, h=h):
                                for j in range(4):
                                    kk = g4 * 4 + j
                                    for i in range(2):
                                        ins = e.matmul(ps[:, j * 64:(j + 1) * 64], ckvn[:, i, kk * 128:(kk + 1) * 128],
                                                       wukv[:, i, h * 128 + 64:h * 128 + 128], start=(i == 0), stop=(i == 1))
                                return ins
                            kb.op("pe", fnv, R=[("wA", 1), ("ckvn", g4)], W=[pk])
                            dstv = Vaug[:, g4 * 4:(g4 + 1) * 4, vo:vo + 64]
                            srcv = ps[:, 0:256].rearrange("p (j c) -> p j c", c=64)
                            if g4 < 4:
                                kb.op("act", ACTF(dstv, srcv, AF.Copy, scale=col("flag")), R=[pk, "cols"], W=["Vaug"])
                            else:
                                kb.op("dve", COPY(dstv, srcv), R=[pk], W=["Vaug"])
                        accs = {}
                        items = [(qb, kk) for qb in range(NTB) for kk in range(16 + 4 * qb + 4)]
                        pend = []

                        def emit_pv(qb, kk, q0, nq, p, ppk, par=par, lo=lo, hi=hi, accs=accs):
                            acc, ak = accs[qb]
                            nkb = 16 + 4 * qb + 4
                            kb.op("pe", MM1(acc[:, q0:TB], Vaug[:, kk, :], p[:, :nq], kk == 0, kk == nkb - 1),
                                  R=["Vaug", ppk], W=[ak])
                            if kk == nkb - 1:
                                kb.op("dve", COPY(accS[:, :], acc[:, :]), R=[ak], W=["accS"])
                                psF, pkF = kb.ps()
                                kb.mm(psF[:, :], [((shE if par == 0 else shO)[:, :], accS[:, :])], R=["shE", "shO", "accS"], W=[pkF])
                                kb.op("act", ACTF(rden[lo:hi, :], psF[lo:hi, :], AF.Ln), R=[pkF], W=["rden2"])
                                kb.op("act", ACTF(rden[lo:hi, :], rden[lo:hi, :], AF.Exp, scale=-1.0), R=["rden2"], W=["rden2"])
                                kb.op("dve", TT(mixa[lo:hi, qb * TB:(qb + 1) * TB], accS[lo:hi, :], rden[lo:hi, :], ALU.mult),
                                      R=["accS", "rden2"], W=[("mixa", qb)])
                                kb.free(ak)
                        QT = QTs[h % 2]
                        for qb, kk in items:
                            if kk == 0:
                                accs[qb] = kb.ps(hold=True)
                                if qb == 1 and h + 1 < 8:
                                    qsetup(h + 1)
                            r = kk - (16 + 4 * qb)
                            q0 = 128 * r if r > 0 else 0
                            nq = TB - q0
                            psS, pkS = kb.ps()
                            kb.mm(psS[:, :nq], [(KT[0:96, kk * 128:(kk + 1) * 128], QT[0:96, qb * TB + q0:(qb + 1) * TB])],
                                  R=[("KTn", kk // 4), ("KTr", kk // 4), ("QT", h % 2, qb), ("QTr", h % 2, qb)], W=[pkS])
                            p, ppk = PT[ip % 5], ("PT", ip % 5)
                            ip += 1
                            kb.op("act", ACTF(p[:, :nq], psS[:, :nq], AF.Exp, scale=QSC), R=[pkS], W=[ppk])
                            if r >= 0:
                                kb.op("pool", TT(p[:, 0:128], p[:, 0:128], tri_bf[:, :], ALU.mult), R=[ppk, "tri_bf"], W=[ppk])
                            pend.append((qb, kk, q0, nq, p, ppk))
                            if len(pend) > 3:
                                emit_pv(*pend.pop(0))
                        while pend:
                            emit_pv(*pend.pop(0))
                        if par == 1:
                            bt, bk = loadB(w_out, [512 + (h // 2) * 128])
                            for tb in range(NTB):
                                contract(bt, bk, [(mixa[:, tb * TB:(tb + 1) * TB], ("mixa", tb))], tb)
                    kb.flush()
                s2.close()
                if sub < 3:
                    return

                with ExitStack() as ss:
                    kb.dma("sp", state[:, :, :], cout_st[0:128, :].rearrange("p (h n) -> p h n", h=4), R=["cout_st"],
                           W=[("state", h) for h in range(4)])
                    for h in range(4):
                        kb.op("dve", TS(state[:, h, :], state[:, h, :], col("flag"), ALU.mult), R=[("state", h), "cols"], W=[("state", h)])
                    NSL = 3
                    hmixs = [sb(f"hmix{i}", [128, 4, TB], BF16, ss) for i in range(2)]
                    wAx = [wA[0], wA[1], sb("wA2", [128, NK * 512], BF16, ss), sb("wA3", [128, NK * 512], BF16, ss)]
                    w_in3 = w_in.rearrange("(kc p) n -> p kc n", p=128)
                    slots = []
                    for i in range(NSL):
                        d = dict(qT=sb(f"mqT{i}", [128, TB], BF16, ss), kT=sb(f"mkT{i}", [128, TB], BF16, ss),
                                 sig=sb(f"msig{i}", [128, TB], F32, ss), kw4=sb(f"kw4{i}", [128, 4, 128], BF16, ss),
                                 va4=sb(f"va4{i}", [128, 4, 256], BF16, ss), nlfrep=sb(f"nlfrep{i}", [128, 4, 128], F32, ss),
                                 E1=sb(f"E1{i}", [128, TB], F32, ss), qs=sb(f"qs{i}", [128, TB], BF16, ss),
                                 Y=sb(f"Y{i}", [128, TB], F32, ss),
                                 SW=sb(f"SW{i}", [128, TB], BF16, ss), sqh=sb(f"sqh{i}", [128, TB], BF16, ss),
                                 Sbf=[sb(f"Sbf{i}{j}", [128, 256], BF16, ss) for j in range(2)], i=i,
                                 )
                        kb.op("dve", MEMSET(d["va4"][:, :, 128:256], 1.0), W=[("va41", i)])
                        slots.append(d)
                    pieces = {}

                    def head_gen(item, sidx):
                        tb, h = item
                        S = slots[sidx]
                        si = S["i"]
                        K = lambda n: (n, si)
                        qT, kT, sig, kw4, va4, nlfrep, E1, qs, Y, SW, sqh = (S[n] for n in (
                            "qT", "kT", "sig", "kw4", "va4", "nlfrep", "E1", "qs", "Y", "SW", "sqh"))
                        Wx = Y
                        hb = hT1[:, :, tb * TB:(tb + 1) * TB]
                        HBK = HBt(tb)
                        hmix = hmixs[tb % 2]
                        R2 = nlfrep[:, :, :].rearrange("p c n -> p (c n)")
                        gk = ("g", tb)
                        if h not in pieces:
                            tpv = wAx[h][:, :].rearrange("p (kc n) -> p kc n", n=512)
                            kpk = ("wA", h) if h < 2 else ("wA2", h)
                            for i_, c0_ in enumerate((OQ, OKk, OV, OO)):
                                kb.dma("pool", tpv[:, :, i_ * 128:(i_ + 1) * 128], w_in3[:, :, c0_ + h * 128:c0_ + (h + 1) * 128], W=[kpk])
                            kb.dma("pool", wB[h // 2][:, h % 2, :], w_out[h * 128:(h + 1) * 128, :], W=[("wB", h // 2)])
                            pieces[h] = (tpv, kpk)
                        tp, kp = pieces[h]
                        yield
                        ps, pk = kb.ps()
                        kb.mm(ps[:, :], [(tp[:, k, 0:128], hb[:, k, :]) for k in range(NK)], R=[kp] + HBK, W=[pk])
                        kb.op("act", ACTF(qT[:, :], ps[:, :], AF.Copy), R=[pk], W=[K("mqT")])
                        ps, pk = kb.ps()
                        kb.mm(ps[:, :], [(tp[:, k, 128:256], hb[:, k, :]) for k in range(NK)], R=[kp] + HBK, W=[pk])
                        kb.op("act", ACTF(kT[:, :], ps[:, :], AF.Copy), R=[pk], W=[K("mkT")])
                        yield
                        ps, pk = kb.ps()
                        kb.mm(ps[:, :], [(tp[:, k, 384:512], hb[:, k, :]) for k in range(NK)], R=[kp] + HBK, W=[pk])
                        kb.op("act", ACTF(sig[:, :], ps[:, :], AF.Sigmoid), R=[pk], W=[K("msig")])
                        psK, pkK = kb.ps()

                        def fnk(e):
                            for cc in range(4):
                                for k in range(NK):
                                    ins = e.matmul(psK[:, cc * 128:(cc + 1) * 128], hb[:, k, cc * 128:(cc + 1) * 128], tp[:, k, 128:256],
                                                   start=(k == 0), stop=(k == NK - 1))
                            return ins
                        kb.op("pe", fnk, R=[kp] + HBK, W=[pkK])
                        for cc in range(4):
                            c = tb * 4 + cc
                            kb.op("dve", TS(kw4[:, cc, :], psK[:, cc * 128:(cc + 1) * 128], gW[:, c, h:h + 1], ALU.mult, SC, ALU.mult),
                                  R=[pkK, gk + ("W",)], W=[K("kw4")])
                        yield
                        psV, pkV = kb.ps()

                        def fnv2(e):
                            for cc in range(4):
                                for k in range(NK):
                                    ins = e.matmul(psV[:, cc * 128:(cc + 1) * 128], hb[:, k, cc * 128:(cc + 1) * 128], tp[:, k, 256:384],
                                                   start=(k == 0), stop=(k == NK - 1))
                            return ins
                        kb.op("pe", fnv2, R=[kp] + HBK, W=[pkV])
                        kb.op("act", ACTF(va4[:, :, 0:128], psV[:, :].rearrange("p (c n) -> p c n", n=128), AF.Copy), R=[pkV], W=[K("va4")])
                        for cc in range(4):
                            c = tb * 4 + cc
                            kb.op("dve", TS(nlfrep[:, cc, :], ones_f[:, :], gNLF[:, c, h:h + 1], ALU.mult), R=["ones_f", gk + ("NLF",)], W=[K("nlfrep")])
                        psR, pkR = kb.ps(hold=True)

                        def fnr(e):
                            for cc in range(4):
                                ins = e.matmul(psR[:, cc * 128:(cc + 1) * 128], nlfrep[:, cc, :], tri_f[:, :], start=True, stop=True)
                            return ins
                        kb.op("pe", fnr, R=[K("nlfrep"), "tri_f"], W=[pkR])
                        yield
                        kb.op("act", ACTF(E1[:, :], psR[:, :], AF.Exp, scale=-1.0), R=[pkR], W=[K("E1")])
                        kb.op("dve", TT(qs[:, :], qT[:, :], E1[:, :], ALU.mult), R=[K("mqT"), K("E1")], W=[K("qs")])
                        for cc in range(4):
                            c = tb * 4 + cc
                            cs4 = slice(cc * 128, (cc + 1) * 128)
                            kb.op("dve", TS(Y[:, cs4], psR[:, cs4], gNB[:, c, h:h + 1], ALU.subtract, 0.0, ALU.max), R=[pkR, gk + ("NB",)], W=[K("Y")])
                            kb.op("act", ACTF(Wx[:, cs4], Y[:, cs4], AF.Exp, scale=-1.0, bias=gI[:, c, h:h + 1]), R=[K("Y"), gk + ("I",)], W=[K("Y")])
                        kb.free(pkR)
                        yield
                        for cc in range(4):
                            cs4 = slice(cc * 128, (cc + 1) * 128)
                            kb.op("pool", TT(Wx[:, cs4], Wx[:, cs4], tri_f[:, :], ALU.mult), R=[K("Y"), "tri_f"], W=[K("Y")])
                        psS, pkS = kb.ps()

                        def fns(e):
                            for cc in range(4):
                                cs4 = slice(cc * 128, (cc + 1) * 128)
                                ins = e.matmul(psS[:, cs4], kT[:, cs4], qT[:, cs4], start=True, stop=True)
                            return ins
                        kb.op("pe", fns, R=[K("mkT"), K("mqT")], W=[pkS])
                        kb.op("dve", STT(SW[:, :], psS[:, :], SC, Wx[:, :], ALU.mult, ALU.mult), R=[pkS, K("Y")], W=[K("SW")])
                        yield
                        psN, pkN = kb.ps(hold=True)
                        psE, pkE = kb.ps(hold=True)
                        for cc in range(4):
                            c = tb * 4 + cc
                            cs4 = slice(cc * 128, (cc + 1) * 128)
                            sbt, sbk = S["Sbf"][cc % 2], ("Sbf", si, cc % 2)
                            kb.op("act", ACTF(sbt[:, :], state[:, h, :], AF.Copy), R=[("state", h)], W=[sbk])

                            def fnn(e, sbt=sbt, cs4=cs4, cc=cc):
                                e.matmul(psN[:, cs4], sbt[:, 0:128], qs[:, cs4], start=True, stop=False)
                                e.matmul(psN[:, cs4], va4[:, cc, 0:128], SW[:, cs4], start=False, stop=True)
                                e.matmul(psE[:, cs4], sbt[:, 128:256], qs[:, cs4], start=True, stop=False)
                                return e.matmul(psE[:, cs4], ones_bf[:, :], SW[:, cs4], start=False, stop=True)
                            kb.op("pe", fnn, R=[sbk, K("qs"), K("va4"), K("SW"), "ones_bf"], W=[pkN, pkE])
                            psD, pkD = kb.ps()
                            kb.mm(psD[:, :256], [(kw4[:, cc, :], va4[:, cc, :])], R=[K("kw4"), K("va4"), ("va41", si)], W=[pkD])
                            kb.op("dve", STT(state[:, h, :], state[:, h, :], gEG[:, c, h:h + 1], psD[:, :256], ALU.mult, ALU.add),
                                  R=[pkD, gk + ("EG",), ("state", h)], W=[("state", h)])
                            yield
                        dd, hm = E1, Y
                        kb.op("act", ACTF(dd[:, :], psE[:, :], AF.Abs), R=[pkE], W=[K("E1")])
                        kb.op("dve", TS(dd[:, :], dd[:, :], 1.0, ALU.max), R=[K("E1")], W=[K("E1")])
                        kb.op("act", ACTF(dd[:, :], dd[:, :], AF.Ln), R=[K("E1")], W=[K("E1")])
                        kb.op("act", ACTF(dd[:, :], dd[:, :], AF.Exp, scale=-1.0), R=[K("E1")], W=[K("E1")])
                        kb.op("dve", TT(hm[:, :], psN[:, :], dd[:, :], ALU.mult), R=[pkN, K("E1")], W=[K("Y")])
                        kb.free(pkN)
                        kb.free(pkE)
                        yield
                        kb.op("pool", TT(hm[:, :], hm[:, :], sig[:, :], ALU.mult), R=[K("Y"), K("msig")], W=[K("Y")])
                        kb.op("act", ACTF(sqh[:, :], hm[:, :], AF.Square), R=[K("Y")], W=[K("sqh")])
                        psQ, pkQ = kb.ps(hold=True)
                        kb.mm(psQ[:, :], [(ones_bf[:, :], sqh[:, :])], R=["ones_bf", K("sqh")], W=[pkQ])
                        yield
                        kb.op("act", ACTF(dd[:, :], psQ[:, :], AF.Ln, scale=1.0 / 128, bias=epsc[:, 0:1]), R=[pkQ, "epsc2"], W=[K("E1")])
                        kb.free(pkQ)
                        kb.op("act", ACTF(dd[:, :], dd[:, :], AF.Exp, scale=-0.5), R=[K("E1")], W=[K("E1")])
                        kb.op("dve", STT(hmix[:, h, :], hm[:, :], col("ml_norm", h), dd[:, :], ALU.mult, ALU.mult),
                              R=[K("Y"), K("E1"), "cols"], W=[("hmix", tb % 2, h)])
                        if h == 3:
                            yield
                            for o in range(NK):
                                ps, pk = kb.ps()
                                osl = slice(o * 128, (o + 1) * 128)
                                kb.mm(ps[:, :], [(wB[hh // 2][:, hh % 2, osl], hmix[:, hh, :]) for hh in range(4)],
                                      R=[("wB", 0), ("wB", 1)] + [("hmix", tb % 2, hh) for hh in range(4)], W=[pk])
                                accum_x(tb, o, ps, pk)
                                if o == 3:
                                    yield

                    run_interleaved([(tb, h) for tb in range(NTB) for h in range(4)], head_gen, nslots=NSL)
                    if post:
                        post()
                    kb.flush()

        def alloc_hT(stack):
            hT_holder[0] = sb("hT_s", [128, NK, T + HALO], BF16, stack)
            st_holder[0] = mk_norm_tmp(stack)

        OCQ_, OCKV_, OKR_ = 2056, 2440, 2696

        def pre_xattn(l):
            def f():
                PRE[f"xk{l}"] = loadA(W["xattn_wkv"][l], [(0, 512)])
                PRE[f"xv{l}"] = loadA(W["xattn_wkv"][l], [(512, 512)])
            return f

        def pre_ffn(l):
            def f():
                PRE[f"ffnA{l}_0"] = loadA(W["ffn_gu"][l], [(0, 256), (FFN_H, 256)])
                PRE[f"ffnB{l}_0"] = loadB(W["ffn_down"][l], [0, 128])
            return f

        def pre_odd():
            wi = W["od_w_in"][0]
            PRE["oddL1"] = loadA(wi, [(OCQ_, 384), (OKR_, 32), (OKR_ + 16, 16), (OKR_, 16)])
            PRE["oddL2"] = loadA(wi, [(OCKV_, 256)])

        with ExitStack() as g0:
            alloc_hT(g0)
            if stop_after >= 1:
                even_mixer(post=pre_xattn(0) if stop_after >= 2 else None)
            if stop_after >= 2:
                xattn(0, post=pre_ffn(0) if stop_after >= 3 else None)
            if stop_after >= 3:
                ffn(0, post=pre_odd if stop_after >= 4 else None)
        if stop_after >= 4:
            odd_mixer(stop_after - 3 if stop_after < 7 else 9, post=pre_xattn(1) if stop_after >= 7 else None)
        if stop_after >= 7:
            with ExitStack() as g1:
                alloc_hT(g1)
                xattn(1, post=pre_ffn(1) if stop_after >= 8 else None)
                if stop_after >= 8:
                    ffn(1)
        final_out("final_norm" if stop_after >= 99 else None)
    return nc


def _host_consts(inputs, h):
    cols = np.zeros((128, NCOL), np.float32)

    def put(name, vec, i0=0):
        v = np.asarray(vec, np.float32).reshape(-1, 128).T
        cols[:, COLS[name] + i0:COLS[name] + i0 + v.shape[1]] = v
    for l in range(2):
        put(f"norm_mix{l}", inputs["norm_mix_g"][l])
        put(f"norm_xattn{l}", inputs["norm_xattn_g"][l])
        put(f"mem_norm{l}", inputs["mem_norm_g"][l])
        put(f"norm_ffn{l}", inputs["norm_ffn_g"][l])
    put("final_norm", inputs["final_norm_g"])
    cw = np.asarray(inputs["ev_conv_w"][0], np.float32)
    for j in range(4):
        for k in range(3):
            cols[:, COLS["conv_w"] + j * 3 + k] = cw[k, j * 128:(j + 1) * 128]
    put("pool_scale", inputs["ev_pool_scale"][0])
    put("ml_norm", inputs["od_ml_norm_g"][0])
    put("q_norm", inputs["od_q_norm_g"][0])
    put("kv_norm", inputs["od_kv_norm_g"][0])
    inv = (10000.0 ** (-np.arange(16, dtype=np.float32) / 16)).astype(np.float32)
    p = np.arange(128)
    cols[:, COLS["inv_freq"]] = inv[p % 16]
    cols[:, COLS["rope_sign"]] = np.where((p % 32) < 16, -1.0, 1.0)
    cols[:, COLS["flag"]] = float(h)
    cols[:, COLS["gate_bias"]:COLS["gate_bias"] + 32] = np.tile(np.asarray(inputs["od_gate_bias"][0], np.float32), 4)[None, :]
    for j, w in enumerate((2, 4, 8, 16)):
        t = np.arange(16)
        corr = (w / np.minimum(t + 1, w)).astype(np.float32) if h == 0 else np.ones(16, np.float32)
        cols[:, COLS["poolcorr"] + j * 16:COLS["poolcorr"] + (j + 1) * 16] = corr[None, :]
    return cols


STOP_AFTER = 99


def kernel(**inputs):
    x = np.asarray(inputs["x"], np.float32)
    mem = np.asarray(inputs["mem"], np.float32)
    pos = np.asarray(inputs["positions"], np.int32)
    B, S, _ = x.shape
    tri = np.triu(np.ones((128, 128), np.float32))
    shE = np.zeros((128, 128), np.float32)
    shO = np.zeros((128, 128), np.float32)
    for i in range(64):
        shE[64 + i, i] = 1.0
        shO[i, 64 + i] = 1.0
    wnames = ["xattn_wq", "xattn_wkv", "xattn_wo", "ffn_w_gate_up", "ffn_w_down", "ev_w_in", "ev_pool_w",
              "ev_w_out", "od_w_in", "od_w_uq", "od_w_ukv", "od_w_out"]
    wts = {n: np.ascontiguousarray(np.asarray(inputs[n], np.float32)) for n in wnames}
    in_maps = []
    for c in range(8):
        b, h = c // 2, c % 2
        xt = np.zeros((D, T + HALO), np.float32)
        xt[:, HALO:] = x[b, h * T:(h + 1) * T, :].T
        if h == 1:
            xt[:, :HALO] = x[b, T - HALO:T, :].T
        m = dict(wts)
        m["xT"] = xt
        m["memT"] = np.ascontiguousarray(mem[b].T)
        m["posb"] = np.ascontiguousarray(np.broadcast_to(pos[b, h * T:(h + 1) * T][None, :], (128, T)))
        m["cols"] = _host_consts(inputs, h)
        m["tri"] = tri
        m["shiftE"] = shE
        m["shiftO"] = shO
        in_maps.append(m)
    nc = build(STOP_AFTER)
    res = run_bass_kernel_spmd(nc, in_maps, core_ids=list(range(8)))
    out = np.zeros((B, S, D), np.float32)
    for c in range(8):
        b, h = c // 2, c % 2
        out[b, h * T:(h + 1) * T, :] = res.results[c]["outT"].T
    return out
```

```python
import numpy as np
from contextlib import ExitStack
import concourse.bass as bass
import concourse.mybir as mybir
from concourse.bass_utils import run_bass_kernel_spmd

F32, BF16, I32 = mybir.dt.float32, mybir.dt.bfloat16, mybir.dt.int32
AF = mybir.ActivationFunctionType
ALU = mybir.AluOpType


def TT(out, in0, in1, op):
    return lambda e: e.tensor_tensor(out=out, in0=in0, in1=in1, op=op)


def TS(out, in0, s1, op0, s2=None, op1=None):
    if op1 is None:
        return lambda e: e.tensor_scalar(out=out, in0=in0, scalar1=s1, scalar2=None, op0=op0)
    return lambda e: e.tensor_scalar(out=out, in0=in0, scalar1=s1, scalar2=s2, op0=op0, op1=op1)


def STT(out, in0, scalar, in1, op0, op1):
    return lambda e: e.scalar_tensor_tensor(out=out, in0=in0, scalar=scalar, in1=in1, op0=op0, op1=op1)


def ACTF(out, in_, func, scale=None, bias=None):
    kw = {}
    if scale is not None:
        kw["scale"] = scale
    if bias is not None:
        kw["bias"] = bias
    return lambda e: e.activation(out=out, in_=in_, func=func, **kw)


def RECIP(out, in_):
    return lambda e: e.reciprocal(out=out, in_=in_)


def RECIPA(out, in_, scratch):
    return lambda e: e.reciprocal_approx_accurate(out=out, in_=in_, scratch=scratch)


def MEMSET(ap, v):
    return lambda e: e.memset(ap, v)


def COPY(out, in_):
    return lambda e: e.tensor_copy(out=out, in_=in_)


def MM1(out, l, r, start, stop):
    return lambda e: e.matmul(out, l, r, start=start, stop=stop)

T = 2048
HALO = 16
TB = 512
NTB = T // TB
D = 1024
NK = 8
FFN_H = 2816
EPS = 1e-6
TWO_PI = float(2.0 * np.pi)
PI = float(np.pi)

COLS = {}
_ncol = 0


def _defcol(name, n):
    global _ncol
    COLS[name] = _ncol
    _ncol += n


for _l in range(2):
    _defcol(f"norm_mix{_l}", 8)
    _defcol(f"norm_xattn{_l}", 8)
    _defcol(f"mem_norm{_l}", 8)
    _defcol(f"norm_ffn{_l}", 8)
_defcol("final_norm", 8)
_defcol("conv_w", 12)
_defcol("pool_scale", 4)
_defcol("ml_norm", 4)
_defcol("q_norm", 3)
_defcol("kv_norm", 2)
_defcol("inv_freq", 1)
_defcol("rope_sign", 1)
_defcol("flag", 1)
_defcol("gate_bias", 32)
_defcol("poolcorr", 64)
NCOL = _ncol


class KB:
    ENG = ("pe", "act", "dve", "pool", "sp")

    def __init__(self, nc, es):
        self.nc, self.es = nc, es
        self.prog = {e: [] for e in self.ENG}
        self.sem, self.cnt = {}, {}
        self.nroll = 0
        for e in self.ENG[:4]:
            self._newsem(e)
        self.seen = {e: {} for e in self.ENG}
        self.lastw, self.reads = {}, {}
        self.dsems = [[es.enter_context(nc.semaphore(f"dq{i}")), 0] for i in range(40)]
        self.dnext = 0
        self.nbank = 0
        self.held = [False] * 8
        self.banks = [es.enter_context(nc.psum_tensor(f"bank{i}", [128, 512], F32)) for i in range(8)]

    def _newsem(self, e):
        self.sem[e] = self.es.enter_context(self.nc.semaphore(f"c_{e}_{self.nroll}"))
        self.nroll += 1
        self.cnt[e] = 0

    def ps(self, hold=False):
        for _ in range(16):
            i = self.nbank % 8
            self.nbank += 1
            if not self.held[i]:
                break
        else:
            raise RuntimeError("all PSUM banks held")
        if hold:
            self.held[i] = True
        return self.banks[i], ("bank", i)

    def free(self, key):
        self.held[key[1]] = False

    def _wait(self, eng, tok):
        sem, val, src = tok
        if src == eng and eng == "pe":
            return
        d = self.seen[eng]
        if d.get(id(sem), 0) >= val:
            return
        d[id(sem)] = val
        self.prog[eng].append(("w", sem, val))

    def _deps(self, eng, R, W):
        for k in R:
            t = self.lastw.get(k)
            if t:
                self._wait(eng, t)
        for k in W:
            t = self.lastw.get(k)
            if t:
                self._wait(eng, t)
            rd = self.reads.get(k)
            if rd:
                for src, toks in rd.items():
                    if src == eng:
                        continue
                    for t in toks:
                        self._wait(eng, t)

    def _record(self, tok, R, W):
        src = tok[2]
        for k in R:
            rd = self.reads.setdefault(k, {})
            if src == "dma":
                rd.setdefault(src, []).append(tok)
            else:
                rd[src] = [tok]
        for k in W:
            self.lastw[k] = tok
            self.reads[k] = {}

    def op(self, eng, fn, R=(), W=()):
        self._deps(eng, R, W)
        self.cnt[eng] += 1
        sem = self.sem[eng]
        tok = (sem, self.cnt[eng], eng)
        self.prog[eng].append(("o", fn, sem))
        self._record(tok, R, W)
        if self.cnt[eng] >= 30000:
            self._newsem(eng)

    def mm(self, out, pairs, R=(), W=()):
        n = len(pairs)

        def fn(e):
            for i, (l, r) in enumerate(pairs):
                ins = e.matmul(out, l, r, start=(i == 0), stop=(i == n - 1))
            return ins
        self.op("pe", fn, R, W)

    def dma(self, q, out, in_, R=(), W=()):
        self._deps(q, R, W)
        ds = self.dsems[self.dnext]
        self.dnext = (self.dnext + 1) % len(self.dsems)
        if ds[1] > 0:
            self._wait(q, (ds[0], ds[1], "dma"))
        ds[1] += 16
        tok = (ds[0], ds[1], "dma")
        self.prog[q].append(("d", out, in_, ds[0]))
        self._record(tok, R, W)
        return tok

    def cc(self, in_ap, out_ap, R=(), W=()):
        self._deps("pool", R, W)
        sem = self.es.enter_context(self.nc.semaphore(f"ccs{self.nroll}"))
        self.nroll += 1
        tok = (sem, 1, "cc")
        self.prog["pool"].append(("c", in_ap, out_ap, sem))
        self._record(tok, R, W)

    def wait_tok(self, eng, tok):
        self._wait(eng, tok)

    def raw(self, eng, fn):
        self.prog[eng].append(("r", fn))

    def flush(self):
        with self.nc.Block(no_gpsimd_drain=True) as block:
            for e, deco in (("pe", block.tensor), ("act", block.scalar), ("dve", block.vector),
                            ("pool", block.gpsimd), ("sp", block.sync)):
                items = self.prog[e]
                self.prog[e] = []

                def body(eng, items=items):
                    for it in items:
                        if it[0] == "w":
                            eng.wait_ge(it[1], it[2])
                        elif it[0] == "o":
                            it[1](eng).then_inc(it[2], 1)
                        elif it[0] == "d":
                            eng.dma_start(out=it[1], in_=it[2]).then_inc(it[3], 16)
                        elif it[0] == "c":
                            eng.collective_compute("AllGather", ALU.bypass,
                                                   replica_groups=[[0, 1], [2, 3], [4, 5], [6, 7]],
                                                   ins=[it[1]], outs=[it[2]]).then_inc(it[3])
                        else:
                            it[1](eng)
                deco(body)


def build(stop_after=99):
    nc = bass.Bass("TRN2", target_bir_lowering=False)

    def din(name, shape, dt=F32):
        return nc.dram_tensor(name, list(shape), dt, kind="ExternalInput").ap()

    xT_d = din("xT", [D, T + HALO])
    memT_d = din("memT", [D, 256])
    pos_d = din("posb", [128, T], I32)
    cols_d = din("cols", [128, NCOL])
    tri_d = din("tri", [128, 128])
    shE_d = din("shiftE", [128, 128])
    shO_d = din("shiftO", [128, 128])
    W = {}
    W["xattn_wq"] = din("xattn_wq", [2, D, 512])
    W["xattn_wkv"] = din("xattn_wkv", [2, D, 1024])
    W["xattn_wo"] = din("xattn_wo", [2, 512, D])
    W["ffn_gu"] = din("ffn_w_gate_up", [2, D, 2 * FFN_H])
    W["ffn_down"] = din("ffn_w_down", [2, FFN_H, D])
    W["ev_w_in"] = din("ev_w_in", [1, D, 2048])
    W["ev_pool_w"] = din("ev_pool_w", [1, 4, 128, 128])
    W["ev_w_out"] = din("ev_w_out", [1, D, D])
    W["od_w_in"] = din("od_w_in", [1, D, 2728])
    W["od_w_uq"] = din("od_w_uq", [1, 384, 768])
    W["od_w_ukv"] = din("od_w_ukv", [1, 256, 1024])
    W["od_w_out"] = din("od_w_out", [1, D, D])
    out_d = nc.dram_tensor("outT", [D, T], F32, kind="ExternalOutput").ap()
    cin_st = nc.dram_tensor("cin_st", [128, 1024], F32, kind="Internal").ap()
    cout_st = nc.dram_tensor("cout_st", [256, 1024], F32, kind="Internal").ap()
    cin_kv = nc.dram_tensor("cin_kv", [128, 2 * T], BF16, kind="Internal").ap()
    cout_kv = nc.dram_tensor("cout_kv", [256, 2 * T], BF16, kind="Internal").ap()
    cin_kr = nc.dram_tensor("cin_kr", [32, T], BF16, kind="Internal").ap()
    cout_kr = nc.dram_tensor("cout_kr", [64, T], BF16, kind="Internal").ap()

    with ExitStack() as es:
        kb = KB(nc, es)

        uid = [0]

        def sb(name, shape, dt, stack=es):
            uid[0] += 1
            return stack.enter_context(nc.sbuf_tensor(f"{name}_{uid[0]}", list(shape), dt))

        xT = sb("xT_s", [128, NK, T + HALO], F32)
        hT_holder = [None]
        cols = sb("cols_s", [128, NCOL], F32)
        ones_bf = sb("ones_bf", [128, 128], BF16)
        ones_f = sb("ones_f", [128, 128], F32)
        tri_f = sb("tri_f", [128, 128], F32)
        tri_bf = sb("tri_bf", [128, 128], BF16)
        shE = sb("shE", [128, 128], F32)
        shO = sb("shO", [128, 128], F32)
        wA = [sb(f"wA{i}", [128, NK * 512], BF16) for i in range(2)]
        wB = [sb(f"wB{i}", [128, 2, D], BF16) for i in range(2)]
        wAn = [0]
        wBn = [0]

        def col(name, i=0):
            c = COLS[name] + i
            return cols[:, c:c + 1]

        def nextA():
            i = wAn[0] % 2
            wAn[0] += 1
            return wA[i], ("wA", i)

        def nextB():
            i = wBn[0] % 2
            wBn[0] += 1
            return wB[i], ("wB", i)

        PRE = {}

        def loadA(src2d, segs, name=None):
            if name is not None and name in PRE:
                return PRE.pop(name)
            t, k = nextA()
            t = t[:, :].rearrange("p (kc n) -> p kc n", n=512)
            o = 0
            v = src2d.rearrange("(kc p) n -> p kc n", p=128)
            for c0, n in segs:
                kb.dma("pool", t[:, :, o:o + n], v[:, :, c0:c0 + n], W=[k])
                o += n
            return t, k

        def loadB(src2d, rows, name=None):
            if name is not None and name in PRE:
                return PRE.pop(name)
            t, k = nextB()
            for i, r0 in enumerate(rows):
                kb.dma("pool", t[:, i, :], src2d[r0:r0 + 128, :], W=[k])
            return t, k

        for k in range(NK):
            kb.dma("sp", xT[:, k, 0:HALO + TB], xT_d[k * 128:(k + 1) * 128, 0:HALO + TB], W=[("x", k, 0), ("x", k, "halo")])
        for tb in range(1, NTB):
            for k in range(NK):
                cs_ = slice(HALO + tb * TB, HALO + (tb + 1) * TB)
                kb.dma("sp", xT[:, k, cs_], xT_d[k * 128:(k + 1) * 128, cs_], W=[("x", k, tb)])
        kb.dma("sp", cols[:], cols_d[:, :], W=["cols"])
        kb.dma("sp", tri_f[:], tri_d[:, :], W=["tri_f"])
        kb.dma("sp", shE[:], shE_d[:, :], W=["shE"])
        kb.dma("sp", shO[:], shO_d[:, :], W=["shO"])
        kb.dma("pool", tri_bf[:], tri_d[:, :], W=["tri_bf"])
        kb.op("dve", MEMSET(ones_bf[:], 1.0), W=["ones_bf"])
        kb.op("dve", MEMSET(ones_f[:], 1.0), W=["ones_f"])

        def mk_norm_tmp(ss):
            st = {"sq": [sb(f"sq{i}", [128, TB], BF16, ss) for i in range(2)], "n": 0,
                  "rs": sb("rs", [128, TB], F32, ss), "eps": sb("epsc", [128, 1], F32, ss)}
            kb.op("dve", MEMSET(st["eps"][:], EPS), W=["epsc"])
            return st

        def rms_block(srcs, dsts, gname, N, Dn, st):
            nk = len(srcs)
            ps, pk = kb.ps()
            for i, (s, sk) in enumerate(srcs):
                sq, sqk = st["sq"][st["n"] % 2], ("sq", st["n"] % 2)
                st["n"] += 1
                kb.op("act", ACTF(sq[:, :N], s, AF.Square), R=[sk], W=[sqk])
                kb.op("pe", MM1(ps[:, :N], ones_bf[:], sq[:, :N], i == 0, i == nk - 1),
                      R=[sqk, "ones_bf"], W=[pk])
            rs = st["rs"]
            kb.op("act", ACTF(rs[:, :N], ps[:, :N], AF.Ln, scale=1.0 / Dn, bias=st["eps"][:, 0:1]),
                  R=[pk, "epsc"], W=["rs"])
            kb.op("act", ACTF(ps[:, :N], rs[:, :N], AF.Exp, scale=-0.5), R=["rs"], W=[pk])
            for i, ((s, sk), (d, dk)) in enumerate(zip(srcs, dsts)):
                kb.op("dve", STT(d, s, col(gname, i), ps[:, :N], ALU.mult, ALU.mult),
                      R=[sk, pk, "cols"], W=[dk])

        st_holder = [None]

        def hkeys(tb):
            return [("h", k, tb) for k in range(NK)]

        def norm_block(gname, tb):
            hT = hT_holder[0]
            c0, n = (0, HALO) if tb == "halo" else (HALO + tb * TB, TB)
            srcs = [(xT[:, k, c0:c0 + n], ("x", k, tb)) for k in range(NK)]
            dsts = [(hT[:, k, c0:c0 + n], ("h", k, tb)) for k in range(NK)]
            rms_block(srcs, dsts, gname, n, D, st_holder[0])

        def accum_x(tb, o, ps, pk):
            xs = xT[:, o, HALO + tb * TB: HALO + (tb + 1) * TB]
            kb.op("dve", TT(xs, xs, ps[:, :], ALU.add), R=[pk, ("x", o, tb)], W=[("x", o, tb)])

        def contract(wt, wk, chunks, tb):
            for o in range(NK):
                ps, pk = kb.ps()
                kb.mm(ps[:, :], [(wt[:, c, o * 128:(o + 1) * 128], a) for c, (a, _) in enumerate(chunks)],
                      R=[wk] + [k for _, k in chunks], W=[pk])
                accum_x(tb, o, ps, pk)

        def run_interleaved(work, make_gen, nslots=2, pre=None):
            active = []
            free_slots = list(range(nslots))
            wi = 0
            while wi < len(work) or active:
                while wi < len(work) and free_slots:
                    if pre is not None:
                        pre(work[wi])
                    sidx = free_slots.pop(0)
                    active.append((make_gen(work[wi], sidx), sidx))
                    wi += 1
                for g in list(active):
                    try:
                        next(g[0])
                    except StopIteration:
                        active.remove(g)
                        free_slots.append(g[1])

        HK = [("h", k) for k in range(NK)]

        def even_mixer(post=None):
            hT = hT_holder[0]
            w_in = W["ev_w_in"][0]
            w_out = W["ev_w_out"][0]
            with ExitStack() as ss:
                poolw = sb("poolw", [128, 4, 128], BF16, ss)
                kb.dma("pool", poolw[:], W["ev_pool_w"][0].rearrange("g c d -> c g d"), W=["poolw"])
                NW = TB + HALO
                SL = []
                for i in range(4):
                    SL.append(dict(z=sb(f"z{i}", [128, 4, NW], F32, ss), u=sb(f"u{i}", [128, NW], F32, ss),
                                   acc=sb(f"cacc{i}", [128, TB], F32, ss), s1=sb(f"s1{i}", [128, NW], F32, ss),
                                   s2=sb(f"s2{i}", [128, NW], F32, ss), pm=sb(f"pm{i}", [128, TB], BF16, ss),
                                   mix=sb(f"mix{i}", [128, 2, TB], BF16, ss)))
                pieces = {}

                def gen(item, si):
                    j, tb = item
                    S = SL[si]
                    zt, u, acc, s1, s2, pmt, mx = S["z"], S["u"], S["acc"], S["s1"], S["s2"], S["pm"], S["mix"]
                    K = lambda *n: n + (si,)
                    if j not in pieces:
                        pieces[j] = (loadA(w_in, [(j * 128, 128), (512 + j * 128, 128), (1024 + j * 128, 128), (1536 + j * 128, 128)]),
                                     loadB(w_out, [j * 128, 512 + j * 128]))
                    (wt, wk), (bt, bk) = pieces[j]
                    win = (2, 4, 8, 16)[j]
                    c0 = tb * TB
                    HKA = hkeys(tb)
                    HKB = hkeys(tb - 1 if tb > 0 else "halo")
                    for i in range(4):
                        psA, pkA = kb.ps()
                        kb.mm(psA[:, :], [(wt[:, k, i * 128:(i + 1) * 128], hT[:, k, c0 + HALO:c0 + HALO + TB])
                                          for k in range(NK)], R=[wk] + HKA, W=[pkA])
                        psB, pkB = kb.ps()
                        kb.mm(psB[:, :HALO], [(wt[:, k, i * 128:(i + 1) * 128], hT[:, k, c0:c0 + HALO])
                                              for k in range(NK)], R=[wk] + HKB, W=[pkB])
                        kb.op("act", ACTF(zt[:, i, HALO:], psA[:, :], AF.Copy), R=[pkA], W=[K("z", i)])
                        kb.op("act", ACTF(zt[:, i, :HALO], psB[:, :HALO], AF.Copy), R=[pkB], W=[K("z", i, "h")])
                        if i % 2 == 1:
                            yield

                    def zkeys(i):
                        return [K("z", i), K("z", i, "h")]
                    kb.op("dve", TT(u[:, :], zt[:, 1, :], zt[:, 2, :], ALU.mult), R=zkeys(1) + zkeys(2), W=[K("u")])
                    kb.op("dve", TS(acc[:, :], u[:, HALO - 2:HALO - 2 + TB], col("conv_w", j * 3 + 0), ALU.mult),
                          R=[K("u"), "cols"], W=[K("cacc")])
                    for kk in (1, 2):
                        kb.op("dve", STT(acc[:, :], u[:, HALO - 2 + kk:HALO - 2 + kk + TB],
                                         col("conv_w", j * 3 + kk), acc[:, :], ALU.mult, ALU.add),
                              R=[K("u"), K("cacc"), "cols"], W=[K("cacc")])
                    kb.op("dve", TT(mx[:, 0, :], zt[:, 0, HALO:], acc[:, :], ALU.mult),
                          R=zkeys(0) + [K("cacc")], W=[K("mix", 0)])
                    yield
                    src, srck = zt[:, 3, :], zkeys(3)
                    bufs = [(s1, K("s1")), (s2, K("s2"))]
                    bi = 0
                    step = 1
                    while step < win:
                        dst, dk = bufs[bi]
                        bi ^= 1
                        kb.op("pool", COPY(dst[:, 0:step], src[:, 0:step]), R=srck, W=[dk])
                        kb.op("pool", TT(dst[:, step:NW], src[:, step:NW], src[:, 0:NW - step], ALU.add),
                              R=srck, W=[dk])
                        src, srck = dst[:, :], [dk]
                        step *= 2
                    yield
                    if tb == 0:
                        pc = COLS["poolcorr"] + j * 16
                        kb.op("dve", TT(src[:, HALO:2 * HALO], src[:, HALO:2 * HALO], cols[:, pc:pc + 16], ALU.mult),
                              R=srck + ["cols"], W=srck[:1])
                    kb.op("dve", STT(pmt[:, :], src[:, HALO:], 1.0 / win, zt[:, 3, HALO:], ALU.mult, ALU.subtract),
                          R=srck + zkeys(3), W=[K("pm")])
                    psP, pkP = kb.ps()
                    kb.mm(psP[:, :], [(poolw[:, j, :], pmt[:, :])], R=["poolw", K("pm")], W=[pkP])
                    kb.op("act", ACTF(mx[:, 1, :], psP[:, :], AF.Copy, scale=col("pool_scale", j)),
                          R=[pkP, "cols"], W=[K("mix", 1)])
                    yield
                    for o in range(NK):
                        ps, pk = kb.ps()
                        kb.mm(ps[:, :], [(bt[:, c, o * 128:(o + 1) * 128], mx[:, c, :]) for c in range(2)],
                              R=[bk, K("mix", 0), K("mix", 1)], W=[pk])
                        accum_x(tb, o, ps, pk)
                        if o == 3:
                            yield
                def pre(item):
                    j, tb = item
                    if j == 0:
                        if tb == 0:
                            norm_block("norm_mix0", "halo")
                        norm_block("norm_mix0", tb)
                run_interleaved([(j, tb) for j in range(4) for tb in range(NTB)], gen, nslots=4, pre=pre)
                if post:
                    post()
                kb.flush()

        def xattn(l, post=None):
            hT = hT_holder[0]
            wq = W["xattn_wq"][l]
            wkv = W["xattn_wkv"][l]
            wo = W["xattn_wo"][l]
            with ExitStack() as ss:
                memf = sb("memf", [128, NK, 256], F32, ss)
                memn = sb("memn", [128, NK, 256], BF16, ss)
                kT = sb("kT", [128, 4, 256], BF16, ss)
                vt = sb("vt", [128, 2, 512], BF16, ss)
                st = st_holder[0]
                for k in range(NK):
                    kb.dma("sp", memf[:, k, :], memT_d[k * 128:(k + 1) * 128, :], W=[("memf", k)])
                rms_block([(memf[:, k, :], ("memf", k)) for k in range(NK)],
                          [(memn[:, k, :], ("memn", k)) for k in range(NK)], f"mem_norm{l}", 256, D, st)
                mk_all = [("memn", k) for k in range(NK)]
                wt, wk = loadA(wkv, [(0, 512)], name=f"xk{l}")
                for h in range(4):
                    ps, pk = kb.ps()
                    kb.mm(ps[:, :256], [(wt[:, k, h * 128:(h + 1) * 128], memn[:, k, :]) for k in range(NK)],
                          R=[wk] + mk_all, W=[pk])
                    kb.op("act", ACTF(kT[:, h, :], ps[:, :256], AF.Copy), R=[pk], W=[("kT", h)])
                wt, wk = loadA(wkv, [(512, 512)], name=f"xv{l}")
                for mc in range(2):
                    ps, pk = kb.ps()
                    kb.mm(ps[:, :], [(memn[:, k, mc * 128:(mc + 1) * 128], wt[:, k, :]) for k in range(NK)],
                          R=[wk] + mk_all, W=[pk])
                    kb.op("act", ACTF(vt[:, mc, :], ps[:, :], AF.Copy), R=[pk], W=[("vt", mc)])
                wqt, wqk = loadA(wq, [(0, 512)])
                qT = [sb(f"qT{i}", [128, TB], BF16, ss) for i in range(4)]
                pT = [sb(f"pT{i}", [128, 2, TB], BF16, ss) for i in range(4)]
                oh = [sb(f"oh{i}", [128, 2, TB], BF16, ss) for i in range(2)]
                rdens = [sb(f"rden{i}", [128, TB], F32, ss) for i in range(4)]
                scale = 128.0 ** -0.5
                pieces = {}

                def gen(item, si):
                    tb, hp, hh = item
                    h = hp * 2 + hh
                    if hp not in pieces:
                        pieces[hp] = loadB(wo, [hp * 256, hp * 256 + 128])
                    bt, bk = pieces[hp]
                    c0 = HALO + tb * TB
                    oi = hp % 2
                    ot, ok = oh[oi], ("oh", oi)
                    q, qk = qT[si], ("qT", si)
                    p, pkk = pT[si], ("pT", si)
                    rden, rk = rdens[si], ("rden", si)
                    ps, pk = kb.ps()
                    kb.mm(ps[:, :], [(wqt[:, k, h * 128:(h + 1) * 128], hT[:, k, c0:c0 + TB]) for k in range(NK)],
                          R=[wqk] + hkeys(tb), W=[pk])
                    kb.op("act", ACTF(q[:, :], ps[:, :], AF.Copy), R=[pk], W=[qk])
                    yield
                    for mc in range(2):
                        ps2, pk2 = kb.ps()
                        kb.mm(ps2[:, :], [(kT[:, h, mc * 128:(mc + 1) * 128], q[:, :])], R=[("kT", h), qk], W=[pk2])
                        kb.op("act", ACTF(p[:, mc, :], ps2[:, :], AF.Exp, scale=scale), R=[pk2], W=[pkk + (mc,)])
                    yield
                    pso, pko = kb.ps()
                    kb.mm(pso[:, :], [(vt[:, mc, h * 128:(h + 1) * 128], p[:, mc, :]) for mc in range(2)],
                          R=[("vt", 0), ("vt", 1), pkk + (0,), pkk + (1,)], W=[pko])
                    psd, pkd = kb.ps()
                    kb.mm(psd[:, :], [(ones_bf[:, :], p[:, mc, :]) for mc in range(2)],
                          R=["ones_bf", pkk + (0,), pkk + (1,)], W=[pkd])
                    kb.op("act", ACTF(rden[:, :], psd[:, :], AF.Ln), R=[pkd], W=[rk])
                    kb.op("act", ACTF(rden[:, :], rden[:, :], AF.Exp, scale=-1.0), R=[rk], W=[rk])
                    kb.op("dve", TT(ot[:, hh, :], pso[:, :], rden[:, :], ALU.mult), R=[pko, rk], W=[ok + (hh,)])
                    if hh == 1:
                        yield
                        for o in range(NK):
                            ps, pk = kb.ps()
                            kb.mm(ps[:, :], [(bt[:, c, o * 128:(o + 1) * 128], ot[:, c, :]) for c in range(2)],
                                  R=[bk, ok + (0,), ok + (1,)], W=[pk])
                            accum_x(tb, o, ps, pk)
                            if o == 3:
                                yield
                normed = set()

                def pre(item):
                    if item[0] not in normed:
                        normed.add(item[0])
                        norm_block(f"norm_xattn{l}", item[0])
                run_interleaved([(tb, hp, hh) for tb in range(NTB) for hp in range(2) for hh in range(2)], gen, nslots=4, pre=pre)
                if post:
                    post()
                kb.flush()

        def ffn(l, post=None, final_g=None):
            hT = hT_holder[0]
            gname = f"norm_ffn{l}"
            gu = W["ffn_gu"][l]
            dn = W["ffn_down"][l]
            with ExitStack() as ss:
                sg = [sb(f"sg{i}", [128, TB], F32, ss) for i in range(2)]
                act = [sb(f"act{i}", [128, 2, TB], BF16, ss) for i in range(2)]
                it = 0
                ia = 0
                NG = FFN_H // 256
                ftoks = []
                if final_g is not None:
                    ob = [sb(f"obf{i}", [128, NK, TB], F32, ss) for i in range(2)]
                for g in range(NG):
                    wt, wk = loadA(gu, [(g * 256, 256), (FFN_H + g * 256, 256)], name=f"ffnA{l}_{g}")
                    bt, bk = loadB(dn, [g * 256, g * 256 + 128], name=f"ffnB{l}_{g}")
                    for tb in range(NTB):
                        c0 = HALO + tb * TB
                        if g == 0:
                            norm_block(gname, tb)
                        HK = hkeys(tb)
                        at, ak = act[ia % 2], ("act", ia % 2)
                        ia += 1
                        for c in range(2):
                            psg, pkg = kb.ps()
                            kb.mm(psg[:, :], [(wt[:, k, c * 128:(c + 1) * 128], hT[:, k, c0:c0 + TB]) for k in range(NK)],
                                  R=[wk] + HK, W=[pkg])
                            psu, pku = kb.ps()
                            kb.mm(psu[:, :], [(wt[:, k, 256 + c * 128:256 + (c + 1) * 128], hT[:, k, c0:c0 + TB])
                                              for k in range(NK)], R=[wk] + HK, W=[pku])
                            s, sk = sg[it % 2], ("sg", it % 2)
                            it += 1
                            kb.op("act", ACTF(s[:, :], psg[:, :], AF.Silu), R=[pkg], W=[sk])
                            kb.op("dve", TT(at[:, c, :], psu[:, :], s[:, :], ALU.mult), R=[pku, sk], W=[ak + (c,)])
                        contract(bt, bk, [(at[:, 0, :], ak + (0,)), (at[:, 1, :], ak + (1,))], tb)
                        if final_g is not None and g == NG - 1:
                            o, okk = ob[tb % 2], ("obf", tb % 2)
                            rms_block([(xT[:, k, c0:c0 + TB], ("x", k, tb)) for k in range(NK)],
                                      [(o[:, k, :], okk + (k,)) for k in range(NK)], final_g, TB, D, st_holder[0])
                            for k in range(NK):
                                ftoks.append(kb.dma("sp", out_d[k * 128:(k + 1) * 128, tb * TB:(tb + 1) * TB], o[:, k, :],
                                                    R=[okk + (k,)]))
                for t in ftoks:
                    kb.wait_tok("sp", t)
                if post:
                    post()
                kb.flush()

        def final_out(gname):
            with ExitStack() as ss:
                st = mk_norm_tmp(ss)
                ob = [sb(f"ob{i}", [128, NK, TB], F32, ss) for i in range(2)]
                toks = []
                for tb in range(NTB):
                    c0 = HALO + tb * TB
                    o, okk = ob[tb % 2], ("ob", tb % 2)
                    if gname is None:
                        for k in range(NK):
                            kb.op("act", ACTF(o[:, k, :], xT[:, k, c0:c0 + TB], AF.Copy), R=[("x", k, tb)], W=[okk + (k,)])
                    else:
                        rms_block([(xT[:, k, c0:c0 + TB], ("x", k, tb)) for k in range(NK)],
                                  [(o[:, k, :], okk + (k,)) for k in range(NK)], gname, TB, D, st)
                    for k in range(NK):
                        toks.append(kb.dma("sp", out_d[k * 128:(k + 1) * 128, tb * TB:(tb + 1) * TB], o[:, k, :],
                                           R=[okk + (k,)]))
                for t in toks:
                    kb.wait_tok("sp", t)
                kb.flush()

        def odd_mixer(sub=9, post=None):
            w_in = W["od_w_in"][0]
            w_out = W["od_w_out"][0]
            OQ, OKk, OV, OO, OG, OCQ, OCKV, OKR = 0, 512, 1024, 1536, 2048, 2056, 2440, 2696
            gb = COLS["gate_bias"]
            P = slice(64, 96)
            SC = 128.0 ** -0.5
            XK = [("x", k) for k in range(NK)]
            def HBt(tb):
                return [("hb", k, tb) for k in range(NK)]
            with ExitStack() as s1:
                s2 = ExitStack()
                hT1 = sb("hT1", [128, NK, T], BF16, s1)
                state = sb("state", [128, 4, 256], F32, s1)
                gI, gNB, gNLF, gEG, gW = [sb(n, [128, 16, 4], F32, s1) for n in ("gI", "gNB", "gNLF", "gEG", "gW")]
                wG = sb("wG", [128, NK, 8], BF16, s1)
                epsc = sb("epsc2", [128, 1], F32, s1)
                cqn = sb("cqn", [128, 3, T], BF16, s2)
                ckvn = sb("ckvn", [128, 2, 2 * T], BF16, s2)
                KT = sb("KT", [128, 2 * T], BF16, s2)
                ctab = sb("ctab", [128, T], BF16, s2)
                stab = sb("stab", [128, T], BF16, s2)
                kb.op("dve", MEMSET(epsc[:], EPS), W=["epsc2"])
                kb.dma("pool", wG[:], w_in.rearrange("(kc p) n -> p kc n", p=128)[:, :, OG:OG + 8], W=["wG"])
                for h in range(4):
                    kb.op("dve", MEMSET(state[:, h, :], 0.0), W=[("state", h)])

                def rope_tables(ss):
                    posi = sb("posi", [128, TB], I32, ss)
                    ang = sb("ang", [128, TB], F32, ss)
                    rr = sb("rr", [128, TB], F32, ss)
                    tmp = sb("rtmp", [128, TB], F32, ss)
                    ki = sb("ki", [128, TB], I32, ss)
                    ifq = cols[64:96, COLS["inv_freq"]:COLS["inv_freq"] + 1]
                    sgn = cols[64:96, COLS["rope_sign"]:COLS["rope_sign"] + 1]
                    for tb in range(NTB):
                        cs = slice(tb * TB, (tb + 1) * TB)
                        kb.dma("sp", posi[P, :], pos_d[64:96, cs], W=["posi"])
                        kb.op("dve", COPY(ang[P, :], posi[P, :]), R=["posi"], W=["ang"])
                        kb.op("dve", TS(ang[P, :], ang[P, :], ifq, ALU.mult), R=["ang", "cols"], W=["ang"])
                        for dst, dk, shift, signed in ((stab, "stab", 0.0, True), (ctab, "ctab", PI / 2, False)):
                            kb.op("dve", TS(tmp[P, :], ang[P, :], shift, ALU.add, 1.0 / TWO_PI, ALU.mult), R=["ang"], W=["rtmp"])
                            kb.op("dve", COPY(ki[P, :], tmp[P, :]), R=["rtmp"], W=["ki"])
                            kb.op("dve", COPY(tmp[P, :], ki[P, :]), R=["ki"], W=["rtmp"])
                            kb.op("dve", STT(rr[P, :], tmp[P, :], -TWO_PI, ang[P, :], ALU.mult, ALU.add),
                                  R=["rtmp", "ang"], W=["rr"])
                            if shift:
                                kb.op("dve", TS(rr[P, :], rr[P, :], shift, ALU.add), R=["rr"], W=["rr"])
                            kb.op("dve", TS(tmp[P, :], rr[P, :], PI, ALU.is_gt, -TWO_PI, ALU.mult), R=["rr"], W=["rtmp"])
                            kb.op("dve", TT(rr[P, :], rr[P, :], tmp[P, :], ALU.add), R=["rr", "rtmp"], W=["rr"])
                            kb.op("dve", TS(tmp[P, :], rr[P, :], -PI, ALU.is_lt, TWO_PI, ALU.mult), R=["rr"], W=["rtmp"])
                            kb.op("dve", TT(rr[P, :], rr[P, :], tmp[P, :], ALU.add), R=["rr", "rtmp"], W=["rr"])
                            kb.op("dve", TS(rr[P, :], rr[P, :], PI, ALU.min, -PI, ALU.max), R=["rr"], W=["rr"])
                            if signed:
                                kb.op("act", ACTF(tmp[P, :], rr[P, :], AF.Sin), R=["rr"], W=["rtmp"])
                                kb.op("dve", TS(dst[P, cs], tmp[P, :], sgn, ALU.mult), R=["rtmp", "cols"], W=[(dk, tb)])
                            else:
                                kb.op("act", ACTF(dst[P, cs], rr[P, :], AF.Sin), R=["rr"], W=[(dk, tb)])

                def gates(tb, psG, pkG, gf, ge, gt):
                    cs = slice(tb * 4, tb * 4 + 4)
                    gk = ("g", tb)
                    pg = psG[:, 0:32].rearrange("p (c g) -> p c g", g=8)
                    gb3 = cols[:, gb:gb + 32].rearrange("p (c g) -> p c g", g=8)
                    gbi = gb3[:, :, 0:4]
                    gbf = gb3[:, :, 4:8]
                    kb.op("dve", TT(gI[:, cs, :], pg[:, :, 0:4], gbi, ALU.add), R=[pkG, "cols"], W=[gk + ("I",)])
                    kb.op("dve", TT(gf[:, :, :], pg[:, :, 4:8], gbf, ALU.add), R=[pkG, "cols"], W=["gf"])
                    kb.op("act", ACTF(ge[:, :, :], gf[:, :, :], AF.Exp, scale=-1.0), R=["gf"], W=["ge"])
                    kb.op("act", ACTF(gNLF[:, cs, :], ge[:, :, :], AF.Ln, bias=ones_f[:, 0:1]), R=["ge", "ones_f"], W=[gk + ("NLF",)])
                    nlf16 = gNLF[:, cs, :].rearrange("p c g -> p (c g)")
                    psNB, pkNB = kb.ps()
                    kb.mm(psNB[:, :16], [(tri_f[:, :], nlf16)], R=["tri_f", gk + ("NLF",)], W=[pkNB])
                    kb.op("act", ACTF(gNB[:, cs, :].rearrange("p c g -> p (c g)"), psNB[:, :16], AF.Copy), R=[pkNB], W=[gk + ("NB",)])
                    psNG, pkNG = kb.ps()
                    kb.mm(psNG[:, :16], [(ones_f[:, :], nlf16)], R=["ones_f", gk + ("NLF",)], W=[pkNG])
                    kb.op("act", ACTF(gEG[:, cs, :].rearrange("p c g -> p (c g)"), psNG[:, :16], AF.Exp, scale=-1.0), R=[pkNG], W=[gk + ("EG",)])
                    g16 = gt[:, :, :].rearrange("p c g -> p (c g)")
                    kb.op("dve", TT(gt[:, :, :], gI[:, cs, :], gNB[:, cs, :], ALU.add), R=[gk + ("I",), gk + ("NB",)], W=["gt"])
                    kb.op("dve", TT(g16, g16, psNG[:, :16], ALU.subtract), R=["gt", pkNG], W=["gt"])
                    kb.op("act", ACTF(gW[:, cs, :], gt[:, :, :], AF.Exp), R=["gt"], W=[gk + ("W",)])

                A0, A1 = wA[0], wA[1]
                wuq = A0[:, 0:2304].rearrange("p (i n) -> p i n", n=768)
                wuqs = A0[:, 2304:3072].rearrange("p (i h c) -> p i h c", i=3, c=32)
                wukv = A1[:, 0:2048].rearrange("p (i n) -> p i n", n=1024)
                uq_d = W["od_w_uq"][0]

                def mla_weights():
                    kb.dma("pool", wuq, uq_d.rearrange("(kc p) n -> p kc n", p=128), W=[("wA", 0)])
                    for i in range(3):
                        uq3 = uq_d[i * 128:(i + 1) * 128, :].rearrange("p (h c) -> p h c", c=96)
                        kb.dma("pool", wuqs[:, i, :, 0:16], uq3[:, :, 80:96], W=[("wA", 0)])
                        kb.dma("pool", wuqs[:, i, :, 16:32], uq3[:, :, 64:80], W=[("wA", 0)])
                    kb.dma("pool", wukv, W["od_w_ukv"][0].rearrange("(kc p) n -> p kc n", p=128), W=[("wA", 1)])

                with ExitStack() as ss:
                    st = mk_norm_tmp(ss)
                    def norm1(tb):
                        c0 = HALO + tb * TB
                        rms_block([(xT[:, k, c0:c0 + TB], ("x", k, tb)) for k in range(NK)],
                                  [(hT1[:, k, tb * TB:(tb + 1) * TB], ("hb", k, tb)) for k in range(NK)], "norm_mix1", TB, D, st)
                    norm1(0)
                    rope_tables(ss)
                    lat = sb("lat", [128, 3, TB], F32, ss)
                    lat2_f = sb("lat2", [128, 4, 256], F32, ss)
                    lat2 = lat2_f[:, :, :].rearrange("p a b -> p (a b)").rearrange("p (i t) -> p i t", t=TB)
                    kra = sb("kra", [128, TB], F32, ss)
                    krb = sb("krb", [128, TB], F32, ss)
                    vaug = [sb(f"vaug{i}", [128, 4, 256], BF16, ss) for i in range(2)]
                    kw = [sb(f"kw{i}", [128, 4, 128], BF16, ss) for i in range(2)]
                    gf = sb("gf", [128, 4, 4], F32, ss)
                    ge = sb("ge", [128, 4, 4], F32, ss)
                    gt = sb("gt", [128, 4, 4], F32, ss)
                    for i in range(2):
                        kb.op("dve", MEMSET(vaug[i][:, :, 128:256], 1.0), W=[("vaug1", i)])
                    t1, k1 = loadA(w_in, [(OCQ, 384), (OKR, 32), (OKR + 16, 16), (OKR, 16)], name="oddL1")
                    t2, k2 = loadA(w_in, [(OCKV, 256)], name="oddL2")
                    for tb in range(NTB):
                        own = slice(T + tb * TB, T + (tb + 1) * TB)
                        hb = hT1[:, :, tb * TB:(tb + 1) * TB]
                        HB = HBt(tb)
                        if tb + 1 < NTB:
                            norm1(tb + 1)
                        for i in range(3):
                            ps, pk = kb.ps()
                            kb.mm(ps[:, :], [(t1[:, k, i * 128:(i + 1) * 128], hb[:, k, :]) for k in range(NK)], R=[k1] + HB, W=[pk])
                            kb.op("act", ACTF(lat[:, i, :], ps[:, :], AF.Copy), R=[pk], W=[("lat", i)])
                        rms_block([(lat[:, i, :], ("lat", i)) for i in range(3)],
                                  [(cqn[:, i, tb * TB:(tb + 1) * TB], ("cqn", tb)) for i in range(3)], "q_norm", TB, 384, st)
                        pst, pkt = kb.ps()
                        kb.mm(pst[P, :], [(t1[:, k, 384:416], hb[:, k, :]) for k in range(NK)], R=[k1] + HB, W=[pkt])
                        pss, pks = kb.ps()
                        kb.mm(pss[P, :], [(t1[:, k, 416:448], hb[:, k, :]) for k in range(NK)], R=[k1] + HB, W=[pks])
                        kb.op("dve", TT(kra[P, :], pst[P, :], ctab[P, tb * TB:(tb + 1) * TB], ALU.mult), R=[pkt, ("ctab", tb)], W=["kra"])
                        kb.op("dve", TT(krb[P, :], pss[P, :], stab[P, tb * TB:(tb + 1) * TB], ALU.mult), R=[pks, ("stab", tb)], W=["krb"])
                        kb.op("dve", TT(KT[P, own], kra[P, :], krb[P, :], ALU.add), R=["kra", "krb"], W=[("KTr", 4 + tb)])
                        for i in range(2):
                            ps, pk = kb.ps()
                            kb.mm(ps[:, :], [(t2[:, k, i * 128:(i + 1) * 128], hb[:, k, :]) for k in range(NK)], R=[k2] + HB, W=[pk])
                            kb.op("dve", COPY(lat2[:, i, :], ps[:, :]), R=[pk], W=[("lat2", i)])
                        rms_block([(lat2[:, i, :], ("lat2", i)) for i in range(2)],
                                  [(ckvn[:, i, own], ("ckvn", 4 + tb)) for i in range(2)], "kv_norm", TB, 256, st)
                    tK, kK = loadA(w_in, [(OKk, 512)])
                    tV, kV = loadA(w_in, [(OV, 512)])
                    kb.dma("sp", cin_kv.rearrange("p (i t) -> p i t", i=2), ckvn[:, :, T:2 * T], R=[("ckvn", 4 + tb) for tb in range(4)], W=["cin_kv"])
                    kb.dma("sp", cin_kr[:, :], KT[P, T:2 * T], R=[("KTr", 4 + tb) for tb in range(4)], W=["cin_kr"])
                    kb.cc(cin_kv[:, :], cout_kv[:, :], R=["cin_kv"], W=["cout_kv"])
                    kb.cc(cin_kr[:, :], cout_kr[:, :], R=["cin_kr"], W=["cout_kr"])
                    kb.dma("sp", ckvn[:, :, 0:T], cout_kv[0:128, :].rearrange("p (i t) -> p i t", i=2), R=["cout_kv"],
                           W=[("ckvn", b) for b in range(4)])
                    kb.dma("sp", KT[P, 0:T], cout_kr[0:32, :], R=["cout_kr"], W=[("KTr", b) for b in range(4)])
                    for tb in range(NTB):
                        hb = hT1[:, :, tb * TB:(tb + 1) * TB]
                        HB = HBt(tb)
                        psG, pkG = kb.ps()

                        def fng(e, psG=psG, hb=hb):
                            for cc in range(4):
                                for k in range(NK):
                                    ins = e.matmul(psG[:, cc * 8:(cc + 1) * 8], hb[:, k, cc * 128:(cc + 1) * 128], wG[:, k, :],
                                                   start=(k == 0), stop=(k == NK - 1))
                            return ins
                        kb.op("pe", fng, R=["wG"] + HB, W=[pkG])
                        gates(tb, psG, pkG, gf, ge, gt)
                        for cc in range(4):
                            c = tb * 4 + cc
                            t0 = cc * 128
                            psK, pkK = kb.ps()
                            kb.mm(psK[:, :], [(hb[:, k, t0:t0 + 128], tK[:, k, :]) for k in range(NK)], R=[kK] + HB, W=[pkK])
                            psV, pkV = kb.ps()
                            kb.mm(psV[:, :], [(hb[:, k, t0:t0 + 128], tV[:, k, :]) for k in range(NK)], R=[kV] + HB, W=[pkV])
                            kwt, kwk = kw[c % 2], ("kw", c % 2)
                            vat, vak = vaug[c % 2], ("vaug", c % 2)
                            for h in range(4):
                                kb.op("dve", TS(kwt[:, h, :], psK[:, h * 128:(h + 1) * 128], gW[:, c, h:h + 1], ALU.mult, SC, ALU.mult),
                                      R=[pkK, ("g", c // 4, "W")], W=[kwk + (h,)])
                                kb.op("act", ACTF(vat[:, h, 0:128], psV[:, h * 128:(h + 1) * 128], AF.Copy), R=[pkV], W=[vak + (h,)])
                                psD, pkD = kb.ps()
                                kb.mm(psD[:, :256], [(kwt[:, h, :], vat[:, h, :])], R=[kwk + (h,), vak + (h,), ("vaug1", c % 2)], W=[pkD])
                                kb.op("dve", STT(state[:, h, :], state[:, h, :], gEG[:, c, h:h + 1], psD[:, :256], ALU.mult, ALU.add),
                                      R=[pkD, ("g", c // 4, "EG"), ("state", h)], W=[("state", h)])
                    mla_weights()
                    kb.dma("sp", cin_st.rearrange("p (h n) -> p h n", h=4), state[:, :, :], R=[("state", h) for h in range(4)], W=["cin_st"])
                    kb.cc(cin_st[:, :], cout_st[:, :], R=["cin_st"], W=["cout_st"])
                    kb.flush()
                if sub < 2:
                    return

                with ExitStack() as ss:
                    Vaug = sb("Vaug", [128, 32, 128], BF16, ss)
                    QTs = [sb(f"QT{i}", [128, T], BF16, ss) for i in range(2)]
                    PT = [sb(f"PT{i}", [128, TB], BF16, ss) for i in range(5)]
                    accS = sb("accS", [128, TB], F32, ss)
                    rden = sb("rden2", [128, TB], F32, ss)
                    mixa = sb("mixa", [128, T], BF16, ss)
                    kra = sb("kra2", [128, TB], F32, ss)
                    krb = sb("krb2", [128, TB], F32, ss)
                    ip = 0
                    nacc = 0
                    QSC = 96.0 ** -0.5
                    def qsetup(h):
                        QTh = QTs[h % 2]
                        for tb in range(NTB):
                            cs = slice(tb * TB, (tb + 1) * TB)
                            ps, pk = kb.ps()
                            kb.mm(ps[0:64, :], [(wuq[:, i, h * 96:h * 96 + 64], cqn[:, i, cs]) for i in range(3)],
                                  R=[("wA", 0), ("cqn", tb)], W=[pk])
                            kb.op("dve", COPY(QTh[0:64, cs], ps[0:64, :]), R=[pk], W=[("QT", h % 2, tb)])
                            pst, pkt = kb.ps()
                            kb.mm(pst[P, :], [(wuq[:, i, h * 96 + 64:h * 96 + 96], cqn[:, i, cs]) for i in range(3)],
                                  R=[("wA", 0), ("cqn", tb)], W=[pkt])
                            pss, pks = kb.ps()
                            kb.mm(pss[P, :], [(wuqs[:, i, h, :], cqn[:, i, cs]) for i in range(3)],
                                  R=[("wA", 0), ("cqn", tb)], W=[pks])
                            kb.op("dve", TT(kra[P, :], pst[P, :], ctab[P, cs], ALU.mult), R=[pkt, ("ctab", tb)], W=["kra2"])
                            kb.op("dve", TT(krb[P, :], pss[P, :], stab[P, cs], ALU.mult), R=[pks, ("stab", tb)], W=["krb2"])
                            kb.op("dve", TT(QTh[P, cs], kra[P, :], krb[P, :], ALU.add), R=["kra2", "krb2"], W=[("QTr", h % 2, tb)])

                    qsetup(0)
                    for h in range(8):
                        par = h % 2
                        vo, oo = (0, 64) if par == 0 else (64, 0)
                        lo, hi = (0, 64) if par == 0 else (64, 128)
                        kb.op("pool", MEMSET(Vaug[:, 16:32, oo:oo + 64], 1.0), W=["Vaug"])
                        kb.op("pool", MEMSET(Vaug[:, 0:16, oo:oo + 64], 1.0), W=["Vaug"])
                        kb.op("dve", TS(Vaug[:, 0:16, oo:oo + 64], Vaug[:, 0:16, oo:oo + 64], col("flag"), ALU.mult, 1.0, ALU.mult), R=["Vaug", "cols"], W=["Vaug"])
                        for blk in range(8):
                            ps, pk = kb.ps()
                            bs = slice(blk * TB, (blk + 1) * TB)
                            kb.mm(ps[0:64, :], [(wukv[:, i, h * 128:h * 128 + 64], ckvn[:, i, bs]) for i in range(2)],
                                  R=[("wA", 1), ("ckvn", blk)], W=[pk])
                            kb.op("dve", COPY(KT[0:64, bs], ps[0:64, :]), R=[pk], W=[("KTn", blk)])
                        for g4 in range(8):
                            ps, pk = kb.ps()

                            def fnv(e, ps=ps, g4=g4, h=h):
                                for j in range(4):
                                    kk = g4 * 4 + j
                                    for i in range(2):
                                        ins = e.matmul(ps[:, j * 64:(j + 1) * 64], ckvn[:, i, kk * 128:(kk + 1) * 128],
                                                       wukv[:, i, h * 128 + 64:h * 128 + 128], start=(i == 0), stop=(i == 1))
                                return ins
                            kb.op("pe", fnv, R=[("wA", 1), ("ckvn", g4)], W=[pk])
                            dstv = Vaug[:, g4 * 4:(g4 + 1) * 4, vo:vo + 64]
                            srcv = ps[:, 0:256].rearrange("p (j c) -> p j c", c=64)
                            if g4 < 4:
                                kb.op("act", ACTF(dstv, srcv, AF.Copy, scale=col("flag")), R=[pk, "cols"], W=["Vaug"])
                            else:
                                kb.op("dve", COPY(dstv, srcv), R=[pk], W=["Vaug"])
                        accs = {}
                        items = [(qb, kk) for qb in range(NTB) for kk in range(16 + 4 * qb + 4)]
                        pend = []

                        def emit_pv(qb, kk, q0, nq, p, ppk, par=par, lo=lo, hi=hi, accs=accs):
                            acc, ak = accs[qb]
                            nkb = 16 + 4 * qb + 4
                            kb.op("pe", MM1(acc[:, q0:TB], Vaug[:, kk, :], p[:, :nq], kk == 0, kk == nkb - 1),
                                  R=["Vaug", ppk], W=[ak])
                            if kk == nkb - 1:
                                kb.op("dve", COPY(accS[:, :], acc[:, :]), R=[ak], W=["accS"])
                                psF, pkF = kb.ps()
                                kb.mm(psF[:, :], [((shE if par == 0 else shO)[:, :], accS[:, :])], R=["shE", "shO", "accS"], W=[pkF])
                                kb.op("act", ACTF(rden[lo:hi, :], psF[lo:hi, :], AF.Ln), R=[pkF], W=["rden2"])
                                kb.op("act", ACTF(rden[lo:hi, :], rden[lo:hi, :], AF.Exp, scale=-1.0), R=["rden2"], W=["rden2"])
                                kb.op("dve", TT(mixa[lo:hi, qb * TB:(qb + 1) * TB], accS[lo:hi, :], rden[lo:hi, :], ALU.mult),
                                      R=["accS", "rden2"], W=[("mixa", qb)])
                                kb.free(ak)
                        QT = QTs[h % 2]
                        for qb, kk in items:
                            if kk == 0:
                                accs[qb] = kb.ps(hold=True)
                                if qb == 1 and h + 1 < 8:
                                    qsetup(h + 1)
                            r = kk - (16 + 4 * qb)
                            q0 = 128 * r if r > 0 else 0
                            nq = TB - q0
                            psS, pkS = kb.ps()
                            kb.mm(psS[:, :nq], [(KT[0:96, kk * 128:(kk + 1) * 128], QT[0:96, qb * TB + q0:(qb + 1) * TB])],
                                  R=[("KTn", kk // 4), ("KTr", kk // 4), ("QT", h % 2, qb), ("QTr", h % 2, qb)], W=[pkS])
                            p, ppk = PT[ip % 5], ("PT", ip % 5)
                            ip += 1
                            kb.op("act", ACTF(p[:, :nq], psS[:, :nq], AF.Exp, scale=QSC), R=[pkS], W=[ppk])
                            if r >= 0:
                                kb.op("pool", TT(p[:, 0:128], p[:, 0:128], tri_bf[:, :], ALU.mult), R=[ppk, "tri_bf"], W=[ppk])
                            pend.append((qb, kk, q0, nq, p, ppk))
                            if len(pend) > 3:
                                emit_pv(*pend.pop(0))
                        while pend:
                            emit_pv(*pend.pop(0))
                        if par == 1:
                            bt, bk = loadB(w_out, [512 + (h // 2) * 128])
                            for tb in range(NTB):
                                contract(bt, bk, [(mixa[:, tb * TB:(tb + 1) * TB], ("mixa", tb))], tb)
                    kb.flush()
                s2.close()
                if sub < 3:
                    return

                with ExitStack() as ss:
                    kb.dma("sp", state[:, :, :], cout_st[0:128, :].rearrange("p (h n) -> p h n", h=4), R=["cout_st"],
                           W=[("state", h) for h in range(4)])
                    for h in range(4):
                        kb.op("dve", TS(state[:, h, :], state[:, h, :], col("flag"), ALU.mult), R=[("state", h), "cols"], W=[("state", h)])
                    NSL = 3
                    hmixs = [sb(f"hmix{i}", [128, 4, TB], BF16, ss) for i in range(2)]
                    wAx = [wA[0], wA[1], sb("wA2", [128, NK * 512], BF16, ss), sb("wA3", [128, NK * 512], BF16, ss)]
                    w_in3 = w_in.rearrange("(kc p) n -> p kc n", p=128)
                    slots = []
                    for i in range(NSL):
                        d = dict(qT=sb(f"mqT{i}", [128, TB], BF16, ss), kT=sb(f"mkT{i}", [128, TB], BF16, ss),
                                 sig=sb(f"msig{i}", [128, TB], F32, ss), kw4=sb(f"kw4{i}", [128, 4, 128], BF16, ss),
                                 va4=sb(f"va4{i}", [128, 4, 256], BF16, ss), nlfrep=sb(f"nlfrep{i}", [128, 4, 128], F32, ss),
                                 E1=sb(f"E1{i}", [128, TB], F32, ss), qs=sb(f"qs{i}", [128, TB], BF16, ss),
                                 Y=sb(f"Y{i}", [128, TB], F32, ss),
                                 SW=sb(f"SW{i}", [128, TB], BF16, ss), sqh=sb(f"sqh{i}", [128, TB], BF16, ss),
                                 Sbf=[sb(f"Sbf{i}{j}", [128, 256], BF16, ss) for j in range(2)], i=i,
                                 )
                        kb.op("dve", MEMSET(d["va4"][:, :, 128:256], 1.0), W=[("va41", i)])
                        slots.append(d)
                    pieces = {}

                    def head_gen(item, sidx):
                        tb, h = item
                        S = slots[sidx]
                        si = S["i"]
                        K = lambda n: (n, si)
                        qT, kT, sig, kw4, va4, nlfrep, E1, qs, Y, SW, sqh = (S[n] for n in (
                            "qT", "kT", "sig", "kw4", "va4", "nlfrep", "E1", "qs", "Y", "SW", "sqh"))
                        Wx = Y
                        hb = hT1[:, :, tb * TB:(tb + 1) * TB]
                        HBK = HBt(tb)
                        hmix = hmixs[tb % 2]
                        R2 = nlfrep[:, :, :].rearrange("p c n -> p (c n)")
                        gk = ("g", tb)
                        if h not in pieces:
                            tpv = wAx[h][:, :].rearrange("p (kc n) -> p kc n", n=512)
                            kpk = ("wA", h) if h < 2 else ("wA2", h)
                            for i_, c0_ in enumerate((OQ, OKk, OV, OO)):
                                kb.dma("pool", tpv[:, :, i_ * 128:(i_ + 1) * 128], w_in3[:, :, c0_ + h * 128:c0_ + (h + 1) * 128], W=[kpk])
                            kb.dma("pool", wB[h // 2][:, h % 2, :], w_out[h * 128:(h + 1) * 128, :], W=[("wB", h // 2)])
                            pieces[h] = (tpv, kpk)
                        tp, kp = pieces[h]
                        yield
                        ps, pk = kb.ps()
                        kb.mm(ps[:, :], [(tp[:, k, 0:128], hb[:, k, :]) for k in range(NK)], R=[kp] + HBK, W=[pk])
                        kb.op("act", ACTF(qT[:, :], ps[:, :], AF.Copy), R=[pk], W=[K("mqT")])
                        ps, pk = kb.ps()
                        kb.mm(ps[:, :], [(tp[:, k, 128:256], hb[:, k, :]) for k in range(NK)], R=[kp] + HBK, W=[pk])
                        kb.op("act", ACTF(kT[:, :], ps[:, :], AF.Copy), R=[pk], W=[K("mkT")])
                        yield
                        ps, pk = kb.ps()
                        kb.mm(ps[:, :], [(tp[:, k, 384:512], hb[:, k, :]) for k in range(NK)], R=[kp] + HBK, W=[pk])
                        kb.op("act", ACTF(sig[:, :], ps[:, :], AF.Sigmoid), R=[pk], W=[K("msig")])
                        psK, pkK = kb.ps()

                        def fnk(e):
                            for cc in range(4):
                                for k in range(NK):
                                    ins = e.matmul(psK[:, cc * 128:(cc + 1) * 128], hb[:, k, cc * 128:(cc + 1) * 128], tp[:, k, 128:256],
                                                   start=(k == 0), stop=(k == NK - 1))
                            return ins
                        kb.op("pe", fnk, R=[kp] + HBK, W=[pkK])
                        for cc in range(4):
                            c = tb * 4 + cc
                            kb.op("dve", TS(kw4[:, cc, :], psK[:, cc * 128:(cc + 1) * 128], gW[:, c, h:h + 1], ALU.mult, SC, ALU.mult),
                                  R=[pkK, gk + ("W",)], W=[K("kw4")])
                        yield
                        psV, pkV = kb.ps()

                        def fnv2(e):
                            for cc in range(4):
                                for k in range(NK):
                                    ins = e.matmul(psV[:, cc * 128:(cc + 1) * 128], hb[:, k, cc * 128:(cc + 1) * 128], tp[:, k, 256:384],
                                                   start=(k == 0), stop=(k == NK - 1))
                            return ins
                        kb.op("pe", fnv2, R=[kp] + HBK, W=[pkV])
                        kb.op("act", ACTF(va4[:, :, 0:128], psV[:, :].rearrange("p (c n) -> p c n", n=128), AF.Copy), R=[pkV], W=[K("va4")])
                        for cc in range(4):
                            c = tb * 4 + cc
                            kb.op("dve", TS(nlfrep[:, cc, :], ones_f[:, :], gNLF[:, c, h:h + 1], ALU.mult), R=["ones_f", gk + ("NLF",)], W=[K("nlfrep")])
                        psR, pkR = kb.ps(hold=True)

                        def fnr(e):
                            for cc in range(4):
                                ins = e.matmul(psR[:, cc * 128:(cc + 1) * 128], nlfrep[:, cc, :], tri_f[:, :], start=True, stop=True)
                            return ins
                        kb.op("pe", fnr, R=[K("nlfrep"), "tri_f"], W=[pkR])
                        yield
                        kb.op("act", ACTF(E1[:, :], psR[:, :], AF.Exp, scale=-1.0), R=[pkR], W=[K("E1")])
                        kb.op("dve", TT(qs[:, :], qT[:, :], E1[:, :], ALU.mult), R=[K("mqT"), K("E1")], W=[K("qs")])
                        for cc in range(4):
                            c = tb * 4 + cc
                            cs4 = slice(cc * 128, (cc + 1) * 128)
                            kb.op("dve", TS(Y[:, cs4], psR[:, cs4], gNB[:, c, h:h + 1], ALU.subtract, 0.0, ALU.max), R=[pkR, gk + ("NB",)], W=[K("Y")])
                            kb.op("act", ACTF(Wx[:, cs4], Y[:, cs4], AF.Exp, scale=-1.0, bias=gI[:, c, h:h + 1]), R=[K("Y"), gk + ("I",)], W=[K("Y")])
                        kb.free(pkR)
                        yield
                        for cc in range(4):
                            cs4 = slice(cc * 128, (cc + 1) * 128)
                            kb.op("pool", TT(Wx[:, cs4], Wx[:, cs4], tri_f[:, :], ALU.mult), R=[K("Y"), "tri_f"], W=[K("Y")])
                        psS, pkS = kb.ps()

                        def fns(e):
                            for cc in range(4):
                                cs4 = slice(cc * 128, (cc + 1) * 128)
                                ins = e.matmul(psS[:, cs4], kT[:, cs4], qT[:, cs4], start=True, stop=True)
                            return ins
                        kb.op("pe", fns, R=[K("mkT"), K("mqT")], W=[pkS])
                        kb.op("dve", STT(SW[:, :], psS[:, :], SC, Wx[:, :], ALU.mult, ALU.mult), R=[pkS, K("Y")], W=[K("SW")])
                        yield
                        psN, pkN = kb.ps(hold=True)
                        psE, pkE = kb.ps(hold=True)
                        for cc in range(4):
                            c = tb * 4 + cc
                            cs4 = slice(cc * 128, (cc + 1) * 128)
                            sbt, sbk = S["Sbf"][cc % 2], ("Sbf", si, cc % 2)
                            kb.op("act", ACTF(sbt[:, :], state[:, h, :], AF.Copy), R=[("state", h)], W=[sbk])

                            def fnn(e, sbt=sbt, cs4=cs4, cc=cc):
                                e.matmul(psN[:, cs4], sbt[:, 0:128], qs[:, cs4], start=True, stop=False)
                                e.matmul(psN[:, cs4], va4[:, cc, 0:128], SW[:, cs4], start=False, stop=True)
                                e.matmul(psE[:, cs4], sbt[:, 128:256], qs[:, cs4], start=True, stop=False)
                                return e.matmul(psE[:, cs4], ones_bf[:, :], SW[:, cs4], start=False, stop=True)
                            kb.op("pe", fnn, R=[sbk, K("qs"), K("va4"), K("SW"), "ones_bf"], W=[pkN, pkE])
                            psD, pkD = kb.ps()
                            kb.mm(psD[:, :256], [(kw4[:, cc, :], va4[:, cc, :])], R=[K("kw4"), K("va4"), ("va41", si)], W=[pkD])
                            kb.op("dve", STT(state[:, h, :], state[:, h, :], gEG[:, c, h:h + 1], psD[:, :256], ALU.mult, ALU.add),
                                  R=[pkD, gk + ("EG",), ("state", h)], W=[("state", h)])
                            yield
                        dd, hm = E1, Y
                        kb.op("act", ACTF(dd[:, :], psE[:, :], AF.Abs), R=[pkE], W=[K("E1")])
                        kb.op("dve", TS(dd[:, :], dd[:, :], 1.0, ALU.max), R=[K("E1")], W=[K("E1")])
                        kb.op("act", ACTF(dd[:, :], dd[:, :], AF.Ln), R=[K("E1")], W=[K("E1")])
                        kb.op("act", ACTF(dd[:, :], dd[:, :], AF.Exp, scale=-1.0), R=[K("E1")], W=[K("E1")])
                        kb.op("dve", TT(hm[:, :], psN[:, :], dd[:, :], ALU.mult), R=[pkN, K("E1")], W=[K("Y")])
                        kb.free(pkN)
                        kb.free(pkE)
                        yield
                        kb.op("pool", TT(hm[:, :], hm[:, :], sig[:, :], ALU.mult), R=[K("Y"), K("msig")], W=[K("Y")])
                        kb.op("act", ACTF(sqh[:, :], hm[:, :], AF.Square), R=[K("Y")], W=[K("sqh")])
                        psQ, pkQ = kb.ps(hold=True)
                        kb.mm(psQ[:, :], [(ones_bf[:, :], sqh[:, :])], R=["ones_bf", K("sqh")], W=[pkQ])
                        yield
                        kb.op("act", ACTF(dd[:, :], psQ[:, :], AF.Ln, scale=1.0 / 128, bias=epsc[:, 0:1]), R=[pkQ, "epsc2"], W=[K("E1")])
                        kb.free(pkQ)
                        kb.op("act", ACTF(dd[:, :], dd[:, :], AF.Exp, scale=-0.5), R=[K("E1")], W=[K("E1")])
                        kb.op("dve", STT(hmix[:, h, :], hm[:, :], col("ml_norm", h), dd[:, :], ALU.mult, ALU.mult),
                              R=[K("Y"), K("E1"), "cols"], W=[("hmix", tb % 2, h)])
                        if h == 3:
                            yield
                            for o in range(NK):
                                ps, pk = kb.ps()
                                osl = slice(o * 128, (o + 1) * 128)
                                kb.mm(ps[:, :], [(wB[hh // 2][:, hh % 2, osl], hmix[:, hh, :]) for hh in range(4)],
                                      R=[("wB", 0), ("wB", 1)] + [("hmix", tb % 2, hh) for hh in range(4)], W=[pk])
                                accum_x(tb, o, ps, pk)
                                if o == 3:
                                    yield

                    run_interleaved([(tb, h) for tb in range(NTB) for h in range(4)], head_gen, nslots=NSL)
                    if post:
                        post()
                    kb.flush()

        def alloc_hT(stack):
            hT_holder[0] = sb("hT_s", [128, NK, T + HALO], BF16, stack)
            st_holder[0] = mk_norm_tmp(stack)

        OCQ_, OCKV_, OKR_ = 2056, 2440, 2696

        def pre_xattn(l):
            def f():
                PRE[f"xk{l}"] = loadA(W["xattn_wkv"][l], [(0, 512)])
                PRE[f"xv{l}"] = loadA(W["xattn_wkv"][l], [(512, 512)])
            return f

        def pre_ffn(l):
            def f():
                PRE[f"ffnA{l}_0"] = loadA(W["ffn_gu"][l], [(0, 256), (FFN_H, 256)])
                PRE[f"ffnB{l}_0"] = loadB(W["ffn_down"][l], [0, 128])
            return f

        def pre_odd():
            wi = W["od_w_in"][0]
            PRE["oddL1"] = loadA(wi, [(OCQ_, 384), (OKR_, 32), (OKR_ + 16, 16), (OKR_, 16)])
            PRE["oddL2"] = loadA(wi, [(OCKV_, 256)])

        with ExitStack() as g0:
            alloc_hT(g0)
            if stop_after >= 1:
                even_mixer(post=pre_xattn(0) if stop_after >= 2 else None)
            if stop_after >= 2:
                xattn(0, post=pre_ffn(0) if stop_after >= 3 else None)
            if stop_after >= 3:
                ffn(0, post=pre_odd if stop_after >= 4 else None)
        if stop_after >= 4:
            odd_mixer(stop_after - 3 if stop_after < 7 else 9, post=pre_xattn(1) if stop_after >= 7 else None)
        if stop_after >= 7:
            with ExitStack() as g1:
                alloc_hT(g1)
                xattn(1, post=pre_ffn(1) if stop_after >= 8 else None)
                if stop_after >= 8:
                    ffn(1, final_g="final_norm" if stop_after >= 99 else None)
        if stop_after < 99:
            final_out(None)
    return nc


def _host_consts(inputs, h):
    cols = np.zeros((128, NCOL), np.float32)

    def put(name, vec, i0=0):
        v = np.asarray(vec, np.float32).reshape(-1, 128).T
        cols[:, COLS[name] + i0:COLS[name] + i0 + v.shape[1]] = v
    for l in range(2):
        put(f"norm_mix{l}", inputs["norm_mix_g"][l])
        put(f"norm_xattn{l}", inputs["norm_xattn_g"][l])
        put(f"mem_norm{l}", inputs["mem_norm_g"][l])
        put(f"norm_ffn{l}", inputs["norm_ffn_g"][l])
    put("final_norm", inputs["final_norm_g"])
    cw = np.asarray(inputs["ev_conv_w"][0], np.float32)
    for j in range(4):
        for k in range(3):
            cols[:, COLS["conv_w"] + j * 3 + k] = cw[k, j * 128:(j + 1) * 128]
    put("pool_scale", inputs["ev_pool_scale"][0])
    put("ml_norm", inputs["od_ml_norm_g"][0])
    put("q_norm", inputs["od_q_norm_g"][0])
    put("kv_norm", inputs["od_kv_norm_g"][0])
    inv = (10000.0 ** (-np.arange(16, dtype=np.float32) / 16)).astype(np.float32)
    p = np.arange(128)
    cols[:, COLS["inv_freq"]] = inv[p % 16]
    cols[:, COLS["rope_sign"]] = np.where((p % 32) < 16, -1.0, 1.0)
    cols[:, COLS["flag"]] = float(h)
    cols[:, COLS["gate_bias"]:COLS["gate_bias"] + 32] = np.tile(np.asarray(inputs["od_gate_bias"][0], np.float32), 4)[None, :]
    for j, w in enumerate((2, 4, 8, 16)):
        t = np.arange(16)
        corr = (w / np.minimum(t + 1, w)).astype(np.float32) if h == 0 else np.ones(16, np.float32)
        cols[:, COLS["poolcorr"] + j * 16:COLS["poolcorr"] + (j + 1) * 16] = corr[None, :]
    return cols


STOP_AFTER = 99


def kernel(**inputs):
    x = np.asarray(inputs["x"], np.float32)
    mem = np.asarray(inputs["mem"], np.float32)
    pos = np.asarray(inputs["positions"], np.int32)
    B, S, _ = x.shape
    tri = np.triu(np.ones((128, 128), np.float32))
    shE = np.zeros((128, 128), np.float32)
    shO = np.zeros((128, 128), np.float32)
    for i in range(64):
        shE[64 + i, i] = 1.0
        shO[i, 64 + i] = 1.0
    wnames = ["xattn_wq", "xattn_wkv", "xattn_wo", "ffn_w_gate_up", "ffn_w_down", "ev_w_in", "ev_pool_w",
              "ev_w_out", "od_w_in", "od_w_uq", "od_w_ukv", "od_w_out"]
    wts = {n: np.ascontiguousarray(np.asarray(inputs[n], np.float32)) for n in wnames}
    in_maps = []
    for c in range(8):
        b, h = c // 2, c % 2
        xt = np.zeros((D, T + HALO), np.float32)
        xt[:, HALO:] = x[b, h * T:(h + 1) * T, :].T
        if h == 1:
            xt[:, :HALO] = x[b, T - HALO:T, :].T
        m = dict(wts)
        m["xT"] = xt
        m["memT"] = np.ascontiguousarray(mem[b].T)
        m["posb"] = np.ascontiguousarray(np.broadcast_to(pos[b, h * T:(h + 1) * T][None, :], (128, T)))
        m["cols"] = _host_consts(inputs, h)
        m["tri"] = tri
        m["shiftE"] = shE
        m["shiftO"] = shO
        in_maps.append(m)
    nc = build(STOP_AFTER)
    res = run_bass_kernel_spmd(nc, in_maps, core_ids=list(range(8)))
    out = np.zeros((B, S, D), np.float32)
    for c in range(8):
        b, h = c // 2, c % 2
        out[b, h * T:(h + 1) * T, :] = res.results[c]["outT"].T
    return out
```

```python
import numpy as np
from contextlib import ExitStack
import concourse.bass as bass
import concourse.mybir as mybir
from concourse.bass_utils import run_bass_kernel_spmd

F32, BF16, I32 = mybir.dt.float32, mybir.dt.bfloat16, mybir.dt.int32
AF = mybir.ActivationFunctionType
ALU = mybir.AluOpType


def TT(out, in0, in1, op):
    return lambda e: e.tensor_tensor(out=out, in0=in0, in1=in1, op=op)


def TS(out, in0, s1, op0, s2=None, op1=None):
    if op1 is None:
        return lambda e: e.tensor_scalar(out=out, in0=in0, scalar1=s1, scalar2=None, op0=op0)
    return lambda e: e.tensor_scalar(out=out, in0=in0, scalar1=s1, scalar2=s2, op0=op0, op1=op1)


def STT(out, in0, scalar, in1, op0, op1):
    return lambda e: e.scalar_tensor_tensor(out=out, in0=in0, scalar=scalar, in1=in1, op0=op0, op1=op1)


def ACTF(out, in_, func, scale=None, bias=None):
    kw = {}
    if scale is not None:
        kw["scale"] = scale
    if bias is not None:
        kw["bias"] = bias
    return lambda e: e.activation(out=out, in_=in_, func=func, **kw)


def RECIP(out, in_):
    return lambda e: e.reciprocal(out=out, in_=in_)


def RECIPA(out, in_, scratch):
    return lambda e: e.reciprocal_approx_accurate(out=out, in_=in_, scratch=scratch)


def MEMSET(ap, v):
    return lambda e: e.memset(ap, v)


def COPY(out, in_):
    return lambda e: e.tensor_copy(out=out, in_=in_)


def MM1(out, l, r, start, stop):
    return lambda e: e.matmul(out, l, r, start=start, stop=stop)

T = 2048
HALO = 16
TB = 512
NTB = T // TB
D = 1024
NK = 8
FFN_H = 2816
EPS = 1e-6
TWO_PI = float(2.0 * np.pi)
PI = float(np.pi)

COLS = {}
_ncol = 0


def _defcol(name, n):
    global _ncol
    COLS[name] = _ncol
    _ncol += n


for _l in range(2):
    _defcol(f"norm_mix{_l}", 8)
    _defcol(f"norm_xattn{_l}", 8)
    _defcol(f"mem_norm{_l}", 8)
    _defcol(f"norm_ffn{_l}", 8)
_defcol("final_norm", 8)
_defcol("conv_w", 12)
_defcol("pool_scale", 4)
_defcol("ml_norm", 4)
_defcol("q_norm", 3)
_defcol("kv_norm", 2)
_defcol("inv_freq", 1)
_defcol("rope_sign", 1)
_defcol("flag", 1)
_defcol("gate_bias", 32)
_defcol("poolcorr", 64)
NCOL = _ncol


class KB:
    ENG = ("pe", "act", "dve", "pool", "sp")

    def __init__(self, nc, es):
        self.nc, self.es = nc, es
        self.prog = {e: [] for e in self.ENG}
        self.sem, self.cnt = {}, {}
        self.nroll = 0
        for e in self.ENG[:4]:
            self._newsem(e)
        self.seen = {e: {} for e in self.ENG}
        self.lastw, self.reads = {}, {}
        self.dsems = [[es.enter_context(nc.semaphore(f"dq{i}")), 0] for i in range(40)]
        self.dnext = 0
        self.nbank = 0
        self.held = [False] * 8
        self.banks = [es.enter_context(nc.psum_tensor(f"bank{i}", [128, 512], F32)) for i in range(8)]

    def _newsem(self, e):
        self.sem[e] = self.es.enter_context(self.nc.semaphore(f"c_{e}_{self.nroll}"))
        self.nroll += 1
        self.cnt[e] = 0

    def ps(self, hold=False):
        for _ in range(16):
            i = self.nbank % 8
            self.nbank += 1
            if not self.held[i]:
                break
        else:
            raise RuntimeError("all PSUM banks held")
        if hold:
            self.held[i] = True
        return self.banks[i], ("bank", i)

    def free(self, key):
        self.held[key[1]] = False

    def _wait(self, eng, tok):
        sem, val, src = tok
        if src == eng and eng == "pe":
            return
        d = self.seen[eng]
        if d.get(id(sem), 0) >= val:
            return
        d[id(sem)] = val
        self.prog[eng].append(("w", sem, val))

    def _deps(self, eng, R, W):
        for k in R:
            t = self.lastw.get(k)
            if t:
                self._wait(eng, t)
        for k in W:
            t = self.lastw.get(k)
            if t:
                self._wait(eng, t)
            rd = self.reads.get(k)
            if rd:
                for src, toks in rd.items():
                    if src == eng:
                        continue
                    for t in toks:
                        self._wait(eng, t)

    def _record(self, tok, R, W):
        src = tok[2]
        for k in R:
            rd = self.reads.setdefault(k, {})
            if src == "dma":
                rd.setdefault(src, []).append(tok)
            else:
                rd[src] = [tok]
        for k in W:
            self.lastw[k] = tok
            self.reads[k] = {}

    def op(self, eng, fn, R=(), W=()):
        self._deps(eng, R, W)
        self.cnt[eng] += 1
        sem = self.sem[eng]
        tok = (sem, self.cnt[eng], eng)
        self.prog[eng].append(("o", fn, sem))
        self._record(tok, R, W)
        if self.cnt[eng] >= 30000:
            self._newsem(eng)

    def mm(self, out, pairs, R=(), W=()):
        n = len(pairs)

        def fn(e):
            for i, (l, r) in enumerate(pairs):
                ins = e.matmul(out, l, r, start=(i == 0), stop=(i == n - 1))
            return ins
        self.op("pe", fn, R, W)

    def dma(self, q, out, in_, R=(), W=()):
        self._deps(q, R, W)
        ds = self.dsems[self.dnext]
        self.dnext = (self.dnext + 1) % len(self.dsems)
        if ds[1] > 0:
            self._wait(q, (ds[0], ds[1], "dma"))
        ds[1] += 16
        tok = (ds[0], ds[1], "dma")
        self.prog[q].append(("d", out, in_, ds[0]))
        self._record(tok, R, W)
        return tok

    def cc(self, in_ap, out_ap, R=(), W=()):
        self._deps("pool", R, W)
        sem = self.es.enter_context(self.nc.semaphore(f"ccs{self.nroll}"))
        self.nroll += 1
        tok = (sem, 1, "cc")
        self.prog["pool"].append(("c", in_ap, out_ap, sem))
        self._record(tok, R, W)

    def wait_tok(self, eng, tok):
        self._wait(eng, tok)

    def raw(self, eng, fn):
        self.prog[eng].append(("r", fn))

    def flush(self):
        with self.nc.Block(no_gpsimd_drain=True) as block:
            for e, deco in (("pe", block.tensor), ("act", block.scalar), ("dve", block.vector),
                            ("pool", block.gpsimd), ("sp", block.sync)):
                items = self.prog[e]
                self.prog[e] = []

                def body(eng, items=items):
                    for it in items:
                        if it[0] == "w":
                            eng.wait_ge(it[1], it[2])
                        elif it[0] == "o":
                            it[1](eng).then_inc(it[2], 1)
                        elif it[0] == "d":
                            eng.dma_start(out=it[1], in_=it[2]).then_inc(it[3], 16)
                        elif it[0] == "c":
                            eng.collective_compute("AllGather", ALU.bypass,
                                                   replica_groups=[[0, 1], [2, 3], [4, 5], [6, 7]],
                                                   ins=[it[1]], outs=[it[2]]).then_inc(it[3])
                        else:
                            it[1](eng)
                deco(body)


def build(stop_after=99):
    nc = bass.Bass("TRN2", target_bir_lowering=False)

    def din(name, shape, dt=F32):
        return nc.dram_tensor(name, list(shape), dt, kind="ExternalInput").ap()

    xT_d = din("xT", [D, T + HALO])
    memT_d = din("memT", [D, 256])
    pos_d = din("posb", [128, T], I32)
    cols_d = din("cols", [128, NCOL])
    tri_d = din("tri", [128, 128])
    shE_d = din("shiftE", [128, 128])
    shO_d = din("shiftO", [128, 128])
    W = {}
    W["xattn_wq"] = din("xattn_wq", [2, D, 512])
    W["xattn_wkv"] = din("xattn_wkv", [2, D, 1024])
    W["xattn_wo"] = din("xattn_wo", [2, 512, D])
    W["ffn_gu"] = din("ffn_w_gate_up", [2, D, 2 * FFN_H])
    W["ffn_down"] = din("ffn_w_down", [2, FFN_H, D])
    W["ev_w_in"] = din("ev_w_in", [1, D, 2048])
    W["ev_pool_w"] = din("ev_pool_w", [1, 4, 128, 128])
    W["ev_w_out"] = din("ev_w_out", [1, D, D])
    W["od_w_in"] = din("od_w_in", [1, D, 2728])
    W["od_w_uq"] = din("od_w_uq", [1, 384, 768])
    W["od_w_ukv"] = din("od_w_ukv", [1, 256, 1024])
    W["od_w_out"] = din("od_w_out", [1, D, D])
    out_d = nc.dram_tensor("outT", [D, T], F32, kind="ExternalOutput").ap()
    cin_st = nc.dram_tensor("cin_st", [128, 1024], F32, kind="Internal").ap()
    cout_st = nc.dram_tensor("cout_st", [256, 1024], F32, kind="Internal").ap()
    cin_kv = nc.dram_tensor("cin_kv", [128, 2 * T], BF16, kind="Internal").ap()
    cout_kv = nc.dram_tensor("cout_kv", [256, 2 * T], BF16, kind="Internal").ap()
    cin_kr = nc.dram_tensor("cin_kr", [32, T], BF16, kind="Internal").ap()
    cout_kr = nc.dram_tensor("cout_kr", [64, T], BF16, kind="Internal").ap()

    with ExitStack() as es:
        kb = KB(nc, es)

        uid = [0]

        def sb(name, shape, dt, stack=es):
            uid[0] += 1
            return stack.enter_context(nc.sbuf_tensor(f"{name}_{uid[0]}", list(shape), dt))

        xT = sb("xT_s", [128, NK, T + HALO], F32)
        hT_holder = [None]
        cols = sb("cols_s", [128, NCOL], F32)
        ones_bf = sb("ones_bf", [128, 128], BF16)
        ones_f = sb("ones_f", [128, 128], F32)
        tri_f = sb("tri_f", [128, 128], F32)
        tri_bf = sb("tri_bf", [128, 128], BF16)
        shE = sb("shE", [128, 128], F32)
        shO = sb("shO", [128, 128], F32)
        wA = [sb(f"wA{i}", [128, NK * 512], BF16) for i in range(2)]
        wB = [sb(f"wB{i}", [128, 2, D], BF16) for i in range(2)]
        wAn = [0]
        wBn = [0]

        def col(name, i=0):
            c = COLS[name] + i
            return cols[:, c:c + 1]

        def nextA():
            i = wAn[0] % 2
            wAn[0] += 1
            return wA[i], ("wA", i)

        def nextB():
            i = wBn[0] % 2
            wBn[0] += 1
            return wB[i], ("wB", i)

        PRE = {}

        def loadA(src2d, segs, name=None):
            if name is not None and name in PRE:
                return PRE.pop(name)
            t, k = nextA()
            t = t[:, :].rearrange("p (kc n) -> p kc n", n=512)
            o = 0
            v = src2d.rearrange("(kc p) n -> p kc n", p=128)
            for c0, n in segs:
                kb.dma("pool", t[:, :, o:o + n], v[:, :, c0:c0 + n], W=[k])
                o += n
            return t, k

        def loadB(src2d, rows, name=None):
            if name is not None and name in PRE:
                return PRE.pop(name)
            t, k = nextB()
            for i, r0 in enumerate(rows):
                kb.dma("pool", t[:, i, :], src2d[r0:r0 + 128, :], W=[k])
            return t, k

        for k in range(NK):
            kb.dma("sp", xT[:, k, 0:HALO + TB], xT_d[k * 128:(k + 1) * 128, 0:HALO + TB], W=[("x", k, 0), ("x", k, "halo")])
        for tb in range(1, NTB):
            for k in range(NK):
                cs_ = slice(HALO + tb * TB, HALO + (tb + 1) * TB)
                kb.dma("sp", xT[:, k, cs_], xT_d[k * 128:(k + 1) * 128, cs_], W=[("x", k, tb)])
        kb.dma("sp", cols[:], cols_d[:, :], W=["cols"])
        kb.dma("sp", tri_f[:], tri_d[:, :], W=["tri_f"])
        kb.dma("sp", shE[:], shE_d[:, :], W=["shE"])
        kb.dma("sp", shO[:], shO_d[:, :], W=["shO"])
        kb.dma("pool", tri_bf[:], tri_d[:, :], W=["tri_bf"])
        kb.op("dve", MEMSET(ones_bf[:], 1.0), W=["ones_bf"])
        kb.op("dve", MEMSET(ones_f[:], 1.0), W=["ones_f"])

        def mk_norm_tmp(ss):
            st = {"sq": [sb(f"sq{i}", [128, TB], BF16, ss) for i in range(2)], "n": 0,
                  "rs": sb("rs", [128, TB], F32, ss), "eps": sb("epsc", [128, 1], F32, ss)}
            kb.op("dve", MEMSET(st["eps"][:], EPS), W=["epsc"])
            return st

        def rms_block(srcs, dsts, gname, N, Dn, st):
            nk = len(srcs)
            ps, pk = kb.ps()
            for i, (s, sk) in enumerate(srcs):
                sq, sqk = st["sq"][st["n"] % 2], ("sq", st["n"] % 2)
                st["n"] += 1
                kb.op("act", ACTF(sq[:, :N], s, AF.Square), R=[sk], W=[sqk])
                kb.op("pe", MM1(ps[:, :N], ones_bf[:], sq[:, :N], i == 0, i == nk - 1),
                      R=[sqk, "ones_bf"], W=[pk])
            rs = st["rs"]
            kb.op("act", ACTF(rs[:, :N], ps[:, :N], AF.Ln, scale=1.0 / Dn, bias=st["eps"][:, 0:1]),
                  R=[pk, "epsc"], W=["rs"])
            kb.op("act", ACTF(ps[:, :N], rs[:, :N], AF.Exp, scale=-0.5), R=["rs"], W=[pk])
            for i, ((s, sk), (d, dk)) in enumerate(zip(srcs, dsts)):
                kb.op("dve", STT(d, s, col(gname, i), ps[:, :N], ALU.mult, ALU.mult),
                      R=[sk, pk, "cols"], W=[dk])

        st_holder = [None]

        def hkeys(tb):
            return [("h", k, tb) for k in range(NK)]

        def norm_block(gname, tb):
            hT = hT_holder[0]
            c0, n = (0, HALO) if tb == "halo" else (HALO + tb * TB, TB)
            srcs = [(xT[:, k, c0:c0 + n], ("x", k, tb)) for k in range(NK)]
            dsts = [(hT[:, k, c0:c0 + n], ("h", k, tb)) for k in range(NK)]
            rms_block(srcs, dsts, gname, n, D, st_holder[0])

        def accum_x(tb, o, ps, pk):
            xs = xT[:, o, HALO + tb * TB: HALO + (tb + 1) * TB]
            kb.op("dve", TT(xs, xs, ps[:, :], ALU.add), R=[pk, ("x", o, tb)], W=[("x", o, tb)])

        def contract(wt, wk, chunks, tb):
            for o in range(NK):
                ps, pk = kb.ps()
                kb.mm(ps[:, :], [(wt[:, c, o * 128:(o + 1) * 128], a) for c, (a, _) in enumerate(chunks)],
                      R=[wk] + [k for _, k in chunks], W=[pk])
                accum_x(tb, o, ps, pk)

        def run_interleaved(work, make_gen, nslots=2, pre=None):
            active = []
            free_slots = list(range(nslots))
            wi = 0
            while wi < len(work) or active:
                while wi < len(work) and free_slots:
                    if pre is not None:
                        pre(work[wi])
                    sidx = free_slots.pop(0)
                    active.append((make_gen(work[wi], sidx), sidx))
                    wi += 1
                for g in list(active):
                    try:
                        next(g[0])
                    except StopIteration:
                        active.remove(g)
                        free_slots.append(g[1])

        HK = [("h", k) for k in range(NK)]

        def even_mixer(post=None):
            hT = hT_holder[0]
            w_in = W["ev_w_in"][0]
            w_out = W["ev_w_out"][0]
            with ExitStack() as ss:
                poolw = sb("poolw", [128, 4, 128], BF16, ss)
                kb.dma("pool", poolw[:], W["ev_pool_w"][0].rearrange("g c d -> c g d"), W=["poolw"])
                NW = TB + HALO
                SL = []
                for i in range(4):
                    SL.append(dict(z=sb(f"z{i}", [128, 4, NW], F32, ss), u=sb(f"u{i}", [128, NW], F32, ss),
                                   acc=sb(f"cacc{i}", [128, TB], F32, ss), s1=sb(f"s1{i}", [128, NW], F32, ss),
                                   s2=sb(f"s2{i}", [128, NW], F32, ss), pm=sb(f"pm{i}", [128, TB], BF16, ss),
                                   mix=sb(f"mix{i}", [128, 2, TB], BF16, ss)))
                pieces = {}

                def gen(item, si):
                    j, tb = item
                    S = SL[si]
                    zt, u, acc, s1, s2, pmt, mx = S["z"], S["u"], S["acc"], S["s1"], S["s2"], S["pm"], S["mix"]
                    K = lambda *n: n + (si,)
                    if j not in pieces:
                        pieces[j] = (loadA(w_in, [(j * 128, 128), (512 + j * 128, 128), (1024 + j * 128, 128), (1536 + j * 128, 128)]),
                                     loadB(w_out, [j * 128, 512 + j * 128]))
                    (wt, wk), (bt, bk) = pieces[j]
                    win = (2, 4, 8, 16)[j]
                    c0 = tb * TB
                    HKA = hkeys(tb)
                    HKB = hkeys(tb - 1 if tb > 0 else "halo")
                    for i in range(4):
                        psA, pkA = kb.ps()
                        kb.mm(psA[:, :], [(wt[:, k, i * 128:(i + 1) * 128], hT[:, k, c0 + HALO:c0 + HALO + TB])
                                          for k in range(NK)], R=[wk] + HKA, W=[pkA])
                        psB, pkB = kb.ps()
                        kb.mm(psB[:, :HALO], [(wt[:, k, i * 128:(i + 1) * 128], hT[:, k, c0:c0 + HALO])
                                              for k in range(NK)], R=[wk] + HKB, W=[pkB])
                        kb.op("act", ACTF(zt[:, i, HALO:], psA[:, :], AF.Copy), R=[pkA], W=[K("z", i)])
                        kb.op("act", ACTF(zt[:, i, :HALO], psB[:, :HALO], AF.Copy), R=[pkB], W=[K("z", i, "h")])
                        if i % 2 == 1:
                            yield

                    def zkeys(i):
                        return [K("z", i), K("z", i, "h")]
                    kb.op("dve", TT(u[:, :], zt[:, 1, :], zt[:, 2, :], ALU.mult), R=zkeys(1) + zkeys(2), W=[K("u")])
                    kb.op("dve", TS(acc[:, :], u[:, HALO - 2:HALO - 2 + TB], col("conv_w", j * 3 + 0), ALU.mult),
                          R=[K("u"), "cols"], W=[K("cacc")])
                    for kk in (1, 2):
                        kb.op("dve", STT(acc[:, :], u[:, HALO - 2 + kk:HALO - 2 + kk + TB],
                                         col("conv_w", j * 3 + kk), acc[:, :], ALU.mult, ALU.add),
                              R=[K("u"), K("cacc"), "cols"], W=[K("cacc")])
                    kb.op("dve", TT(mx[:, 0, :], zt[:, 0, HALO:], acc[:, :], ALU.mult),
                          R=zkeys(0) + [K("cacc")], W=[K("mix", 0)])
                    yield
                    src, srck = zt[:, 3, :], zkeys(3)
                    bufs = [(s1, K("s1")), (s2, K("s2"))]
                    bi = 0
                    step = 1
                    while step < win:
                        dst, dk = bufs[bi]
                        bi ^= 1
                        kb.op("pool", COPY(dst[:, 0:step], src[:, 0:step]), R=srck, W=[dk])
                        kb.op("pool", TT(dst[:, step:NW], src[:, step:NW], src[:, 0:NW - step], ALU.add),
                              R=srck, W=[dk])
                        src, srck = dst[:, :], [dk]
                        step *= 2
                    yield
                    if tb == 0:
                        pc = COLS["poolcorr"] + j * 16
                        kb.op("dve", TT(src[:, HALO:2 * HALO], src[:, HALO:2 * HALO], cols[:, pc:pc + 16], ALU.mult),
                              R=srck + ["cols"], W=srck[:1])
                    kb.op("dve", STT(pmt[:, :], src[:, HALO:], 1.0 / win, zt[:, 3, HALO:], ALU.mult, ALU.subtract),
                          R=srck + zkeys(3), W=[K("pm")])
                    psP, pkP = kb.ps()
                    kb.mm(psP[:, :], [(poolw[:, j, :], pmt[:, :])], R=["poolw", K("pm")], W=[pkP])
                    kb.op("act", ACTF(mx[:, 1, :], psP[:, :], AF.Copy, scale=col("pool_scale", j)),
                          R=[pkP, "cols"], W=[K("mix", 1)])
                    yield
                    for o in range(NK):
                        ps, pk = kb.ps()
                        kb.mm(ps[:, :], [(bt[:, c, o * 128:(o + 1) * 128], mx[:, c, :]) for c in range(2)],
                              R=[bk, K("mix", 0), K("mix", 1)], W=[pk])
                        accum_x(tb, o, ps, pk)
                        if o == 3:
                            yield
                def pre(item):
                    j, tb = item
                    if j == 0:
                        if tb == 0:
                            norm_block("norm_mix0", "halo")
                        norm_block("norm_mix0", tb)
                run_interleaved([(j, tb) for j in range(4) for tb in range(NTB)], gen, nslots=4, pre=pre)
                if post:
                    post()
                kb.flush()

        def xattn(l, post=None):
            hT = hT_holder[0]
            wq = W["xattn_wq"][l]
            wkv = W["xattn_wkv"][l]
            wo = W["xattn_wo"][l]
            with ExitStack() as ss:
                memf = sb("memf", [128, NK, 256], F32, ss)
                memn = sb("memn", [128, NK, 256], BF16, ss)
                kT = sb("kT", [128, 4, 256], BF16, ss)
                vt = sb("vt", [128, 2, 512], BF16, ss)
                st = st_holder[0]
                for k in range(NK):
                    kb.dma("sp", memf[:, k, :], memT_d[k * 128:(k + 1) * 128, :], W=[("memf", k)])
                rms_block([(memf[:, k, :], ("memf", k)) for k in range(NK)],
                          [(memn[:, k, :], ("memn", k)) for k in range(NK)], f"mem_norm{l}", 256, D, st)
                mk_all = [("memn", k) for k in range(NK)]
                wt, wk = loadA(wkv, [(0, 512)], name=f"xk{l}")
                for h in range(4):
                    ps, pk = kb.ps()
                    kb.mm(ps[:, :256], [(wt[:, k, h * 128:(h + 1) * 128], memn[:, k, :]) for k in range(NK)],
                          R=[wk] + mk_all, W=[pk])
                    kb.op("act", ACTF(kT[:, h, :], ps[:, :256], AF.Copy), R=[pk], W=[("kT", h)])
                wt, wk = loadA(wkv, [(512, 512)], name=f"xv{l}")
                for mc in range(2):
                    ps, pk = kb.ps()
                    kb.mm(ps[:, :], [(memn[:, k, mc * 128:(mc + 1) * 128], wt[:, k, :]) for k in range(NK)],
                          R=[wk] + mk_all, W=[pk])
                    kb.op("act", ACTF(vt[:, mc, :], ps[:, :], AF.Copy), R=[pk], W=[("vt", mc)])
                wqt, wqk = loadA(wq, [(0, 512)])
                qT = [sb(f"qT{i}", [128, TB], BF16, ss) for i in range(4)]
                pT = [sb(f"pT{i}", [128, 2, TB], BF16, ss) for i in range(4)]
                oh = [sb(f"oh{i}", [128, 4, TB], BF16, ss) for i in range(2)]
                rdens = [sb(f"rden{i}", [128, TB], F32, ss) for i in range(4)]
                scale = 128.0 ** -0.5
                pieces = {}

                def gen(item, si):
                    tb, hp, hh = item
                    h = hp * 2 + hh
                    if hp not in pieces:
                        pieces[hp] = loadB(wo, [hp * 256, hp * 256 + 128])
                    bt, bk = pieces[hp]
                    c0 = HALO + tb * TB
                    oi = tb % 2
                    ot, ok = oh[oi], ("oh", oi)
                    q, qk = qT[si], ("qT", si)
                    p, pkk = pT[si], ("pT", si)
                    rden, rk = rdens[si], ("rden", si)
                    ps, pk = kb.ps()
                    kb.mm(ps[:, :], [(wqt[:, k, h * 128:(h + 1) * 128], hT[:, k, c0:c0 + TB]) for k in range(NK)],
                          R=[wqk] + hkeys(tb), W=[pk])
                    kb.op("act", ACTF(q[:, :], ps[:, :], AF.Copy), R=[pk], W=[qk])
                    yield
                    for mc in range(2):
                        ps2, pk2 = kb.ps()
                        kb.mm(ps2[:, :], [(kT[:, h, mc * 128:(mc + 1) * 128], q[:, :])], R=[("kT", h), qk], W=[pk2])
                        kb.op("act", ACTF(p[:, mc, :], ps2[:, :], AF.Exp, scale=scale), R=[pk2], W=[pkk + (mc,)])
                    yield
                    pso, pko = kb.ps()
                    kb.mm(pso[:, :], [(vt[:, mc, h * 128:(h + 1) * 128], p[:, mc, :]) for mc in range(2)],
                          R=[("vt", 0), ("vt", 1), pkk + (0,), pkk + (1,)], W=[pko])
                    psd, pkd = kb.ps()
                    kb.mm(psd[:, :], [(ones_bf[:, :], p[:, mc, :]) for mc in range(2)],
                          R=["ones_bf", pkk + (0,), pkk + (1,)], W=[pkd])
                    kb.op("act", ACTF(rden[:, :], psd[:, :], AF.Ln), R=[pkd], W=[rk])
                    kb.op("act", ACTF(rden[:, :], rden[:, :], AF.Exp, scale=-1.0), R=[rk], W=[rk])
                    kb.op("dve", TT(ot[:, h, :], pso[:, :], rden[:, :], ALU.mult), R=[pko, rk], W=[ok + (h,)])
                    if h == 3:
                        yield
                        (b0, k0), (b1, k1) = pieces[0], pieces[1]
                        for o in range(NK):
                            ps, pk = kb.ps()
                            osl = slice(o * 128, (o + 1) * 128)
                            kb.mm(ps[:, :], [(b0[:, 0, osl], ot[:, 0, :]), (b0[:, 1, osl], ot[:, 1, :]),
                                             (b1[:, 0, osl], ot[:, 2, :]), (b1[:, 1, osl], ot[:, 3, :])],
                                  R=[k0, k1] + [ok + (hh_,) for hh_ in range(4)], W=[pk])
                            accum_x(tb, o, ps, pk)
                            if o == 3:
                                yield
                normed = set()

                def pre(item):
                    if item[0] not in normed:
                        normed.add(item[0])
                        norm_block(f"norm_xattn{l}", item[0])
                run_interleaved([(tb, hp, hh) for tb in range(NTB) for hp in range(2) for hh in range(2)], gen, nslots=4, pre=pre)
                if post:
                    post()
                kb.flush()

        def ffn(l, post=None, final_g=None):
            hT = hT_holder[0]
            gname = f"norm_ffn{l}"
            gu = W["ffn_gu"][l]
            dn = W["ffn_down"][l]
            with ExitStack() as ss:
                sg = [sb(f"sg{i}", [128, TB], F32, ss) for i in range(2)]
                act = [sb(f"act{i}", [128, 2, TB], BF16, ss) for i in range(2)]
                it = 0
                ia = 0
                NG = FFN_H // 256
                ftoks = []
                if final_g is not None:
                    ob = [sb(f"obf{i}", [128, NK, TB], F32, ss) for i in range(2)]
                for g in range(NG):
                    wt, wk = loadA(gu, [(g * 256, 256), (FFN_H + g * 256, 256)], name=f"ffnA{l}_{g}")
                    bt, bk = loadB(dn, [g * 256, g * 256 + 128], name=f"ffnB{l}_{g}")
                    for tb in range(NTB):
                        c0 = HALO + tb * TB
                        if g == 0:
                            norm_block(gname, tb)
                        HK = hkeys(tb)
                        at, ak = act[ia % 2], ("act", ia % 2)
                        ia += 1
                        for c in range(2):
                            psg, pkg = kb.ps()
                            kb.mm(psg[:, :], [(wt[:, k, c * 128:(c + 1) * 128], hT[:, k, c0:c0 + TB]) for k in range(NK)],
                                  R=[wk] + HK, W=[pkg])
                            psu, pku = kb.ps()
                            kb.mm(psu[:, :], [(wt[:, k, 256 + c * 128:256 + (c + 1) * 128], hT[:, k, c0:c0 + TB])
                                              for k in range(NK)], R=[wk] + HK, W=[pku])
                            s, sk = sg[it % 2], ("sg", it % 2)
                            it += 1
                            kb.op("act", ACTF(s[:, :], psg[:, :], AF.Silu), R=[pkg], W=[sk])
                            kb.op("dve", TT(at[:, c, :], psu[:, :], s[:, :], ALU.mult), R=[pku, sk], W=[ak + (c,)])
                        contract(bt, bk, [(at[:, 0, :], ak + (0,)), (at[:, 1, :], ak + (1,))], tb)
                        if final_g is not None and g == NG - 1:
                            o, okk = ob[tb % 2], ("obf", tb % 2)
                            rms_block([(xT[:, k, c0:c0 + TB], ("x", k, tb)) for k in range(NK)],
                                      [(o[:, k, :], okk + (k,)) for k in range(NK)], final_g, TB, D, st_holder[0])
                            for k in range(NK):
                                ftoks.append(kb.dma("sp", out_d[k * 128:(k + 1) * 128, tb * TB:(tb + 1) * TB], o[:, k, :],
                                                    R=[okk + (k,)]))
                for t in ftoks:
                    kb.wait_tok("sp", t)
                if post:
                    post()
                kb.flush()

        def final_out(gname):
            with ExitStack() as ss:
                st = mk_norm_tmp(ss)
                ob = [sb(f"ob{i}", [128, NK, TB], F32, ss) for i in range(2)]
                toks = []
                for tb in range(NTB):
                    c0 = HALO + tb * TB
                    o, okk = ob[tb % 2], ("ob", tb % 2)
                    if gname is None:
                        for k in range(NK):
                            kb.op("act", ACTF(o[:, k, :], xT[:, k, c0:c0 + TB], AF.Copy), R=[("x", k, tb)], W=[okk + (k,)])
                    else:
                        rms_block([(xT[:, k, c0:c0 + TB], ("x", k, tb)) for k in range(NK)],
                                  [(o[:, k, :], okk + (k,)) for k in range(NK)], gname, TB, D, st)
                    for k in range(NK):
                        toks.append(kb.dma("sp", out_d[k * 128:(k + 1) * 128, tb * TB:(tb + 1) * TB], o[:, k, :],
                                           R=[okk + (k,)]))
                for t in toks:
                    kb.wait_tok("sp", t)
                kb.flush()

        def odd_mixer(sub=9, post=None):
            w_in = W["od_w_in"][0]
            w_out = W["od_w_out"][0]
            OQ, OKk, OV, OO, OG, OCQ, OCKV, OKR = 0, 512, 1024, 1536, 2048, 2056, 2440, 2696
            gb = COLS["gate_bias"]
            P = slice(64, 96)
            SC = 128.0 ** -0.5
            XK = [("x", k) for k in range(NK)]
            def HBt(tb):
                return [("hb", k, tb) for k in range(NK)]
            with ExitStack() as s1:
                s2 = ExitStack()
                hT1 = sb("hT1", [128, NK, T], BF16, s1)
                state = sb("state", [128, 4, 256], F32, s1)
                gI, gNB, gNLF, gEG, gW = [sb(n, [128, 16, 4], F32, s1) for n in ("gI", "gNB", "gNLF", "gEG", "gW")]
                wG = sb("wG", [128, NK, 8], BF16, s1)
                epsc = sb("epsc2", [128, 1], F32, s1)
                cqn = sb("cqn", [128, 3, T], BF16, s2)
                ckvn = sb("ckvn", [128, 2, 2 * T], BF16, s2)
                KT = sb("KT", [128, 2 * T], BF16, s2)
                ctab = sb("ctab", [128, T], BF16, s2)
                stab = sb("stab", [128, T], BF16, s2)
                kb.op("dve", MEMSET(epsc[:], EPS), W=["epsc2"])
                kb.dma("pool", wG[:], w_in.rearrange("(kc p) n -> p kc n", p=128)[:, :, OG:OG + 8], W=["wG"])
                for h in range(4):
                    kb.op("dve", MEMSET(state[:, h, :], 0.0), W=[("state", h)])

                def rope_tables(ss):
                    posi = sb("posi", [128, TB], I32, ss)
                    ang = sb("ang", [128, TB], F32, ss)
                    rr = sb("rr", [128, TB], F32, ss)
                    tmp = sb("rtmp", [128, TB], F32, ss)
                    ki = sb("ki", [128, TB], I32, ss)
                    ifq = cols[64:96, COLS["inv_freq"]:COLS["inv_freq"] + 1]
                    sgn = cols[64:96, COLS["rope_sign"]:COLS["rope_sign"] + 1]
                    for tb in range(NTB):
                        cs = slice(tb * TB, (tb + 1) * TB)
                        kb.dma("sp", posi[P, :], pos_d[64:96, cs], W=["posi"])
                        kb.op("dve", COPY(ang[P, :], posi[P, :]), R=["posi"], W=["ang"])
                        kb.op("dve", TS(ang[P, :], ang[P, :], ifq, ALU.mult), R=["ang", "cols"], W=["ang"])
                        for dst, dk, shift, signed in ((stab, "stab", 0.0, True), (ctab, "ctab", PI / 2, False)):
                            kb.op("dve", TS(tmp[P, :], ang[P, :], shift, ALU.add, 1.0 / TWO_PI, ALU.mult), R=["ang"], W=["rtmp"])
                            kb.op("dve", COPY(ki[P, :], tmp[P, :]), R=["rtmp"], W=["ki"])
                            kb.op("dve", COPY(tmp[P, :], ki[P, :]), R=["ki"], W=["rtmp"])
                            kb.op("dve", STT(rr[P, :], tmp[P, :], -TWO_PI, ang[P, :], ALU.mult, ALU.add),
                                  R=["rtmp", "ang"], W=["rr"])
                            if shift:
                                kb.op("dve", TS(rr[P, :], rr[P, :], shift, ALU.add), R=["rr"], W=["rr"])
                            kb.op("dve", TS(tmp[P, :], rr[P, :], PI, ALU.is_gt, -TWO_PI, ALU.mult), R=["rr"], W=["rtmp"])
                            kb.op("dve", TT(rr[P, :], rr[P, :], tmp[P, :], ALU.add), R=["rr", "rtmp"], W=["rr"])
                            kb.op("dve", TS(tmp[P, :], rr[P, :], -PI, ALU.is_lt, TWO_PI, ALU.mult), R=["rr"], W=["rtmp"])
                            kb.op("dve", TT(rr[P, :], rr[P, :], tmp[P, :], ALU.add), R=["rr", "rtmp"], W=["rr"])
                            kb.op("dve", TS(rr[P, :], rr[P, :], PI, ALU.min, -PI, ALU.max), R=["rr"], W=["rr"])
                            if signed:
                                kb.op("act", ACTF(tmp[P, :], rr[P, :], AF.Sin), R=["rr"], W=["rtmp"])
                                kb.op("dve", TS(dst[P, cs], tmp[P, :], sgn, ALU.mult), R=["rtmp", "cols"], W=[(dk, tb)])
                            else:
                                kb.op("act", ACTF(dst[P, cs], rr[P, :], AF.Sin), R=["rr"], W=[(dk, tb)])

                def gates(tb, psG, pkG, gf, ge, gt):
                    cs = slice(tb * 4, tb * 4 + 4)
                    gk = ("g", tb)
                    pg = psG[:, 0:32].rearrange("p (c g) -> p c g", g=8)
                    gb3 = cols[:, gb:gb + 32].rearrange("p (c g) -> p c g", g=8)
                    gbi = gb3[:, :, 0:4]
                    gbf = gb3[:, :, 4:8]
                    kb.op("dve", TT(gI[:, cs, :], pg[:, :, 0:4], gbi, ALU.add), R=[pkG, "cols"], W=[gk + ("I",)])
                    kb.op("dve", TT(gf[:, :, :], pg[:, :, 4:8], gbf, ALU.add), R=[pkG, "cols"], W=["gf"])
                    kb.op("act", ACTF(ge[:, :, :], gf[:, :, :], AF.Exp, scale=-1.0), R=["gf"], W=["ge"])
                    kb.op("act", ACTF(gNLF[:, cs, :], ge[:, :, :], AF.Ln, bias=ones_f[:, 0:1]), R=["ge", "ones_f"], W=[gk + ("NLF",)])
                    nlf16 = gNLF[:, cs, :].rearrange("p c g -> p (c g)")
                    psNB, pkNB = kb.ps()
                    kb.mm(psNB[:, :16], [(tri_f[:, :], nlf16)], R=["tri_f", gk + ("NLF",)], W=[pkNB])
                    kb.op("act", ACTF(gNB[:, cs, :].rearrange("p c g -> p (c g)"), psNB[:, :16], AF.Copy), R=[pkNB], W=[gk + ("NB",)])
                    psNG, pkNG = kb.ps()
                    kb.mm(psNG[:, :16], [(ones_f[:, :], nlf16)], R=["ones_f", gk + ("NLF",)], W=[pkNG])
                    kb.op("act", ACTF(gEG[:, cs, :].rearrange("p c g -> p (c g)"), psNG[:, :16], AF.Exp, scale=-1.0), R=[pkNG], W=[gk + ("EG",)])
                    g16 = gt[:, :, :].rearrange("p c g -> p (c g)")
                    kb.op("dve", TT(gt[:, :, :], gI[:, cs, :], gNB[:, cs, :], ALU.add), R=[gk + ("I",), gk + ("NB",)], W=["gt"])
                    kb.op("dve", TT(g16, g16, psNG[:, :16], ALU.subtract), R=["gt", pkNG], W=["gt"])
                    kb.op("act", ACTF(gW[:, cs, :], gt[:, :, :], AF.Exp), R=["gt"], W=[gk + ("W",)])

                A0, A1 = wA[0], wA[1]
                wuq = A0[:, 0:2304].rearrange("p (i n) -> p i n", n=768)
                wuqs = A0[:, 2304:3072].rearrange("p (i h c) -> p i h c", i=3, c=32)
                wukv = A1[:, 0:2048].rearrange("p (i n) -> p i n", n=1024)
                uq_d = W["od_w_uq"][0]

                def mla_weights():
                    kb.dma("pool", wuq, uq_d.rearrange("(kc p) n -> p kc n", p=128), W=[("wA", 0)])
                    for i in range(3):
                        uq3 = uq_d[i * 128:(i + 1) * 128, :].rearrange("p (h c) -> p h c", c=96)
                        kb.dma("pool", wuqs[:, i, :, 0:16], uq3[:, :, 80:96], W=[("wA", 0)])
                        kb.dma("pool", wuqs[:, i, :, 16:32], uq3[:, :, 64:80], W=[("wA", 0)])
                    kb.dma("pool", wukv, W["od_w_ukv"][0].rearrange("(kc p) n -> p kc n", p=128), W=[("wA", 1)])

                with ExitStack() as ss:
                    st = mk_norm_tmp(ss)
                    def norm1(tb):
                        c0 = HALO + tb * TB
                        rms_block([(xT[:, k, c0:c0 + TB], ("x", k, tb)) for k in range(NK)],
                                  [(hT1[:, k, tb * TB:(tb + 1) * TB], ("hb", k, tb)) for k in range(NK)], "norm_mix1", TB, D, st)
                    norm1(0)
                    rope_tables(ss)
                    lat = sb("lat", [128, 3, TB], F32, ss)
                    lat2_f = sb("lat2", [128, 4, 256], F32, ss)
                    lat2 = lat2_f[:, :, :].rearrange("p a b -> p (a b)").rearrange("p (i t) -> p i t", t=TB)
                    kra = sb("kra", [128, TB], F32, ss)
                    krb = sb("krb", [128, TB], F32, ss)
                    vaug = [sb(f"vaug{i}", [128, 4, 256], BF16, ss) for i in range(2)]
                    kw = [sb(f"kw{i}", [128, 4, 128], BF16, ss) for i in range(2)]
                    gf = sb("gf", [128, 4, 4], F32, ss)
                    ge = sb("ge", [128, 4, 4], F32, ss)
                    gt = sb("gt", [128, 4, 4], F32, ss)
                    for i in range(2):
                        kb.op("dve", MEMSET(vaug[i][:, :, 128:256], 1.0), W=[("vaug1", i)])
                    t1, k1 = loadA(w_in, [(OCQ, 384), (OKR, 32), (OKR + 16, 16), (OKR, 16)], name="oddL1")
                    t2, k2 = loadA(w_in, [(OCKV, 256)], name="oddL2")
                    for tb in range(NTB):
                        own = slice(T + tb * TB, T + (tb + 1) * TB)
                        hb = hT1[:, :, tb * TB:(tb + 1) * TB]
                        HB = HBt(tb)
                        if tb + 1 < NTB:
                            norm1(tb + 1)
                        for i in range(3):
                            ps, pk = kb.ps()
                            kb.mm(ps[:, :], [(t1[:, k, i * 128:(i + 1) * 128], hb[:, k, :]) for k in range(NK)], R=[k1] + HB, W=[pk])
                            kb.op("act", ACTF(lat[:, i, :], ps[:, :], AF.Copy), R=[pk], W=[("lat", i)])
                        rms_block([(lat[:, i, :], ("lat", i)) for i in range(3)],
                                  [(cqn[:, i, tb * TB:(tb + 1) * TB], ("cqn", tb)) for i in range(3)], "q_norm", TB, 384, st)
                        pst, pkt = kb.ps()
                        kb.mm(pst[P, :], [(t1[:, k, 384:416], hb[:, k, :]) for k in range(NK)], R=[k1] + HB, W=[pkt])
                        pss, pks = kb.ps()
                        kb.mm(pss[P, :], [(t1[:, k, 416:448], hb[:, k, :]) for k in range(NK)], R=[k1] + HB, W=[pks])
                        kb.op("dve", TT(kra[P, :], pst[P, :], ctab[P, tb * TB:(tb + 1) * TB], ALU.mult), R=[pkt, ("ctab", tb)], W=["kra"])
                        kb.op("dve", TT(krb[P, :], pss[P, :], stab[P, tb * TB:(tb + 1) * TB], ALU.mult), R=[pks, ("stab", tb)], W=["krb"])
                        kb.op("dve", TT(KT[P, own], kra[P, :], krb[P, :], ALU.add), R=["kra", "krb"], W=[("KTr", 4 + tb)])
                        for i in range(2):
                            ps, pk = kb.ps()
                            kb.mm(ps[:, :], [(t2[:, k, i * 128:(i + 1) * 128], hb[:, k, :]) for k in range(NK)], R=[k2] + HB, W=[pk])
                            kb.op("dve", COPY(lat2[:, i, :], ps[:, :]), R=[pk], W=[("lat2", i)])
                        rms_block([(lat2[:, i, :], ("lat2", i)) for i in range(2)],
                                  [(ckvn[:, i, own], ("ckvn", 4 + tb)) for i in range(2)], "kv_norm", TB, 256, st)
                    tK, kK = loadA(w_in, [(OKk, 512)])
                    tV, kV = loadA(w_in, [(OV, 512)])
                    kb.dma("sp", cin_kv.rearrange("p (i t) -> p i t", i=2), ckvn[:, :, T:2 * T], R=[("ckvn", 4 + tb) for tb in range(4)], W=["cin_kv"])
                    kb.dma("sp", cin_kr[:, :], KT[P, T:2 * T], R=[("KTr", 4 + tb) for tb in range(4)], W=["cin_kr"])
                    kb.cc(cin_kv[:, :], cout_kv[:, :], R=["cin_kv"], W=["cout_kv"])
                    kb.cc(cin_kr[:, :], cout_kr[:, :], R=["cin_kr"], W=["cout_kr"])
                    kb.dma("sp", ckvn[:, :, 0:T], cout_kv[0:128, :].rearrange("p (i t) -> p i t", i=2), R=["cout_kv"],
                           W=[("ckvn", b) for b in range(4)])
                    kb.dma("sp", KT[P, 0:T], cout_kr[0:32, :], R=["cout_kr"], W=[("KTr", b) for b in range(4)])
                    for tb in range(NTB):
                        hb = hT1[:, :, tb * TB:(tb + 1) * TB]
                        HB = HBt(tb)
                        psG, pkG = kb.ps()

                        def fng(e, psG=psG, hb=hb):
                            for cc in range(4):
                                for k in range(NK):
                                    ins = e.matmul(psG[:, cc * 8:(cc + 1) * 8], hb[:, k, cc * 128:(cc + 1) * 128], wG[:, k, :],
                                                   start=(k == 0), stop=(k == NK - 1))
                            return ins
                        kb.op("pe", fng, R=["wG"] + HB, W=[pkG])
                        gates(tb, psG, pkG, gf, ge, gt)
                        for cc in range(4):
                            c = tb * 4 + cc
                            t0 = cc * 128
                            psK, pkK = kb.ps()
                            kb.mm(psK[:, :], [(hb[:, k, t0:t0 + 128], tK[:, k, :]) for k in range(NK)], R=[kK] + HB, W=[pkK])
                            psV, pkV = kb.ps()
                            kb.mm(psV[:, :], [(hb[:, k, t0:t0 + 128], tV[:, k, :]) for k in range(NK)], R=[kV] + HB, W=[pkV])
                            kwt, kwk = kw[c % 2], ("kw", c % 2)
                            vat, vak = vaug[c % 2], ("vaug", c % 2)
                            for h in range(4):
                                kb.op("dve", TS(kwt[:, h, :], psK[:, h * 128:(h + 1) * 128], gW[:, c, h:h + 1], ALU.mult, SC, ALU.mult),
                                      R=[pkK, ("g", c // 4, "W")], W=[kwk + (h,)])
                                kb.op("act", ACTF(vat[:, h, 0:128], psV[:, h * 128:(h + 1) * 128], AF.Copy), R=[pkV], W=[vak + (h,)])
                                psD, pkD = kb.ps()
                                kb.mm(psD[:, :256], [(kwt[:, h, :], vat[:, h, :])], R=[kwk + (h,), vak + (h,), ("vaug1", c % 2)], W=[pkD])
                                kb.op("dve", STT(state[:, h, :], state[:, h, :], gEG[:, c, h:h + 1], psD[:, :256], ALU.mult, ALU.add),
                                      R=[pkD, ("g", c // 4, "EG"), ("state", h)], W=[("state", h)])
                    mla_weights()
                    kb.dma("sp", cin_st.rearrange("p (h n) -> p h n", h=4), state[:, :, :], R=[("state", h) for h in range(4)], W=["cin_st"])
                    kb.cc(cin_st[:, :], cout_st[:, :], R=["cin_st"], W=["cout_st"])
                    kb.flush()
                if sub < 2:
                    return

                with ExitStack() as ss:
                    Vaug = sb("Vaug", [128, 32, 128], BF16, ss)
                    QTs = [sb(f"QT{i}", [128, T], BF16, ss) for i in range(2)]
                    PT = [sb(f"PT{i}", [128, TB], BF16, ss) for i in range(5)]
                    accS = sb("accS", [128, TB], F32, ss)
                    rden = sb("rden2", [128, TB], F32, ss)
                    mixa = sb("mixa", [128, T], BF16, ss)
                    kra = sb("kra2", [128, TB], F32, ss)
                    krb = sb("krb2", [128, TB], F32, ss)
                    ip = 0
                    nacc = 0
                    QSC = 96.0 ** -0.5
                    def qsetup(h):
                        QTh = QTs[h % 2]
                        for tb in range(NTB):
                            cs = slice(tb * TB, (tb + 1) * TB)
                            ps, pk = kb.ps()
                            kb.mm(ps[0:64, :], [(wuq[:, i, h * 96:h * 96 + 64], cqn[:, i, cs]) for i in range(3)],
                                  R=[("wA", 0), ("cqn", tb)], W=[pk])
                            kb.op("dve", COPY(QTh[0:64, cs], ps[0:64, :]), R=[pk], W=[("QT", h % 2, tb)])
                            pst, pkt = kb.ps()
                            kb.mm(pst[P, :], [(wuq[:, i, h * 96 + 64:h * 96 + 96], cqn[:, i, cs]) for i in range(3)],
                                  R=[("wA", 0), ("cqn", tb)], W=[pkt])
                            pss, pks = kb.ps()
                            kb.mm(pss[P, :], [(wuqs[:, i, h, :], cqn[:, i, cs]) for i in range(3)],
                                  R=[("wA", 0), ("cqn", tb)], W=[pks])
                            kb.op("dve", TT(kra[P, :], pst[P, :], ctab[P, cs], ALU.mult), R=[pkt, ("ctab", tb)], W=["kra2"])
                            kb.op("dve", TT(krb[P, :], pss[P, :], stab[P, cs], ALU.mult), R=[pks, ("stab", tb)], W=["krb2"])
                            kb.op("dve", TT(QTh[P, cs], kra[P, :], krb[P, :], ALU.add), R=["kra2", "krb2"], W=[("QTr", h % 2, tb)])

                    qsetup(0)
                    for h in range(8):
                        par = h % 2
                        vo, oo = (0, 64) if par == 0 else (64, 0)
                        lo, hi = (0, 64) if par == 0 else (64, 128)
                        kb.op("pool", MEMSET(Vaug[:, 16:32, oo:oo + 64], 1.0), W=["Vaug"])
                        kb.op("pool", MEMSET(Vaug[:, 0:16, oo:oo + 64], 1.0), W=["Vaug"])
                        kb.op("dve", TS(Vaug[:, 0:16, oo:oo + 64], Vaug[:, 0:16, oo:oo + 64], col("flag"), ALU.mult, 1.0, ALU.mult), R=["Vaug", "cols"], W=["Vaug"])
                        for blk in range(8):
                            ps, pk = kb.ps()
                            bs = slice(blk * TB, (blk + 1) * TB)
                            kb.mm(ps[0:64, :], [(wukv[:, i, h * 128:h * 128 + 64], ckvn[:, i, bs]) for i in range(2)],
                                  R=[("wA", 1), ("ckvn", blk)], W=[pk])
                            kb.op("dve", COPY(KT[0:64, bs], ps[0:64, :]), R=[pk], W=[("KTn", blk)])
                        for g4 in range(8):
                            ps, pk = kb.ps()

                            def fnv(e, ps=ps, g4=g4, h=h):
                                for j in range(4):
                                    kk = g4 * 4 + j
                                    for i in range(2):
                                        ins = e.matmul(ps[:, j * 64:(j + 1) * 64], ckvn[:, i, kk * 128:(kk + 1) * 128],
                                                       wukv[:, i, h * 128 + 64:h * 128 + 128], start=(i == 0), stop=(i == 1))
                                return ins
                            kb.op("pe", fnv, R=[("wA", 1), ("ckvn", g4)], W=[pk])
                            dstv = Vaug[:, g4 * 4:(g4 + 1) * 4, vo:vo + 64]
                            srcv = ps[:, 0:256].rearrange("p (j c) -> p j c", c=64)
                            if g4 < 4:
                                kb.op("act", ACTF(dstv, srcv, AF.Copy, scale=col("flag")), R=[pk, "cols"], W=["Vaug"])
                            else:
                                kb.op("dve", COPY(dstv, srcv), R=[pk], W=["Vaug"])
                        accs = {}
                        items = [(qb, kk) for qb in range(NTB) for kk in range(16 + 4 * qb + 4)]
                        pend = []

                        def emit_pv(qb, kk, q0, nq, p, ppk, par=par, lo=lo, hi=hi, accs=accs):
                            acc, ak = accs[qb]
                            nkb = 16 + 4 * qb + 4
                            kb.op("pe", MM1(acc[:, q0:TB], Vaug[:, kk, :], p[:, :nq], kk == 0, kk == nkb - 1),
                                  R=["Vaug", ppk], W=[ak])
                            if kk == nkb - 1:
                                kb.op("dve", COPY(accS[:, :], acc[:, :]), R=[ak], W=["accS"])
                                psF, pkF = kb.ps()
                                kb.mm(psF[:, :], [((shE if par == 0 else shO)[:, :], accS[:, :])], R=["shE", "shO", "accS"], W=[pkF])
                                kb.op("act", ACTF(rden[lo:hi, :], psF[lo:hi, :], AF.Ln), R=[pkF], W=["rden2"])
                                kb.op("act", ACTF(rden[lo:hi, :], rden[lo:hi, :], AF.Exp, scale=-1.0), R=["rden2"], W=["rden2"])
                                kb.op("dve", TT(mixa[lo:hi, qb * TB:(qb + 1) * TB], accS[lo:hi, :], rden[lo:hi, :], ALU.mult),
                                      R=["accS", "rden2"], W=[("mixa", qb)])
                                kb.free(ak)
                        QT = QTs[h % 2]
                        for qb, kk in items:
                            if kk == 0:
                                accs[qb] = kb.ps(hold=True)
                                if qb == 1 and h + 1 < 8:
                                    qsetup(h + 1)
                            r = kk - (16 + 4 * qb)
                            q0 = 128 * r if r > 0 else 0
                            nq = TB - q0
                            psS, pkS = kb.ps()
                            kb.mm(psS[:, :nq], [(KT[0:96, kk * 128:(kk + 1) * 128], QT[0:96, qb * TB + q0:(qb + 1) * TB])],
                                  R=[("KTn", kk // 4), ("KTr", kk // 4), ("QT", h % 2, qb), ("QTr", h % 2, qb)], W=[pkS])
                            p, ppk = PT[ip % 5], ("PT", ip % 5)
                            ip += 1
                            kb.op("act", ACTF(p[:, :nq], psS[:, :nq], AF.Exp, scale=QSC), R=[pkS], W=[ppk])
                            if r >= 0:
                                kb.op("pool", TT(p[:, 0:128], p[:, 0:128], tri_bf[:, :], ALU.mult), R=[ppk, "tri_bf"], W=[ppk])
                            pend.append((qb, kk, q0, nq, p, ppk))
                            if len(pend) > 3:
                                emit_pv(*pend.pop(0))
                        while pend:
                            emit_pv(*pend.pop(0))
                        if par == 1:
                            bt, bk = loadB(w_out, [512 + (h // 2) * 128])
                            for tb in range(NTB):
                                contract(bt, bk, [(mixa[:, tb * TB:(tb + 1) * TB], ("mixa", tb))], tb)
                    kb.flush()
                s2.close()
                if sub < 3:
                    return

                with ExitStack() as ss:
                    kb.dma("sp", state[:, :, :], cout_st[0:128, :].rearrange("p (h n) -> p h n", h=4), R=["cout_st"],
                           W=[("state", h) for h in range(4)])
                    for h in range(4):
                        kb.op("dve", TS(state[:, h, :], state[:, h, :], col("flag"), ALU.mult), R=[("state", h), "cols"], W=[("state", h)])
                    NSL = 3
                    hmixs = [sb(f"hmix{i}", [128, 4, TB], BF16, ss) for i in range(2)]
                    wAx = [wA[0], wA[1], sb("wA2", [128, NK * 512], BF16, ss), sb("wA3", [128, NK * 512], BF16, ss)]
                    w_in3 = w_in.rearrange("(kc p) n -> p kc n", p=128)
                    slots = []
                    for i in range(NSL):
                        d = dict(qT=sb(f"mqT{i}", [128, TB], BF16, ss), kT=sb(f"mkT{i}", [128, TB], BF16, ss),
                                 sig=sb(f"msig{i}", [128, TB], F32, ss), kw4=sb(f"kw4{i}", [128, 4, 128], BF16, ss),
                                 va4=sb(f"va4{i}", [128, 4, 256], BF16, ss), nlfrep=sb(f"nlfrep{i}", [128, 4, 128], F32, ss),
                                 E1=sb(f"E1{i}", [128, TB], F32, ss), qs=sb(f"qs{i}", [128, TB], BF16, ss),
                                 Y=sb(f"Y{i}", [128, TB], F32, ss),
                                 SW=sb(f"SW{i}", [128, TB], BF16, ss), sqh=sb(f"sqh{i}", [128, TB], BF16, ss),
                                 Sbf=[sb(f"Sbf{i}{j}", [128, 256], BF16, ss) for j in range(2)], i=i,
                                 )
                        kb.op("dve", MEMSET(d["va4"][:, :, 128:256], 1.0), W=[("va41", i)])
                        slots.append(d)
                    pieces = {}

                    def head_gen(item, sidx):
                        tb, h = item
                        S = slots[sidx]
                        si = S["i"]
                        K = lambda n: (n, si)
                        qT, kT, sig, kw4, va4, nlfrep, E1, qs, Y, SW, sqh = (S[n] for n in (
                            "qT", "kT", "sig", "kw4", "va4", "nlfrep", "E1", "qs", "Y", "SW", "sqh"))
                        Wx = Y
                        hb = hT1[:, :, tb * TB:(tb + 1) * TB]
                        HBK = HBt(tb)
                        hmix = hmixs[tb % 2]
                        R2 = nlfrep[:, :, :].rearrange("p c n -> p (c n)")
                        gk = ("g", tb)
                        if h not in pieces:
                            tpv = wAx[h][:, :].rearrange("p (kc n) -> p kc n", n=512)
                            kpk = ("wA", h) if h < 2 else ("wA2", h)
                            for i_, c0_ in enumerate((OQ, OKk, OV, OO)):
                                kb.dma("pool", tpv[:, :, i_ * 128:(i_ + 1) * 128], w_in3[:, :, c0_ + h * 128:c0_ + (h + 1) * 128], W=[kpk])
                            kb.dma("pool", wB[h // 2][:, h % 2, :], w_out[h * 128:(h + 1) * 128, :], W=[("wB", h // 2)])
                            pieces[h] = (tpv, kpk)
                        tp, kp = pieces[h]
                        yield
                        ps, pk = kb.ps()
                        kb.mm(ps[:, :], [(tp[:, k, 0:128], hb[:, k, :]) for k in range(NK)], R=[kp] + HBK, W=[pk])
                        kb.op("act", ACTF(qT[:, :], ps[:, :], AF.Copy), R=[pk], W=[K("mqT")])
                        ps, pk = kb.ps()
                        kb.mm(ps[:, :], [(tp[:, k, 128:256], hb[:, k, :]) for k in range(NK)], R=[kp] + HBK, W=[pk])
                        kb.op("act", ACTF(kT[:, :], ps[:, :], AF.Copy), R=[pk], W=[K("mkT")])
                        yield
                        ps, pk = kb.ps()
                        kb.mm(ps[:, :], [(tp[:, k, 384:512], hb[:, k, :]) for k in range(NK)], R=[kp] + HBK, W=[pk])
                        kb.op("act", ACTF(sig[:, :], ps[:, :], AF.Sigmoid), R=[pk], W=[K("msig")])
                        psK, pkK = kb.ps()

                        def fnk(e):
                            for cc in range(4):
                                for k in range(NK):
                                    ins = e.matmul(psK[:, cc * 128:(cc + 1) * 128], hb[:, k, cc * 128:(cc + 1) * 128], tp[:, k, 128:256],
                                                   start=(k == 0), stop=(k == NK - 1))
                            return ins
                        kb.op("pe", fnk, R=[kp] + HBK, W=[pkK])
                        for cc in range(4):
                            c = tb * 4 + cc
                            kb.op("dve", TS(kw4[:, cc, :], psK[:, cc * 128:(cc + 1) * 128], gW[:, c, h:h + 1], ALU.mult, SC, ALU.mult),
                                  R=[pkK, gk + ("W",)], W=[K("kw4")])
                        yield
                        psV, pkV = kb.ps()

                        def fnv2(e):
                            for cc in range(4):
                                for k in range(NK):
                                    ins = e.matmul(psV[:, cc * 128:(cc + 1) * 128], hb[:, k, cc * 128:(cc + 1) * 128], tp[:, k, 256:384],
                                                   start=(k == 0), stop=(k == NK - 1))
                            return ins
                        kb.op("pe", fnv2, R=[kp] + HBK, W=[pkV])
                        kb.op("act", ACTF(va4[:, :, 0:128], psV[:, :].rearrange("p (c n) -> p c n", n=128), AF.Copy), R=[pkV], W=[K("va4")])
                        for cc in range(4):
                            c = tb * 4 + cc
                            kb.op("dve", TS(nlfrep[:, cc, :], ones_f[:, :], gNLF[:, c, h:h + 1], ALU.mult), R=["ones_f", gk + ("NLF",)], W=[K("nlfrep")])
                        psR, pkR = kb.ps(hold=True)

                        def fnr(e):
                            for cc in range(4):
                                ins = e.matmul(psR[:, cc * 128:(cc + 1) * 128], nlfrep[:, cc, :], tri_f[:, :], start=True, stop=True)
                            return ins
                        kb.op("pe", fnr, R=[K("nlfrep"), "tri_f"], W=[pkR])
                        yield
                        kb.op("act", ACTF(E1[:, :], psR[:, :], AF.Exp, scale=-1.0), R=[pkR], W=[K("E1")])
                        kb.op("dve", TT(qs[:, :], qT[:, :], E1[:, :], ALU.mult), R=[K("mqT"), K("E1")], W=[K("qs")])
                        for cc in range(4):
                            c = tb * 4 + cc
                            cs4 = slice(cc * 128, (cc + 1) * 128)
                            kb.op("dve", TS(Y[:, cs4], psR[:, cs4], gNB[:, c, h:h + 1], ALU.subtract, 0.0, ALU.max), R=[pkR, gk + ("NB",)], W=[K("Y")])
                            kb.op("act", ACTF(Wx[:, cs4], Y[:, cs4], AF.Exp, scale=-1.0, bias=gI[:, c, h:h + 1]), R=[K("Y"), gk + ("I",)], W=[K("Y")])
                        kb.free(pkR)
                        yield
                        for cc in range(4):
                            cs4 = slice(cc * 128, (cc + 1) * 128)
                            kb.op("pool", TT(Wx[:, cs4], Wx[:, cs4], tri_f[:, :], ALU.mult), R=[K("Y"), "tri_f"], W=[K("Y")])
                        psS, pkS = kb.ps()

                        def fns(e):
                            for cc in range(4):
                                cs4 = slice(cc * 128, (cc + 1) * 128)
                                ins = e.matmul(psS[:, cs4], kT[:, cs4], qT[:, cs4], start=True, stop=True)
                            return ins
                        kb.op("pe", fns, R=[K("mkT"), K("mqT")], W=[pkS])
                        kb.op("dve", STT(SW[:, :], psS[:, :], SC, Wx[:, :], ALU.mult, ALU.mult), R=[pkS, K("Y")], W=[K("SW")])
                        yield
                        psN, pkN = kb.ps(hold=True)
                        psE, pkE = kb.ps(hold=True)
                        for cc in range(4):
                            c = tb * 4 + cc
                            cs4 = slice(cc * 128, (cc + 1) * 128)
                            sbt, sbk = S["Sbf"][cc % 2], ("Sbf", si, cc % 2)
                            kb.op("act", ACTF(sbt[:, :], state[:, h, :], AF.Copy), R=[("state", h)], W=[sbk])

                            def fnn(e, sbt=sbt, cs4=cs4, cc=cc):
                                e.matmul(psN[:, cs4], sbt[:, 0:128], qs[:, cs4], start=True, stop=False)
                                e.matmul(psN[:, cs4], va4[:, cc, 0:128], SW[:, cs4], start=False, stop=True)
                                e.matmul(psE[:, cs4], sbt[:, 128:256], qs[:, cs4], start=True, stop=False)
                                return e.matmul(psE[:, cs4], ones_bf[:, :], SW[:, cs4], start=False, stop=True)
                            kb.op("pe", fnn, R=[sbk, K("qs"), K("va4"), K("SW"), "ones_bf"], W=[pkN, pkE])
                            psD, pkD = kb.ps()
                            kb.mm(psD[:, :256], [(kw4[:, cc, :], va4[:, cc, :])], R=[K("kw4"), K("va4"), ("va41", si)], W=[pkD])
                            kb.op("dve", STT(state[:, h, :], state[:, h, :], gEG[:, c, h:h + 1], psD[:, :256], ALU.mult, ALU.add),
                                  R=[pkD, gk + ("EG",), ("state", h)], W=[("state", h)])
                            yield
                        dd, hm = E1, Y
                        kb.op("act", ACTF(dd[:, :], psE[:, :], AF.Abs), R=[pkE], W=[K("E1")])
                        kb.op("dve", TS(dd[:, :], dd[:, :], 1.0, ALU.max), R=[K("E1")], W=[K("E1")])
                        kb.op("act", ACTF(dd[:, :], dd[:, :], AF.Ln), R=[K("E1")], W=[K("E1")])
                        kb.op("act", ACTF(dd[:, :], dd[:, :], AF.Exp, scale=-1.0), R=[K("E1")], W=[K("E1")])
                        kb.op("dve", TT(hm[:, :], psN[:, :], dd[:, :], ALU.mult), R=[pkN, K("E1")], W=[K("Y")])
                        kb.free(pkN)
                        kb.free(pkE)
                        yield
                        kb.op("pool", TT(hm[:, :], hm[:, :], sig[:, :], ALU.mult), R=[K("Y"), K("msig")], W=[K("Y")])
                        kb.op("act", ACTF(sqh[:, :], hm[:, :], AF.Square), R=[K("Y")], W=[K("sqh")])
                        psQ, pkQ = kb.ps(hold=True)
                        kb.mm(psQ[:, :], [(ones_bf[:, :], sqh[:, :])], R=["ones_bf", K("sqh")], W=[pkQ])
                        yield
                        kb.op("act", ACTF(dd[:, :], psQ[:, :], AF.Ln, scale=1.0 / 128, bias=epsc[:, 0:1]), R=[pkQ, "epsc2"], W=[K("E1")])
                        kb.free(pkQ)
                        kb.op("act", ACTF(dd[:, :], dd[:, :], AF.Exp, scale=-0.5), R=[K("E1")], W=[K("E1")])
                        kb.op("dve", STT(hmix[:, h, :], hm[:, :], col("ml_norm", h), dd[:, :], ALU.mult, ALU.mult),
                              R=[K("Y"), K("E1"), "cols"], W=[("hmix", tb % 2, h)])
                        if h == 3:
                            yield
                            for o in range(NK):
                                ps, pk = kb.ps()
                                osl = slice(o * 128, (o + 1) * 128)
                                kb.mm(ps[:, :], [(wB[hh // 2][:, hh % 2, osl], hmix[:, hh, :]) for hh in range(4)],
                                      R=[("wB", 0), ("wB", 1)] + [("hmix", tb % 2, hh) for hh in range(4)], W=[pk])
                                accum_x(tb, o, ps, pk)
                                if o == 3:
                                    yield

                    run_interleaved([(tb, h) for tb in range(NTB) for h in range(4)], head_gen, nslots=NSL)
                    if post:
                        post()
                    kb.flush()

        def alloc_hT(stack):
            hT_holder[0] = sb("hT_s", [128, NK, T + HALO], BF16, stack)
            st_holder[0] = mk_norm_tmp(stack)

        OCQ_, OCKV_, OKR_ = 2056, 2440, 2696

        def pre_xattn(l):
            def f():
                PRE[f"xk{l}"] = loadA(W["xattn_wkv"][l], [(0, 512)])
                PRE[f"xv{l}"] = loadA(W["xattn_wkv"][l], [(512, 512)])
            return f

        def pre_ffn(l):
            def f():
                PRE[f"ffnA{l}_0"] = loadA(W["ffn_gu"][l], [(0, 256), (FFN_H, 256)])
                PRE[f"ffnB{l}_0"] = loadB(W["ffn_down"][l], [0, 128])
            return f

        def pre_odd():
            wi = W["od_w_in"][0]
            PRE["oddL1"] = loadA(wi, [(OCQ_, 384), (OKR_, 32), (OKR_ + 16, 16), (OKR_, 16)])
            PRE["oddL2"] = loadA(wi, [(OCKV_, 256)])

        with ExitStack() as g0:
            alloc_hT(g0)
            if stop_after >= 1:
                even_mixer(post=pre_xattn(0) if stop_after >= 2 else None)
            if stop_after >= 2:
                xattn(0, post=pre_ffn(0) if stop_after >= 3 else None)
            if stop_after >= 3:
                ffn(0, post=pre_odd if stop_after >= 4 else None)
        if stop_after >= 4:
            odd_mixer(stop_after - 3 if stop_after < 7 else 9, post=pre_xattn(1) if stop_after >= 7 else None)
        if stop_after >= 7:
            with ExitStack() as g1:
                alloc_hT(g1)
                xattn(1, post=pre_ffn(1) if stop_after >= 8 else None)
                if stop_after >= 8:
                    ffn(1, final_g="final_norm" if stop_after >= 99 else None)
        if stop_after < 99:
            final_out(None)
    return nc


def _host_consts(inputs, h):
    cols = np.zeros((128, NCOL), np.float32)

    def put(name, vec, i0=0):
        v = np.asarray(vec, np.float32).reshape(-1, 128).T
        cols[:, COLS[name] + i0:COLS[name] + i0 + v.shape[1]] = v
    for l in range(2):
        put(f"norm_mix{l}", inputs["norm_mix_g"][l])
        put(f"norm_xattn{l}", inputs["norm_xattn_g"][l])
        put(f"mem_norm{l}", inputs["mem_norm_g"][l])
        put(f"norm_ffn{l}", inputs["norm_ffn_g"][l])
    put("final_norm", inputs["final_norm_g"])
    cw = np.asarray(inputs["ev_conv_w"][0], np.float32)
    for j in range(4):
        for k in range(3):
            cols[:, COLS["conv_w"] + j * 3 + k] = cw[k, j * 128:(j + 1) * 128]
    put("pool_scale", inputs["ev_pool_scale"][0])
    put("ml_norm", inputs["od_ml_norm_g"][0])
    put("q_norm", inputs["od_q_norm_g"][0])
    put("kv_norm", inputs["od_kv_norm_g"][0])
    inv = (10000.0 ** (-np.arange(16, dtype=np.float32) / 16)).astype(np.float32)
    p = np.arange(128)
    cols[:, COLS["inv_freq"]] = inv[p % 16]
    cols[:, COLS["rope_sign"]] = np.where((p % 32) < 16, -1.0, 1.0)
    cols[:, COLS["flag"]] = float(h)
    cols[:, COLS["gate_bias"]:COLS["gate_bias"] + 32] = np.tile(np.asarray(inputs["od_gate_bias"][0], np.float32), 4)[None, :]
    for j, w in enumerate((2, 4, 8, 16)):
        t = np.arange(16)
        corr = (w / np.minimum(t + 1, w)).astype(np.float32) if h == 0 else np.ones(16, np.float32)
        cols[:, COLS["poolcorr"] + j * 16:COLS["poolcorr"] + (j + 1) * 16] = corr[None, :]
    return cols


STOP_AFTER = 99


def kernel(**inputs):
    x = np.asarray(inputs["x"], np.float32)
    mem = np.asarray(inputs["mem"], np.float32)
    pos = np.asarray(inputs["positions"], np.int32)
    B, S, _ = x.shape
    tri = np.triu(np.ones((128, 128), np.float32))
    shE = np.zeros((128, 128), np.float32)
    shO = np.zeros((128, 128), np.float32)
    for i in range(64):
        shE[64 + i, i] = 1.0
        shO[i, 64 + i] = 1.0
    wnames = ["xattn_wq", "xattn_wkv", "xattn_wo", "ffn_w_gate_up", "ffn_w_down", "ev_w_in", "ev_pool_w",
              "ev_w_out", "od_w_in", "od_w_uq", "od_w_ukv", "od_w_out"]
    wts = {n: np.ascontiguousarray(np.asarray(inputs[n], np.float32)) for n in wnames}
    in_maps = []
    for c in range(8):
        b, h = c // 2, c % 2
        xt = np.zeros((D, T + HALO), np.float32)
        xt[:, HALO:] = x[b, h * T:(h + 1) * T, :].T
        if h == 1:
            xt[:, :HALO] = x[b, T - HALO:T, :].T
        m = dict(wts)
        m["xT"] = xt
        m["memT"] = np.ascontiguousarray(mem[b].T)
        m["posb"] = np.ascontiguousarray(np.broadcast_to(pos[b, h * T:(h + 1) * T][None, :], (128, T)))
        m["cols"] = _host_consts(inputs, h)
        m["tri"] = tri
        m["shiftE"] = shE
        m["shiftO"] = shO
        in_maps.append(m)
    nc = build(STOP_AFTER)
    res = run_bass_kernel_spmd(nc, in_maps, core_ids=list(range(8)))
    out = np.zeros((B, S, D), np.float32)
    for c in range(8):
        b, h = c // 2, c % 2
        out[b, h * T:(h + 1) * T, :] = res.results[c]["outT"].T
    return out
```
